# Optimizing a Trainium2 kernel written in Bass

```python
import math
import jax
import jax.numpy as jnp
from jax import lax
import numpy as np

D_MODEL = 4096
BATCH = 2
SEQ = 8192
DEPTH = 2

GRID_W = 64
CTX_LEN = 256
HEAD_DIM = 128
N_DIR = 2
GDN_HEADS = 12
GDN_WIDTH = GDN_HEADS * HEAD_DIM
GDN_CONV_W = 5
GDN_CHUNK = 64
FN_GROUPS = 8
FN_GROUP_DIM = 128
FN_WIDTH = FN_GROUPS * FN_GROUP_DIM
GQA_HEADS = 12
GQA_KV_HEADS = 4
GQA_GROUP = GQA_HEADS // GQA_KV_HEADS
GQA_WIDTH = GQA_HEADS * HEAD_DIM
GQA_KV_WIDTH = GQA_KV_HEADS * HEAD_DIM
Q_BLOCK = 128
ROPE_THETA = 10000.0
MIX_WIDTH = GDN_WIDTH + FN_WIDTH + GQA_WIDTH
IN_SPLITS = (3 * GDN_WIDTH, GDN_WIDTH, N_DIR * GDN_HEADS, N_DIR * GDN_HEADS,
             FN_WIDTH, GQA_WIDTH, GQA_KV_WIDTH, GQA_KV_WIDTH)
N_IN = sum(IN_SPLITS)
MOE_GROUPS = 4
MOE_EXPERTS_PER_GROUP = 8
MOE_EXPERTS = MOE_GROUPS * MOE_EXPERTS_PER_GROUP
MOE_TOP_K = 2
MOE_D_FF = 512
DEEPNORM_ALPHA = (2 * DEPTH) ** 0.25
DEEPNORM_BETA = (8 * DEPTH) ** -0.25
LN_EPS = 1e-5
RMS_EPS = 1e-6
F32 = jnp.float32

kernel_name = 'hybrid_gdn_fnet_gqa_hmoe_dit'


def _layernorm(x, g, b):
    xf = x.astype(F32)
    mu = jnp.mean(xf, -1, keepdims=True)
    var = jnp.mean(jnp.square(xf - mu), -1, keepdims=True)
    return ((xf - mu) * lax.rsqrt(var + LN_EPS)).astype(x.dtype) * g + b


def _rmsnorm(x, w):
    xf = x.astype(F32)
    return (xf * lax.rsqrt(jnp.mean(xf * xf, -1, keepdims=True) + RMS_EPS)).astype(x.dtype) * w


def _l2norm(x):
    xf = x.astype(F32)
    return (xf * lax.rsqrt(jnp.sum(xf * xf, -1, keepdims=True) + RMS_EPS)).astype(x.dtype)


def _split_in(p):
    return jnp.split(p, np.cumsum(IN_SPLITS)[:-1].tolist(), axis=-1)


def _axial_rope(rows):
    row = jnp.repeat(jnp.arange(rows, dtype=F32), GRID_W)
    col = jnp.tile(jnp.arange(GRID_W, dtype=F32), rows)
    axis_dim = HEAD_DIM // 2
    inv = ROPE_THETA ** (-jnp.arange(0, axis_dim, 2, dtype=F32) / axis_dim)
    ang = jnp.concatenate([row[:, None] * inv, col[:, None] * inv], -1)
    return jnp.cos(ang), jnp.sin(ang)


def _rope(x, cos, sin):
    x1, x2 = jnp.split(x, 2, axis=-1)
    cos = cos[:, None, :].astype(x.dtype)
    sin = sin[:, None, :].astype(x.dtype)
    return jnp.concatenate([x1 * cos - x2 * sin, x2 * cos + x1 * sin], -1)


def _short_conv(u, w):
    pad = GDN_CONV_W // 2
    return lax.conv_general_dilated(u, w[:, None, :].astype(u.dtype), window_strides=(1,),
                                    padding=[(pad, pad)], dimension_numbers=('NWC', 'WIO', 'NWC'),
                                    feature_group_count=u.shape[-1])


def _gdn_inputs(qkv, a, b, conv_w, a_log, dt_bias):
    B, T, _ = qkv.shape
    r = jax.nn.silu(_short_conv(qkv, conv_w)).reshape(B, T, 3, GDN_HEADS, HEAD_DIM)
    q = _l2norm(r[:, :, 0]) * HEAD_DIM ** -0.5
    k = _l2norm(r[:, :, 1])
    v = r[:, :, 2]
    a = a.reshape(B, T, N_DIR, GDN_HEADS).astype(F32)
    g = -jnp.exp(a_log) * jax.nn.softplus(a + dt_bias)
    beta = jax.nn.sigmoid(b.reshape(B, T, N_DIR, GDN_HEADS).astype(F32))
    return q, k, v, g, beta


def _gated_delta_chunked(q, k, v, g, beta, s0):
    B, T, H, _ = q.shape
    dv = v.shape[-1]
    n = T // GDN_CHUNK

    def chunks(t):
        t = t.astype(F32).reshape(B, n, GDN_CHUNK, H, *t.shape[3:])
        return jnp.moveaxis(t, (1, 3), (0, 2))

    qf, kf, vf, bt = chunks(q), chunks(k), chunks(v), chunks(beta)
    gc = jnp.cumsum(chunks(g), axis=-1)
    idx = jnp.arange(GDN_CHUNK)
    causal = idx[:, None] >= idx[None, :]
    strict = idx[:, None] > idx[None, :]
    decay = jnp.exp(jnp.where(causal, gc[..., :, None] - gc[..., None, :], -jnp.inf))
    kb = kf * bt[..., None]
    a_mat = jnp.where(strict, jnp.einsum('nbhcd,nbhsd->nbhcs', kb, kf) * decay, 0.0)
    rhs = jnp.concatenate([vf * bt[..., None], kb * jnp.exp(gc)[..., None]], -1)
    sol = lax.linalg.triangular_solve(jnp.eye(GDN_CHUNK, dtype=F32) + a_mat, rhs,
                                      left_side=True, lower=True, unit_diagonal=True)
    u, w = sol[..., :dv], sol[..., dv:]
    qk = jnp.einsum('nbhcd,nbhsd->nbhcs', qf, kf) * decay

    def step(S, xs):
        qi, ki, ui, wi, gi, qki = xs
        v_new = ui - jnp.einsum('bhcd,bhde->bhce', wi, S)
        o = (jnp.einsum('bhcd,bhde->bhce', qi * jnp.exp(gi)[..., None], S)
             + jnp.einsum('bhcs,bhse->bhce', qki, v_new))
        g_last = gi[..., -1:]
        S = (S * jnp.exp(g_last)[..., None]
             + jnp.einsum('bhcd,bhce->bhde', ki * jnp.exp(g_last - gi)[..., None], v_new))
        return S, o

    S, o = lax.scan(step, s0.astype(F32), (qf, kf, u, w, gc, qk))
    o = jnp.moveaxis(o, (0, 2), (1, 3)).reshape(B, T, H, dv)
    return o.astype(v.dtype), S


def _flip(t, d):
    return t[:, ::-1] if d == 1 else t


def _gdn_bidirectional(lat, ctx):
    ql, kl, vl, gl, bl = lat
    qc, kc, vc, gcx, bc = ctx
    B = ql.shape[0]
    outs_l, outs_c = [], []
    for d in range(N_DIR):
        s0 = jnp.zeros((B, GDN_HEADS, HEAD_DIM, HEAD_DIM), F32)
        oc, s_ctx = _gated_delta_chunked(_flip(qc, d), _flip(kc, d), _flip(vc, d),
                                         _flip(gcx[:, :, d], d), _flip(bc[:, :, d], d), s0)
        ol, _ = _gated_delta_chunked(_flip(ql, d), _flip(kl, d), _flip(vl, d),
                                     _flip(gl[:, :, d], d), _flip(bl[:, :, d], d), s_ctx)
        outs_l.append(_flip(ol, d))
        outs_c.append(_flip(oc, d))
    return outs_l[0] + outs_l[1], outs_c[0] + outs_c[1]


def _gdn_output(o, z, norm_w):
    B, T = o.shape[:2]
    y = _rmsnorm(o, norm_w) * jax.nn.silu(z.reshape(B, T, GDN_HEADS, HEAD_DIM))
    return y.reshape(B, T, GDN_WIDTH)


def _fourier_mix(f, fn_w):
    B, T, _ = f.shape
    fr = jnp.fft.fft2(f.astype(F32).reshape(B, T, FN_GROUPS, FN_GROUP_DIM), axes=(1, 3), norm='ortho').real
    return fr.astype(f.dtype).reshape(B, T, FN_WIDTH) @ fn_w


def _gqa_heads(q, k, v, q_norm_w, k_norm_w):
    B, T, _ = q.shape
    q = _rmsnorm(q.reshape(B, T, GQA_HEADS, HEAD_DIM), q_norm_w)
    k = _rmsnorm(k.reshape(B, T, GQA_KV_HEADS, HEAD_DIM), k_norm_w)
    return q, k, v.reshape(B, T, GQA_KV_HEADS, HEAD_DIM)


def _attend(q, k, v):
    B, Tq = q.shape[:2]
    qg = q.reshape(B, Tq, GQA_KV_HEADS, GQA_GROUP, HEAD_DIM)
    s = jnp.einsum('bqkgd,bskd->bkgqs', qg, k).astype(F32) * HEAD_DIM ** -0.5
    p = jax.nn.softmax(s, axis=-1).astype(v.dtype)
    return jnp.einsum('bkgqs,bskd->bqkgd', p, v).reshape(B, Tq, GQA_WIDTH)


def _attend_blocked(q, k, v):
    B, T = q.shape[:2]
    nb = T // Q_BLOCK
    qb = jnp.moveaxis(q.reshape(B, nb, Q_BLOCK, GQA_HEADS, HEAD_DIM), 1, 0)
    o = lax.map(lambda qi: _attend(qi, k, v), qb)
    return jnp.moveaxis(o, 0, 1).reshape(B, T, GQA_WIDTH)


def _token_mixers(pl, pc, rope, conv_w, a_log, dt_bias, gdn_norm_w, fn_w, q_norm_w, k_norm_w, need_ctx):
    qkv, z, a, b, f, q, k, v = pl
    qkv_c, z_c, a_c, b_c, f_c, q_c, k_c, v_c = pc
    o_l, o_c = _gdn_bidirectional(_gdn_inputs(qkv, a, b, conv_w, a_log, dt_bias),
                                  _gdn_inputs(qkv_c, a_c, b_c, conv_w, a_log, dt_bias))
    gdn_l = _gdn_output(o_l, z, gdn_norm_w)
    fn_l = _fourier_mix(f, fn_w)
    ql, kl, vl = _gqa_heads(q, k, v, q_norm_w, k_norm_w)
    ql, kl = _rope(ql, *rope), _rope(kl, *rope)
    qc, kc, vc = _gqa_heads(q_c, k_c, v_c, q_norm_w, k_norm_w)
    at_l = _attend_blocked(ql, jnp.concatenate([kc, kl], 1), jnp.concatenate([vc, vl], 1))
    mix_l = jnp.concatenate([gdn_l, fn_l, at_l], -1)
    if not need_ctx:
        return mix_l, None
    mix_c = jnp.concatenate([_gdn_output(o_c, z_c, gdn_norm_w), _fourier_mix(f_c, fn_w),
                             _attend(qc, kc, vc)], -1)
    return mix_l, mix_c


def _hier_moe(h, wg, bg, we, be, w1, w3, w2):
    shp = h.shape
    h = h.reshape(-1, shp[-1])
    n = h.shape[0]
    rows = jnp.arange(n)
    lg = (h @ wg + bg).astype(F32)
    grp = jnp.argmax(lg, -1)
    p_grp = jax.nn.softmax(lg, -1)[rows, grp][:, None]
    le = (h @ we + be).astype(F32).reshape(n, MOE_GROUPS, MOE_EXPERTS_PER_GROUP)[rows, grp]
    top_v, top_i = lax.top_k(le, MOE_TOP_K)
    w_sel = p_grp * jax.nn.softmax(top_v, -1)
    eid = grp[:, None] * MOE_EXPERTS_PER_GROUP + top_i
    gates = jnp.einsum('nk,nke->ne', w_sel, jax.nn.one_hot(eid, MOE_EXPERTS, dtype=F32)).astype(h.dtype)
    out = jnp.zeros_like(h)
    for e in range(MOE_EXPERTS):
        y = (jax.nn.silu(h @ w1[e]) * (h @ w3[e])) @ w2[e]
        out = out + gates[:, e:e + 1] * y
    return out.reshape(shp)


def setup_inputs(seed: int = 0) -> dict:
    key = jax.random.key(seed)
    ks = jax.random.split(key, 32)
    L, D = DEPTH, D_MODEL

    def nrm(k, shape, scale):
        return jax.random.normal(k, shape, F32) * scale

    dt = jnp.exp(jax.random.uniform(ks[9], (L, N_DIR, GDN_HEADS), F32, math.log(1e-3), math.log(1e-1)))
    return {
        'x': nrm(ks[0], (BATCH, SEQ, D), 1.0),
        'c': nrm(ks[1], (BATCH, D), 1.0),
        'ctx': nrm(ks[2], (BATCH, CTX_LEN, D), 1.0),
        'c_ctx': nrm(ks[3], (D,), 1.0),
        'w_mod': nrm(ks[4], (L, D, 6 * D), D ** -0.5),
        'b_mod': nrm(ks[5], (L, 6 * D), 0.02),
        'w_in': nrm(ks[6], (L, D, N_IN), D ** -0.5),
        'gdn_conv': nrm(ks[7], (L, GDN_CONV_W, 3 * GDN_WIDTH), GDN_CONV_W ** -0.5),
        'gdn_a_log': jnp.log(jax.random.uniform(ks[8], (L, N_DIR, GDN_HEADS), F32, 1.0, 16.0)),
        'gdn_dt_bias': dt + jnp.log(-jnp.expm1(-dt)),
        'gdn_norm': 1.0 + nrm(ks[10], (L, HEAD_DIM), 0.02),
        'fn_w': nrm(ks[11], (L, FN_WIDTH, FN_WIDTH), FN_WIDTH ** -0.5),
        'q_norm': 1.0 + nrm(ks[12], (L, HEAD_DIM), 0.02),
        'k_norm': 1.0 + nrm(ks[13], (L, HEAD_DIM), 0.02),
        'w_out': nrm(ks[14], (L, MIX_WIDTH, D), MIX_WIDTH ** -0.5 * DEEPNORM_BETA),
        'ln1_g': 1.0 + nrm(ks[15], (L, D), 0.02),
        'ln1_b': nrm(ks[16], (L, D), 0.02),
        'ln2_g': 1.0 + nrm(ks[17], (L, D), 0.02),
        'ln2_b': nrm(ks[18], (L, D), 0.02),
        'router_g': nrm(ks[19], (L, D, MOE_GROUPS), D ** -0.5),
        'router_g_b': nrm(ks[20], (L, MOE_GROUPS), 0.01),
        'router_e': nrm(ks[21], (L, D, MOE_EXPERTS), D ** -0.5),
        'router_e_b': nrm(ks[22], (L, MOE_EXPERTS), 0.01),
        'w1': nrm(ks[23], (L, MOE_EXPERTS, D, MOE_D_FF), D ** -0.5),
        'w3': nrm(ks[24], (L, MOE_EXPERTS, D, MOE_D_FF), D ** -0.5),
        'w2': nrm(ks[25], (L, MOE_EXPERTS, MOE_D_FF, D), MOE_D_FF ** -0.5 * DEEPNORM_BETA),
    }


def reference(x, c, ctx, c_ctx, w_mod, b_mod, w_in, gdn_conv, gdn_a_log, gdn_dt_bias, gdn_norm, fn_w,
              q_norm, k_norm, w_out, ln1_g, ln1_b, ln2_g, ln2_b, router_g, router_g_b, router_e,
              router_e_b, w1, w3, w2):
    T = x.shape[1]
    n_ctx = ctx.shape[1]
    rows = T // GRID_W
    rope = _axial_rope(rows)
    sc = jax.nn.silu(c)
    scc = jax.nn.silu(c_ctx)
    xc = ctx
    for l in range(DEPTH):
        last = l == DEPTH - 1
        sh1, s1, g1, sh2, s2, g2 = (m[:, None] for m in jnp.split(sc @ w_mod[l] + b_mod[l], 6, -1))
        csh1, cs1, cg1, csh2, cs2, cg2 = jnp.split(scc @ w_mod[l] + b_mod[l], 6, -1)
        pl = _split_in((x * (1 + s1) + sh1) @ w_in[l])
        pc = _split_in((xc * (1 + cs1) + csh1) @ w_in[l])
        mix_l, mix_c = _token_mixers(pl, pc, rope, gdn_conv[l], gdn_a_log[l], gdn_dt_bias[l], gdn_norm[l],
                                     fn_w[l], q_norm[l], k_norm[l], need_ctx=not last)
        x = _layernorm(DEEPNORM_ALPHA * x + g1 * (mix_l @ w_out[l]), ln1_g[l], ln1_b[l])
        moe_w = (router_g[l], router_g_b[l], router_e[l], router_e_b[l], w1[l], w3[l], w2[l])
        if last:
            y = _hier_moe(x * (1 + s2) + sh2, *moe_w)
            x = _layernorm(DEEPNORM_ALPHA * x + g2 * y, ln2_g[l], ln2_b[l])
        else:
            xc = _layernorm(DEEPNORM_ALPHA * xc + cg1 * (mix_c @ w_out[l]), ln1_g[l], ln1_b[l])
            h_all = jnp.concatenate([xc * (1 + cs2) + csh2, x * (1 + s2) + sh2], axis=1)
            y = _hier_moe(h_all, *moe_w)
            x = _layernorm(DEEPNORM_ALPHA * x + g2 * y[:, n_ctx:], ln2_g[l], ln2_b[l])
            xc = _layernorm(DEEPNORM_ALPHA * xc + cg2 * y[:, :n_ctx], ln2_g[l], ln2_b[l])
    return x
```

```python
import numpy as np
import ml_dtypes
import concourse.bass as bass
import concourse.mybir as mybir
from concourse.bass_utils import run_bass_kernel_spmd

F32 = mybir.dt.float32
BF16 = mybir.dt.bfloat16
I32 = mybir.dt.int32
AF = mybir.ActivationFunctionType
ALU = mybir.AluOpType
AX = mybir.AxisListType

P = 128


class Buf:
    __slots__ = ("name", "w", "r")

    def __init__(self, name):
        self.name = name
        self.w = None
        self.r = {}


class Sched:
    COMPUTE = ("pe", "act", "dve", "pool")
    LIMIT = 24000

    def __init__(self, nc, n_dma=8):
        self.nc = nc
        self.eng = {"pe": nc.tensor, "act": nc.scalar, "dve": nc.vector, "pool": nc.gpsimd, "sp": nc.sync}
        self.prog = {e: [] for e in self.eng}
        self.n_dma = n_dma
        self.dma_rr = 0
        self.bufs = []
        self.epoch = 0
        self._new_epoch()

    def _new_epoch(self):
        self.sem = {}
        self.cnt = {}
        for p in list(self.COMPUTE) + ["d%d" % i for i in range(self.n_dma)]:
            self.sem[p] = self.nc.alloc_semaphore(name="s_%s_%d" % (p, self.epoch))
            self.cnt[p] = 0
        self.waited = {}
        for b in self.bufs:
            b.w = None
            b.r = {}
        self.epoch += 1

    def buf(self, name):
        b = Buf(name)
        self.bufs.append(b)
        return b

    def barrier(self):
        for e in self.eng:
            for p, c in self.cnt.items():
                if c > 0 and self.waited.get((e, p), 0) < c:
                    self.prog[e].append(("wait", self.sem[p], c))
                    self.waited[(e, p)] = c

    def _maybe_epoch(self):
        pass

    def _deps(self, e, reads, writes):
        need = {}
        for b in reads:
            if b.w is not None:
                need[b.w[0]] = max(need.get(b.w[0], 0), b.w[1])
        for b in writes:
            if b.w is not None:
                need[b.w[0]] = max(need.get(b.w[0], 0), b.w[1])
            for p, c in b.r.items():
                need[p] = max(need.get(p, 0), c)
        for p, c in need.items():
            if p == e == "pe":
                continue
            if self.waited.get((e, p), 0) < c:
                self.prog[e].append(("wait", self.sem[p], c))
                self.waited[(e, p)] = c

    def op(self, e, fn, reads=(), writes=()):
        self._maybe_epoch()
        self._deps(e, reads, writes)
        self.cnt[e] += 1
        c = self.cnt[e]
        self.prog[e].append(("ins", fn, self.sem[e], 1))
        for b in reads:
            b.r[e] = c
        for b in writes:
            b.w = (e, c)
            b.r = {}

    def dma(self, out, in_, reads=(), writes=(), q="sp", **kw):
        self._maybe_epoch()
        d = "d%d" % self.dma_rr
        self.dma_rr = (self.dma_rr + 1) % self.n_dma
        if self.cnt[d] > 0 and self.waited.get((q, d), 0) < self.cnt[d]:
            self.prog[q].append(("wait", self.sem[d], self.cnt[d]))
            self.waited[(q, d)] = self.cnt[d]
        self._deps(q, reads, writes)
        self.cnt[d] += 16
        c = self.cnt[d]
        self.prog[q].append(("ins", lambda eng: eng.dma_start(out=out, in_=in_, **kw), self.sem[d], 16))
        for b in reads:
            b.r[d] = c
        for b in writes:
            b.w = (d, c)
            b.r = {}

    def finish(self):
        for e in self.eng:
            for p, c in self.cnt.items():
                if c > 0:
                    self.prog[e].append(("wait", self.sem[p], c))

    def emit(self, block):
        def mk(e):
            def body(eng):
                for it in self.prog[e]:
                    if it[0] == "wait":
                        eng.wait_ge(it[1], it[2])
                    else:
                        it[1](eng).then_inc(it[2], it[3])
            return body
        block.tensor(mk("pe"))
        block.scalar(mk("act"))
        block.vector(mk("dve"))
        block.gpsimd(mk("pool"))
        block.sync(mk("sp"))


D = 4096
KD = D // P
HD = 128
GH = 12
NIN = 9776
ALPHA = 4 ** 0.25
CH = 64
NE = 32
NG = 4
EPG = 8


class Cfg:
    def __init__(self, T=8192, NCTX=256, DFF=512, L=2):
        self.T, self.NCTX, self.DFF, self.L = T, NCTX, DFF, L
        self.S = T + NCTX
        self.NT = self.S // P
        self.NTC = NCTX // P


class K:
    def __init__(self, cfg):
        self.cfg = cfg
        self.nc = bass.Bass("TRN2", target_bir_lowering=False)
        self.S = Sched(self.nc)
        self.uid = 0
        self.ins = {}

    def inp(self, name, shape, dt=F32):
        t = self.nc.dram_tensor(name, list(shape), dt, kind="ExternalInput").ap()
        self.ins[name] = t
        return t

    def scratch(self, name, shape, dt=F32):
        return self.nc.dram_tensor(name, list(shape), dt, kind="Internal").ap(), self.S.buf(name)

    def sb(self, es, name, shape, dt=F32):
        self.uid += 1
        t = es.enter_context(self.nc.sbuf_tensor("%s_%d" % (name, self.uid), list(shape), dt))
        return t, self.S.buf(name)

    def ps(self, es, name, shape, dt=F32):
        self.uid += 1
        t = es.enter_context(self.nc.psum_tensor("%s_%d" % (name, self.uid), list(shape), dt))
        return t, self.S.buf(name)

    def phase_end(self):
        self.S.barrier()

    def bcast_tile(self, es, name, rows_dram, r, n, add=0.0, dt=F32):
        S = self.S
        out, b_out = self.sb(es, name, [P, n], dt)
        for j in range(0, n, 512):
            w = min(512, n - j)
            rows, b_rows = self.rowstg[self.rs_i % 2]
            self.rs_i += 1
            S.dma(rows[:, 0:w], rows_dram[:, j:j + w], writes=[b_rows])
            ps, b_ps = self.next_ps()
            S.op("pe", lambda e, ps=ps, rows=rows, w=w: e.matmul(out=ps[:, 0:w], lhsT=self.sel[:, r * P:(r + 1) * P],
                                                               rhs=rows[:, 0:w], start=True, stop=True),
                 reads=[b_rows, self.b_sel], writes=[b_ps])
            S.op("act", lambda e, ps=ps, j=j, w=w: e.activation(out=out[:, j:j + w], in_=ps[:, 0:w], func=AF.Identity,
                                                              bias=float(add), scale=1.0),
                 reads=[b_ps], writes=[b_out])
        return out, b_out

    def modcols(self, es, name, rows_dram, add=0.0):
        S = self.S
        out, b_out = self.sb(es, name, [P, KD, 2])
        for j in range(0, D, 512):
            rows, b_rows = self.rowstg[self.rs_i % 2]
            self.rs_i += 1
            S.dma(rows[:, :], rows_dram[:, j:j + 512], writes=[b_rows])
            ps, b_ps = self.next_ps()
            for c in range(4):
                S.op("pe", lambda e, ps=ps, rows=rows, c=c: e.transpose(out=ps[:, 2 * c:2 * c + 2], in_=rows[:, c * P:(c + 1) * P],
                                                                     identity=self.identf[0:2, 0:2]),
                     reads=[b_rows, self.b_identf], writes=[b_ps])
            c0 = j // P
            S.op("act", lambda e, ps=ps, c0=c0: e.activation(out=out[:, c0:c0 + 4, :], in_=ps[:, 0:8].rearrange("p (c r) -> p c r", r=2),
                                                            func=AF.Identity, bias=float(add), scale=1.0),
                 reads=[b_ps], writes=[b_out])
        return out, b_out

    def consts(self, es):
        S = self.S
        self.identf, self.b_identf = self.sb(es, "identf", [P, P])
        self.identb, self.b_identb = self.sb(es, "identb", [P, P], BF16)
        self.sel, self.b_sel = self.sb(es, "sel", [2, 2 * P])
        S.dma(self.identf[:], self.ins["identf"], writes=[self.b_identf])
        S.dma(self.sel[:], self.ins["sel"], writes=[self.b_sel])
        S.op("dve", lambda e: e.tensor_copy(out=self.identb[:], in_=self.identf[:]), reads=[self.b_identf],
             writes=[self.b_identb])
        _fc, _n1, _cpp, _n2, _fpp, _ns = _moe_geom(self.cfg)
        self.G12, self.b_G12 = self.sb(es, "G12", [P, self.cfg.NT, 2])
        self.POSI, self.b_POSI = self.sb(es, "POSI", [P, self.cfg.NT, 2], I32)
        self.IDXI, self.b_IDXI = self.sb(es, "IDXI", [P, _ns], I32)
        self.rowstg = [self.sb(es, "rowstg%d" % i, [2, 512]) for i in range(2)]
        self.rs_i = 0
        self.tpall, _ = self.ps(es, "tpall", [P, 8 * 512])
        self.tp = [(self.tpall[:, i * 512:(i + 1) * 512], self.S.buf("tp%d" % i)) for i in range(8)]
        self.tp_i = 0

    def next_ps(self, lo=0, hi=8):
        p = lo + self.tp_i % (hi - lo)
        self.tp_i += 1
        return self.tp[p]

    def cast_w(self, src, dst, rows, cols):
        import contextlib
        S = self.S
        with contextlib.ExitStack() as es:
            CW = 2048
            st = [self.sb(es, "cw_f%d" % i, [P, CW]) for i in range(3)]
            sbf = [self.sb(es, "cw_b%d" % i, [P, CW], BF16) for i in range(3)]
            i = 0
            engs = ["dve", "pool", "act"]
            for r in range(0, rows, P):
                for c in range(0, cols, CW):
                    w = min(CW, cols - c)
                    s = i % 3
                    f, bf_ = st[s]
                    o, bo = sbf[s]
                    S.dma(f[:, 0:w], src[r:r + P, c:c + w], writes=[bf_])
                    en = engs[i % 3]
                    if en == "act":
                        S.op("act", lambda e, f=f, o=o, w=w: e.copy(out=o[:, 0:w], in_=f[:, 0:w]), reads=[bf_], writes=[bo])
                    else:
                        S.op(en, lambda e, f=f, o=o, w=w: e.tensor_copy(out=o[:, 0:w], in_=f[:, 0:w]), reads=[bf_],
                             writes=[bo])
                    S.dma(dst[r:r + P, c:c + w], o[:, 0:w], reads=[bo])
                    i += 1
        self.phase_end()

    def p0_mods(self):
        import contextlib
        S, cfg = self.S, self.cfg
        cc, w_mod, bm = self.ins["cc"], self.ins["w_mod"], self.ins["bm"]
        self.MODROW, _ = self.scratch("modrow", [cfg.L, 2, 6 * D])
        with contextlib.ExitStack() as es:
            sc, b_sc = self.sb(es, "sc", [P, KD, 2])
            S.dma(sc[:], cc, writes=[b_sc])
            S.op("act", lambda e: e.activation(out=sc[:], in_=sc[:], func=AF.Silu), reads=[b_sc], writes=[b_sc])
            NB = 512
            wm = [self.sb(es, "wm%d" % i, [P, KD, NB]) for i in range(2)]
            bmt = [self.sb(es, "bmt%d" % i, [2, NB]) for i in range(2)]
            mr = [self.sb(es, "mr%d" % i, [2, NB]) for i in range(2)]
            it = 0
            for l in range(cfg.L):
                for n in range(0, 6 * D, NB):
                    s = it % 2
                    it += 1
                    w, b_w = wm[s]
                    bt, b_bt = bmt[s]
                    m, b_m = mr[s]
                    S.dma(w[:], w_mod[l].rearrange("(c p) n -> p c n", p=P)[:, :, n:n + NB], writes=[b_w])
                    S.dma(bt[:], bm[l, :, n:n + NB], writes=[b_bt])
                    ps, b_ps = self.next_ps()
                    for c in range(KD):
                        S.op("pe", lambda e, ps=ps, w=w, c=c: e.matmul(out=ps[0:2, :], lhsT=sc[:, c, :], rhs=w[:, c, :],
                                                                      start=(c == 0), stop=(c == KD - 1)),
                             reads=[b_sc, b_w], writes=[b_ps])
                    S.op("dve", lambda e, ps=ps, m=m, bt=bt: e.tensor_tensor(out=m[:], in0=ps[0:2, :], in1=bt[:], op=ALU.add),
                         reads=[b_ps, b_bt], writes=[b_m])
                    S.dma(self.MODROW[l, :, n:n + NB], m[:], reads=[b_m])
        self.phase_end()

    def gemm(self, src_rows, Kdim, wb, blocks, mod=None, tiles=None):
        import contextlib
        S, cfg = self.S, self.cfg
        KC = Kdim // P
        tiles = list(range(cfg.NT)) if tiles is None else tiles
        wview = wb.rearrange("(c p) n -> p c n", p=P)
        with contextlib.ExitStack() as es:
            xt = [self.sb(es, "g_xt%d" % i, [P, Kdim]) for i in range(2)]
            aT = [self.sb(es, "g_aT%d" % i, [P, KC, 512], BF16) for i in range(2)]
            wt = [self.sb(es, "g_w%d" % i, [P, KC, 512], BF16) for i in range(2)]
            xi = 0
            wi = 0
            for bi, t0 in enumerate(range(0, len(tiles), 4)):
                tb = tiles[t0:t0 + 4]
                a, b_a = aT[bi % 2]
                for j, t in enumerate(tb):
                    x, b_x = xt[xi % 2]
                    xi += 1
                    S.dma(x[:], src_rows(t), writes=[b_x])
                    for c4 in range(0, KC, 4):
                        ps, b_ps = self.next_ps()
                        for c in range(c4, c4 + 4):
                            S.op("pe", lambda e, ps=ps, x=x, c=c, c4=c4: e.transpose(
                                out=ps[:, (c - c4) * P:(c - c4 + 1) * P], in_=x[:, c * P:(c + 1) * P], identity=self.identf[:]),
                                reads=[b_x, self.b_identf], writes=[b_ps])
                        if mod is not None:
                            (msc, b_msc), (msh, b_msh) = mod
                            r = 1 if t < cfg.NTC else 0
                            for c in range(c4, c4 + 4):
                                S.op("act", lambda e, ps=ps, c=c, c4=c4, j=j, r=r, a=a: e.activation(
                                    out=a[:, c, j * P:(j + 1) * P], in_=ps[:, (c - c4) * P:(c - c4 + 1) * P], func=AF.Identity,
                                    scale=msc[:, c, r:r + 1], bias=msh[:, c, r:r + 1]), reads=[b_ps, b_msc, b_msh], writes=[b_a])
                        else:
                            eng = "act" if (c4 // 4) % 2 == 0 else "dve"
                            dst = a[:, c4:c4 + 4, j * P:(j + 1) * P]
                            src = ps[:].rearrange("p (c t) -> p c t", c=4)
                            if eng == "act":
                                S.op("act", lambda e, dst=dst, src=src: e.copy(out=dst, in_=src), reads=[b_ps], writes=[b_a])
                            else:
                                S.op("dve", lambda e, dst=dst, src=src: e.tensor_copy(out=dst, in_=src), reads=[b_ps], writes=[b_a])
                ntok = len(tb) * P
                for (c0, ncols, mode, sink) in blocks:
                    w, b_w = wt[wi % 2]
                    wi += 1
                    S.dma(w[:, :, 0:ncols], wview[:, :, c0:c0 + ncols], writes=[b_w])
                    if mode == "A":
                        for j, t in enumerate(tb):
                            ps, b_ps = self.next_ps()
                            for c in range(KC):
                                S.op("pe", lambda e, ps=ps, a=a, w=w, c=c, j=j, ncols=ncols: e.matmul(
                                    out=ps[:, 0:ncols], lhsT=a[:, c, j * P:(j + 1) * P], rhs=w[:, c, 0:ncols],
                                    start=(c == 0), stop=(c == KC - 1)), reads=[b_a, b_w], writes=[b_ps])
                            sink(ps, b_ps, t, c0, ncols)
                    else:
                        for cs in range(0, ncols, P):
                            ps, b_ps = self.next_ps()
                            for c in range(KC):
                                S.op("pe", lambda e, ps=ps, a=a, w=w, c=c, cs=cs, ntok=ntok: e.matmul(
                                    out=ps[:, 0:ntok], lhsT=w[:, c, cs:cs + P], rhs=a[:, c, 0:ntok],
                                    start=(c == 0), stop=(c == KC - 1)), reads=[b_a, b_w], writes=[b_ps])
                            sink(ps, b_ps, tb[0], ntok, c0 + cs)
        self.phase_end()

    def p1_proj(self, l):
        import contextlib
        S, cfg = self.S, self.cfg
        with contextlib.ExitStack() as es:
            msc = self.modcols(es, "m_s1", self.MODROW[l, :, D:2 * D], add=1.0)
            msh = self.modcols(es, "m_sh1", self.MODROW[l, :, 0:D])
            stg = [self.sb(es, "p1_stg%d" % i, [P, 512]) for i in range(3)]
            stgb = [self.sb(es, "p1_stgb%d" % i, [P, 512], BF16) for i in range(2)]
            cnt = [0]

            def sinkA(dst, off):
                def f(ps, b_ps, t, c0, ncols):
                    s, b_s = stg[cnt[0] % 3]
                    cnt[0] += 1
                    S.op("act", lambda e: e.copy(out=s[:, 0:ncols], in_=ps[:, 0:ncols]), reads=[b_ps], writes=[b_s])
                    S.dma(dst[t * P:(t + 1) * P, c0 - off:c0 - off + ncols], s[:, 0:ncols], reads=[b_s])
                return f

            def sinkB_f32(ps, b_ps, t0, ntok, col0):
                s, b_s = stg[cnt[0] % 3]
                cnt[0] += 1
                S.op("act", lambda e: e.copy(out=s[:, 0:ntok], in_=ps[:, 0:ntok]), reads=[b_ps], writes=[b_s])
                S.dma(self.QKVT[col0 // P, :, t0 * P:t0 * P + ntok], s[:, 0:ntok], reads=[b_s])

            def sinkB_bf(ps, b_ps, t0, ntok, col0):
                s, b_s = stgb[cnt[0] % 2]
                cnt[0] += 1
                S.op("dve", lambda e: e.tensor_copy(out=s[:, 0:ntok], in_=ps[:, 0:ntok]), reads=[b_ps], writes=[b_s])
                S.dma(self.FT[(col0 - 6192) // P, :, t0 * P:t0 * P + ntok], s[:, 0:ntok], reads=[b_s])

            blocks = []
            for c0 in range(0, 4608, 512):
                blocks.append((c0, 512, "B", sinkB_f32))
            for c0 in range(4608, 6144, 512):
                blocks.append((c0, 512, "A", sinkA(self.PZ, 4608)))
            blocks.append((6144, 48, "A", sinkA(self.PAB, 6144)))
            for c0 in range(6192, 7216, 512):
                blocks.append((c0, 512, "B", sinkB_bf))
            for c0 in range(7216, 8752, 512):
                blocks.append((c0, 512, "A", sinkA(self.PQ, 7216)))
            blocks.append((8752, 512, "A", sinkA(self.PK, 8752)))
            blocks.append((9264, 512, "A", sinkA(self.PV, 9264)))
            self.gemm(lambda t: self.XRES[t * P:(t + 1) * P, :], D, self.WINB[l], blocks, mod=(msc, msh))

    def alloc_scratch(self):
        cfg = self.cfg
        S_ = cfg.S
        self.XRES, _ = self.scratch("xres", [S_, D])
        self.WINB = [self.scratch("winb%d" % l, [D, NIN], BF16)[0] for l in range(cfg.L)]
        self.WOUTB = [self.scratch("woutb%d" % l, [D, D], BF16)[0] for l in range(cfg.L)]
        self.QKVT, _ = self.scratch("qkvt", [36, P, S_])
        self.FT, _ = self.scratch("ft", [8, P, S_], BF16)
        self.PZ, _ = self.scratch("pz", [S_, 1536])
        self.PAB, _ = self.scratch("pab", [S_, 48])
        self.PQ, _ = self.scratch("pq", [S_, 1536])
        self.PK, _ = self.scratch("pk", [S_, 512])
        self.PV, _ = self.scratch("pv", [S_, 512])
        self.VB, _ = self.scratch("vb", [S_, 512], BF16)
        self.QT, _ = self.scratch("qt", [12, P, S_], BF16)
        self.KT, _ = self.scratch("kt", [4, P, S_], BF16)
        self.YT, _ = self.scratch("yt", [8, P, S_], BF16)
        self.QNT, _ = self.scratch("qnt", [12, P, S_])
        self.KNT, _ = self.scratch("knt", [12, P, S_])
        self.KTOK, _ = self.scratch("ktok", [S_, 1536])
        self.VTOK, _ = self.scratch("vtok", [S_, 1536])
        self.GB, _ = self.scratch("gb", [S_, 48])
        self.OG = [self.scratch("og%d" % d, [S_, 1536])[0] for d in range(2)]
        self.MIX, _ = self.scratch("mix", [S_, D])
        self.Y1, _ = self.scratch("y1", [S_, D])
        self.HM, _ = self.scratch("hm", [S_, D])

    def load_x(self):
        import contextlib
        S, cfg = self.S, self.cfg
        with contextlib.ExitStack() as es:
            st = [self.sb(es, "lx%d" % i, [P, D]) for i in range(3)]
            for t in range(cfg.NT):
                s, b_s = st[t % 3]
                src = self.ins["ctx"][t * P:(t + 1) * P, :] if t < cfg.NTC else self.ins["x"][(t - cfg.NTC) * P:(t - cfg.NTC + 1) * P, :]
                S.dma(s[:], src, writes=[b_s])
                S.dma(self.XRES[t * P:(t + 1) * P, :], s[:], reads=[b_s])
        self.phase_end()

    def dump(self, name, src, shape, dt=F32):
        import contextlib
        S = self.S
        out = self.nc.dram_tensor(name, list(shape), dt, kind="ExternalOutput").ap()
        rows, cols = shape
        with contextlib.ExitStack() as es:
            st = [self.sb(es, "dmp%d" % i, [P, cols], dt) for i in range(2)]
            for i, r in enumerate(range(0, rows, P)):
                n = min(P, rows - r)
                s, b_s = st[i % 2]
                S.dma(s[0:n, :], src[r:r + n, :], writes=[b_s])
                S.dma(out[r:r + n, :], s[0:n, :], reads=[b_s])
        self.phase_end()

    def finish(self):
        self.S.finish()
        with self.nc.Block() as block:
            self.S.emit(block)

    def p4_attn(self, l, ctx_out=True):
        import contextlib
        S, cfg = self.S, self.cfg
        NT, NTC = cfg.NT, cfg.NTC
        with contextlib.ExitStack() as es:
            qw, b_qw = self.bcast_tile(es, "qw", self.ins["qknw"][l], 0, HD)
            kw, b_kw = self.bcast_tile(es, "kw", self.ins["qknw"][l], 1, HD)
            S.op("dve", lambda e: e.tensor_scalar(out=qw[:], in0=qw[:], scalar1=float(HD ** -0.5), scalar2=None, op0=ALU.mult),
                 reads=[b_qw], writes=[b_qw])
            bufs = {}
            for nm, H in (("q", 12), ("k", 4)):
                bufs[nm] = dict(
                    x=[self.sb(es, nm + "x%d" % i, [P, H, HD]) for i in range(2)],
                    sq=self.sb(es, nm + "sq", [P, H, HD]), ss=self.sb(es, nm + "ss", [P, H]),
                    xn=self.sb(es, nm + "xn", [P, H, HD]), xr=self.sb(es, nm + "xr", [P, H, HD]),
                    t1=self.sb(es, nm + "t1", [P, H, 64]), t2=self.sb(es, nm + "t2", [P, H, 64]),
                    xT=[self.sb(es, nm + "xT%d" % i, [P, H, P], BF16) for i in range(2)])
            vx = [self.sb(es, "vx%d" % i, [P, 512]) for i in range(2)]
            vb = [self.sb(es, "vb%d" % i, [P, 512], BF16) for i in range(2)]
            rp = [self.sb(es, "rp%d" % i, [P, HD]) for i in range(2)]
            for t in range(NT):
                lat = t >= NTC
                r, b_r = rp[t % 2]
                if lat:
                    S.dma(r[:], self.ins["rope"][(t - NTC) * P:(t - NTC + 1) * P, :], writes=[b_r])
                v, b_v = vx[t % 2]
                vbb, b_vb = vb[t % 2]
                S.dma(v[:], self.PV[t * P:(t + 1) * P, :], writes=[b_v])
                S.op("pool", lambda e, v=v, vbb=vbb: e.tensor_copy(out=vbb[:], in_=v[:]), reads=[b_v], writes=[b_vb])
                S.dma(self.VB[t * P:(t + 1) * P, :], vbb[:], reads=[b_vb])
                for nm, H, src, w, b_w, dstT in (("q", 12, self.PQ, qw, b_qw, self.QT), ("k", 4, self.PK, kw, b_kw, self.KT)):
                    B = bufs[nm]
                    x, b_x = B["x"][t % 2]
                    sq, b_sq = B["sq"]
                    ss, b_ss = B["ss"]
                    xn, b_xn = B["xn"]
                    xr, b_xr = B["xr"]
                    t1, b_t1 = B["t1"]
                    t2, b_t2 = B["t2"]
                    xT, b_xT = B["xT"][t % 2]
                    S.dma(x[:], src[t * P:(t + 1) * P, :].rearrange("p (h d) -> p h d", d=HD), writes=[b_x])
                    S.op("pool", lambda e, x=x, sq=sq: e.tensor_tensor(out=sq[:], in0=x[:], in1=x[:], op=ALU.mult), reads=[b_x], writes=[b_sq])
                    S.op("dve", lambda e, sq=sq, ss=ss: e.tensor_reduce(out=ss[:], in_=sq[:], axis=AX.X, op=ALU.add), reads=[b_sq], writes=[b_ss])
                    S.op("act", lambda e, ss=ss: e.activation(out=ss[:], in_=ss[:], func=AF.Sqrt, scale=1.0 / HD, bias=1e-6), reads=[b_ss], writes=[b_ss])
                    S.op("dve", lambda e, ss=ss: e.reciprocal(out=ss[:], in_=ss[:]), reads=[b_ss], writes=[b_ss])
                    S.op("dve", lambda e, x=x, ss=ss, xn=xn, H=H: e.tensor_tensor(out=xn[:], in0=x[:], in1=ss[:].unsqueeze(2).broadcast_to([P, H, HD]), op=ALU.mult),
                         reads=[b_x, b_ss], writes=[b_xn])
                    fin = xr if lat else xn
                    b_fin = b_xr if lat else b_xn
                    S.op("pool", lambda e, xn=xn, w=w, H=H: e.tensor_tensor(out=xn[:], in0=xn[:], in1=w[:].unsqueeze(1).broadcast_to([P, H, HD]), op=ALU.mult),
                         reads=[b_xn, b_w], writes=[b_xn])
                    if lat:
                        cosb = r[:, 0:64].unsqueeze(1).broadcast_to([P, H, 64])
                        sinb = r[:, 64:128].unsqueeze(1).broadcast_to([P, H, 64])
                        x1, x2 = xn[:, :, 0:64], xn[:, :, 64:128]
                        S.op("dve", lambda e, t1=t1, x1=x1, cosb=cosb: e.tensor_tensor(out=t1[:], in0=x1, in1=cosb, op=ALU.mult), reads=[b_xn, b_r], writes=[b_t1])
                        S.op("pool", lambda e, t2=t2, x2=x2, sinb=sinb: e.tensor_tensor(out=t2[:], in0=x2, in1=sinb, op=ALU.mult), reads=[b_xn, b_r], writes=[b_t2])
                        S.op("dve", lambda e, xr=xr, t1=t1, t2=t2: e.tensor_tensor(out=xr[:, :, 0:64], in0=t1[:], in1=t2[:], op=ALU.subtract), reads=[b_t1, b_t2], writes=[b_xr])
                        S.op("pool", lambda e, t1=t1, x2=x2, cosb=cosb: e.tensor_tensor(out=t1[:], in0=x2, in1=cosb, op=ALU.mult), reads=[b_xn, b_r, b_xr], writes=[b_t1])
                        S.op("dve", lambda e, t2=t2, x1=x1, sinb=sinb: e.tensor_tensor(out=t2[:], in0=x1, in1=sinb, op=ALU.mult), reads=[b_xn, b_r, b_xr], writes=[b_t2])
                        S.op("pool", lambda e, xr=xr, t1=t1, t2=t2: e.tensor_tensor(out=xr[:, :, 64:128], in0=t1[:], in1=t2[:], op=ALU.add), reads=[b_t1, b_t2], writes=[b_xr])
                    for h4 in range(0, H, 4):
                        ps, b_ps = self.next_ps()
                        for h in range(h4, h4 + 4):
                            S.op("pe", lambda e, ps=ps, fin=fin, h=h, h4=h4: e.transpose(out=ps[:, (h - h4) * P:(h - h4 + 1) * P], in_=fin[:, h, :], identity=self.identf[:]),
                                 reads=[b_fin, self.b_identf], writes=[b_ps])
                        S.op("act", lambda e, ps=ps, xT=xT, h4=h4: e.copy(out=xT[:, h4:h4 + 4, :], in_=ps[:].rearrange("p (h t) -> p h t", h=4)),
                             reads=[b_ps], writes=[b_xT])
                    S.dma(dstT[:, :, t * P:(t + 1) * P].rearrange("h p t -> p h t"), xT[:], reads=[b_xT])
        self.phase_end()
        with contextlib.ExitStack() as es:
            ones, b_ones = self.sb(es, "a_ones", [P, P], BF16)
            S.op("dve", lambda e: e.memset(ones[:], 1.0), writes=[b_ones])
            ktg = [self.sb(es, "ktg%d" % i, [P, cfg.S], BF16) for i in range(2)]
            vg = [self.sb(es, "vg%d" % i, [P, NT, HD], BF16) for i in range(2)]
            qt = [self.sb(es, "qt%d" % i, [P, 512], BF16) for i in range(2)]
            pT = [self.sb(es, "pT%d" % i, [P, 512], BF16) for i in range(3)]
            rinv = self.sb(es, "rinv", [P, 512])
            osb = [self.sb(es, "osb%d" % i, [P, 512]) for i in range(2)]
            ostg = [self.sb(es, "ostg%d" % i, [P, 4, HD]) for i in range(2)]
            qblocks = [(q0, min(512, cfg.S - q0), 0, NT) for q0 in range(cfg.NCTX, cfg.S, 512)]
            if ctx_out:
                qblocks += [(q0, min(512, cfg.NCTX - q0), 0, NTC) for q0 in range(0, cfg.NCTX, 512)]
            ui = 0
            for g in range(4):
                kt, b_kt = ktg[g % 2]
                vv, b_vv = vg[g % 2]
                S.dma(kt[:], self.KT[g], writes=[b_kt])
                for n0 in range(0, NT, 16):
                    n1 = min(NT, n0 + 16)
                    S.dma(vv[:, n0:n1, :], self.VB[n0 * P:n1 * P, g * HD:(g + 1) * HD].rearrange("(n p) d -> p n d", p=P), writes=[b_vv])
                for (q0, nq, s0, s1) in qblocks:
                    for hh in range(3):
                        h = g * 3 + hh
                        q, b_q = qt[ui % 2]
                        oT, b_oT = self.tp[4 + 2 * (ui % 2)]
                        rs, b_rs = self.tp[5 + 2 * (ui % 2)]
                        o, b_o = osb[ui % 2]
                        og, b_og = ostg[ui % 2]
                        ui += 1
                        S.dma(q[:, 0:nq], self.QT[h, :, q0:q0 + nq], writes=[b_q])
                        for s in range(s0, s1):
                            st, b_st = self.next_ps(0, 4)
                            pt, b_pt = pT[s % 3]
                            S.op("pe", lambda e, st=st, kt=kt, q=q, s=s, nq=nq: e.matmul(out=st[:, 0:nq], lhsT=kt[:, s * P:(s + 1) * P], rhs=q[:, 0:nq], start=True, stop=True),
                                 reads=[b_kt, b_q], writes=[b_st])
                            S.op("act", lambda e, st=st, pt=pt, nq=nq: e.activation(out=pt[:, 0:nq], in_=st[:, 0:nq], func=AF.Exp), reads=[b_st], writes=[b_pt])
                            S.op("pe", lambda e, oT=oT, vv=vv, pt=pt, s=s, nq=nq, s0=s0, s1=s1: e.matmul(out=oT[:, 0:nq], lhsT=vv[:, s, :], rhs=pt[:, 0:nq], start=(s == s0), stop=(s == s1 - 1)),
                                 reads=[b_vv, b_pt], writes=[b_oT])
                            S.op("pe", lambda e, rs=rs, pt=pt, nq=nq, s=s, s0=s0, s1=s1: e.matmul(out=rs[:, 0:nq], lhsT=ones[:], rhs=pt[:, 0:nq], start=(s == s0), stop=(s == s1 - 1)),
                                 reads=[b_ones, b_pt], writes=[b_rs])
                        ri, b_ri = rinv
                        S.op("dve", lambda e, ri=ri, rs=rs, nq=nq: e.reciprocal(out=ri[:, 0:nq], in_=rs[:, 0:nq]), reads=[b_rs], writes=[b_ri])
                        S.op("dve", lambda e, o=o, oT=oT, ri=ri, nq=nq: e.tensor_tensor(out=o[:, 0:nq], in0=oT[:, 0:nq], in1=ri[:, 0:nq], op=ALU.mult),
                             reads=[b_oT, b_ri], writes=[b_o])
                        ps, b_ps = self.next_ps(0, 4)
                        for j in range(nq // P):
                            S.op("pe", lambda e, ps=ps, o=o, j=j: e.transpose(out=ps[:, j * P:(j + 1) * P], in_=o[:, j * P:(j + 1) * P], identity=self.identf[:]),
                                 reads=[b_o, self.b_identf], writes=[b_ps])
                        nj = nq // P
                        S.op("act", lambda e, ps=ps, og=og, nj=nj: e.copy(out=og[:, 0:nj, :], in_=ps[:, 0:nj * P].rearrange("p (j d) -> p j d", d=HD)),
                             reads=[b_ps], writes=[b_og])
                        S.dma(self.MIX[q0:q0 + nq, 2560 + h * HD:2560 + (h + 1) * HD].rearrange("(j p) d -> p j d", p=P), og[:, 0:nj, :], reads=[b_og])
        self.phase_end()

    def p3_fnet(self, l, ctx_out=True):
        import contextlib
        S, cfg = self.S, self.cfg
        NT, NTC = cfg.NT, cfg.NTC
        segs = [(NTC, NT, self.ins["dft_c"], self.ins["dft_ns"], cfg.T)]
        if ctx_out:
            segs.append((0, NTC, self.ins["dftc_c"], self.ins["dftc_ns"], cfg.NCTX))
        with contextlib.ExitStack() as es:
            dch, b_dch = self.sb(es, "dch", [P, 256], BF16)
            S.dma(dch[:], self.ins["dft_ch"], writes=[b_dch])
            xcs, b_xcs = self.sb(es, "xcs", [P, NT, 2, 256], BF16)
            ftt = [self.sb(es, "ftt%d" % i, [P, 2, 512], BF16) for i in range(2)]
            tab = [self.sb(es, "tab%d" % i, [P, 2, 512], BF16) for i in range(3)]
            ysb = [self.sb(es, "ysb%d" % i, [P, 512], BF16) for i in range(2)]
            it = 0
            for gp in range(4):
                for t0 in range(0, NT, 4):
                    nt = min(4, NT - t0)
                    f, b_f = ftt[(t0 // 4) % 2]
                    S.dma(f[:, :, 0:nt * P], self.FT[2 * gp:2 * gp + 2, :, t0 * P:(t0 + nt) * P].rearrange("g p t -> p g t"), writes=[b_f])
                    for j in range(nt):
                        ps, b_ps = self.next_ps(0, 4)
                        for gi in range(2):
                            S.op("pe", lambda e, ps=ps, f=f, gi=gi, j=j: e.matmul(out=ps[:, gi * 256:(gi + 1) * 256], lhsT=f[:, gi, j * P:(j + 1) * P], rhs=dch[:],
                                                                                 start=True, stop=True), reads=[b_f, b_dch], writes=[b_ps])
                        S.op("act", lambda e, ps=ps, t=t0 + j: e.copy(out=xcs[:, t, :, :], in_=ps[:].rearrange("p (g c) -> p g c", g=2)),
                             reads=[b_ps], writes=[b_xcs])
                for (ta, tb_, dc, dns, n) in segs:
                    for k0 in range(0, n, 512):
                        nk = min(512, n - k0)
                        acc = [self.tp[4 + 2 * (it % 2)], self.tp[5 + 2 * (it % 2)]]
                        for ti, t in enumerate(range(ta, tb_)):
                            tb2, b_tab = tab[ti % 3]
                            S.dma(tb2[:, 0, 0:nk], dc[ti * P:(ti + 1) * P, k0:k0 + nk], writes=[b_tab])
                            S.dma(tb2[:, 1, 0:nk], dns[ti * P:(ti + 1) * P, k0:k0 + nk], writes=[b_tab])
                            for gi in range(2):
                                a_, b_a = acc[gi]
                                S.op("pe", lambda e, a_=a_, t=t, gi=gi, tb2=tb2, nk=nk, ti=ti: e.matmul(out=a_[:, 0:nk], lhsT=xcs[:, t, gi, 0:128], rhs=tb2[:, 0, 0:nk],
                                                                                                    start=(ti == 0), stop=False), reads=[b_xcs, b_tab], writes=[b_a])
                                S.op("pe", lambda e, a_=a_, t=t, gi=gi, tb2=tb2, nk=nk, last=(t == tb_ - 1): e.matmul(out=a_[:, 0:nk], lhsT=xcs[:, t, gi, 128:256], rhs=tb2[:, 1, 0:nk],
                                                                                                                  start=False, stop=last), reads=[b_xcs, b_tab], writes=[b_a])
                        for gi in range(2):
                            a_, b_a = acc[gi]
                            y, b_y = ysb[gi]
                            S.op("act" if gi == 0 else "dve", (lambda e, y=y, a_=a_, nk=nk: e.copy(out=y[:, 0:nk], in_=a_[:, 0:nk])) if gi == 0 else
                                 (lambda e, y=y, a_=a_, nk=nk: e.tensor_copy(out=y[:, 0:nk], in_=a_[:, 0:nk])), reads=[b_a], writes=[b_y])
                            S.dma(self.YT[2 * gp + gi, :, ta * P + k0:ta * P + k0 + nk], y[:, 0:nk], reads=[b_y])
                        it += 1
        self.phase_end()
        with contextlib.ExitStack() as es:
            fwf, b_fwf = self.sb(es, "fwf", [P, 8, 1024])
            fwb, b_fwb = self.sb(es, "fwb", [P, 8, 1024], BF16)
            S.dma(fwf[:], self.ins["fn_w"][l].rearrange("(g p) n -> p g n", p=P), writes=[b_fwf])
            S.op("dve", lambda e: e.tensor_copy(out=fwb[:], in_=fwf[:]), reads=[b_fwf], writes=[b_fwb])
            y8 = [self.sb(es, "y8_%d" % i, [P, 8, P], BF16) for i in range(2)]
            fo = [self.sb(es, "fo%d" % i, [P, 1024]) for i in range(2)]
            tiles = range(NT) if ctx_out else range(NTC, NT)
            for t in tiles:
                y, b_y = y8[t % 2]
                o, b_o = fo[t % 2]
                S.dma(y[:], self.YT[:, :, t * P:(t + 1) * P].rearrange("g p t -> p g t"), writes=[b_y])
                for half in range(2):
                    ps, b_ps = self.next_ps()
                    for g in range(8):
                        S.op("pe", lambda e, ps=ps, y=y, g=g, half=half: e.matmul(out=ps[:], lhsT=y[:, g, :], rhs=fwb[:, g, half * 512:(half + 1) * 512],
                                                                                 start=(g == 0), stop=(g == 7)), reads=[b_y, b_fwb], writes=[b_ps])
                    S.op("act", lambda e, ps=ps, o=o, half=half: e.copy(out=o[:, half * 512:(half + 1) * 512], in_=ps[:]), reads=[b_ps], writes=[b_o])
                S.dma(self.MIX[t * P:(t + 1) * P, 1536:2560], o[:], reads=[b_o])
        self.phase_end()

    def ln_pass(self, l, which, ysrc, make_h):
        import contextlib
        S, cfg = self.S, self.cfg
        gcol = 2 if which == 1 else 5
        lnrows = self.ins["lnp"][l, which - 1]
        for r, tiles in ((0, range(cfg.NTC, cfg.NT)), (1, range(0, cfg.NTC))):
            with contextlib.ExitStack() as es:
                gt, b_gt = self.bcast_tile(es, "ln_gate", self.MODROW[l, :, gcol * D:(gcol + 1) * D], r, D)
                lg, b_lg = self.bcast_tile(es, "ln_g", lnrows, 0, D)
                lb, b_lb = self.bcast_tile(es, "ln_b", lnrows, 1, D)
                if make_h:
                    s2, b_s2 = self.bcast_tile(es, "ln_s2", self.MODROW[l, :, 4 * D:5 * D], r, D, add=1.0)
                    sh2, b_sh2 = self.bcast_tile(es, "ln_sh2", self.MODROW[l, :, 3 * D:4 * D], r, D)
                xs = [self.sb(es, "ln_x%d" % i, [P, D]) for i in range(2)]
                ys = [self.sb(es, "ln_y%d" % i, [P, D]) for i in range(2)]
                st, b_st = self.sb(es, "ln_st", [P, 8, 6])
                mv, b_mv = self.sb(es, "ln_mv", [P, 2])
                rstd, b_rstd = self.sb(es, "ln_rstd", [P, 1])
                nmr, b_nmr = self.sb(es, "ln_nmr", [P, 1])
                for i, t in enumerate(tiles):
                    x, b_x = xs[i % 2]
                    y, b_y = ys[i % 2]
                    S.dma(x[:], self.XRES[t * P:(t + 1) * P, :], writes=[b_x])
                    S.dma(y[:], ysrc[t * P:(t + 1) * P, :], writes=[b_y])
                    S.op("pool", lambda e, y=y: e.tensor_tensor(out=y[:], in0=y[:], in1=gt[:], op=ALU.mult), reads=[b_y, b_gt], writes=[b_y])
                    S.op("dve", lambda e, x=x, y=y: e.scalar_tensor_tensor(out=x[:], in0=x[:], scalar=float(ALPHA), in1=y[:], op0=ALU.mult, op1=ALU.add),
                         reads=[b_x, b_y], writes=[b_x])
                    for c in range(8):
                        S.op("dve", lambda e, x=x, c=c: e.bn_stats(out=st[:, c, :], in_=x[:, c * 512:(c + 1) * 512]), reads=[b_x], writes=[b_st])
                    S.op("dve", lambda e: e.bn_aggr(out=mv[:], in_=st[:].rearrange("p c s -> p (c s)")), reads=[b_st], writes=[b_mv])
                    S.op("act", lambda e: e.activation(out=rstd[:], in_=mv[:, 1:2], func=AF.Sqrt, bias=1e-5, scale=1.0), reads=[b_mv], writes=[b_rstd])
                    S.op("dve", lambda e: e.reciprocal(out=rstd[:], in_=rstd[:]), reads=[b_rstd], writes=[b_rstd])
                    S.op("dve", lambda e: e.scalar_tensor_tensor(out=nmr[:], in0=mv[:, 0:1], scalar=-1.0, in1=rstd[:], op0=ALU.mult, op1=ALU.mult),
                         reads=[b_mv, b_rstd], writes=[b_nmr])
                    S.op("act", lambda e, x=x: e.activation(out=x[:], in_=x[:], func=AF.Identity, scale=rstd[:, 0:1], bias=nmr[:, 0:1]),
                         reads=[b_x, b_rstd, b_nmr], writes=[b_x])
                    S.op("pool", lambda e, x=x: e.tensor_tensor(out=x[:], in0=x[:], in1=lg[:], op=ALU.mult), reads=[b_x, b_lg], writes=[b_x])
                    S.op("dve", lambda e, x=x: e.tensor_tensor(out=x[:], in0=x[:], in1=lb[:], op=ALU.add), reads=[b_x, b_lb], writes=[b_x])
                    S.dma(self.XRES[t * P:(t + 1) * P, :], x[:], reads=[b_x])
                    if make_h:
                        S.op("pool", lambda e, x=x, y=y: e.tensor_tensor(out=y[:], in0=x[:], in1=s2[:], op=ALU.mult), reads=[b_x, b_s2], writes=[b_y])
                        S.op("dve", lambda e, y=y: e.tensor_tensor(out=y[:], in0=y[:], in1=sh2[:], op=ALU.add), reads=[b_y, b_sh2], writes=[b_y])
                        S.dma(self.HM[t * P:(t + 1) * P, :], y[:], reads=[b_y])
            self.phase_end()

    def p5_out(self, l):
        import contextlib
        S = self.S
        with contextlib.ExitStack() as es:
            stg = [self.sb(es, "p5_stg%d" % i, [P, 512]) for i in range(3)]
            cnt = [0]

            def sink(ps, b_ps, t, c0, ncols):
                s, b_s = stg[cnt[0] % 3]
                cnt[0] += 1
                S.op("act", lambda e: e.copy(out=s[:, 0:ncols], in_=ps[:, 0:ncols]), reads=[b_ps], writes=[b_s])
                S.dma(self.Y1[t * P:(t + 1) * P, c0:c0 + ncols], s[:, 0:ncols], reads=[b_s])
            blocks = [(c0, 512, "A", sink) for c0 in range(0, D, 512)]
            self.gemm(lambda t: self.MIX[t * P:(t + 1) * P, :], D, self.WOUTB[l], blocks)
        self.ln_pass(l, 1, self.Y1, make_h=True)

    def p2_gdn_prep(self, l):
        import contextlib
        S, cfg = self.S, self.cfg
        NT, NTC, T, NCTX, S_ = cfg.NT, cfg.NTC, cfg.T, cfg.NCTX, cfg.S
        with contextlib.ExitStack() as es:
            cw, b_cw = self.sb(es, "cw", [P, 36, 5])
            S.dma(cw[:], self.ins["convw"][l], writes=[b_cw])
            onesf, b_onesf = self.sb(es, "onesf", [P, P])
            S.op("dve", lambda e: e.memset(onesf[:], 1.0), writes=[b_onesf])
            xin = [self.sb(es, "xin%d" % i, [P, S_ + 8]) for i in range(2)]
            for x, b_x in xin:
                S.op("pool", lambda e, x=x: e.memset(x[:], 0.0), writes=[b_x])
            acc = [self.sb(es, "cacc%d" % i, [P, S_]) for i in range(2)]
            sq = [self.sb(es, "csq%d" % i, [P, 512]) for i in range(2)]
            rr = [self.sb(es, "crr%d" % i, [P, 512]) for i in range(2)]
            tk = [self.sb(es, "ctk%d" % i, [P, 4, P]) for i in range(2)]
            OC, OL = 2, NCTX + 6
            for c in range(36):
                x, b_x = xin[c % 2]
                a, b_a = acc[c % 2]
                S.dma(x[:, OC:OC + NCTX], self.QKVT[c, :, 0:NCTX], writes=[b_x])
                S.dma(x[:, OL:OL + T], self.QKVT[c, :, NCTX:S_], writes=[b_x])
                for (o0, n, a0) in ((OC, NCTX, 0), (OL, T, NCTX)):
                    S.op("dve", lambda e, x=x, a=a, c=c, o0=o0, n=n, a0=a0: e.tensor_scalar(out=a[:, a0:a0 + n], in0=x[:, o0 - 2:o0 - 2 + n], scalar1=cw[:, c, 0:1], scalar2=None, op0=ALU.mult),
                         reads=[b_x, b_cw], writes=[b_a])
                    for j in range(1, 5):
                        S.op("dve", lambda e, x=x, a=a, c=c, o0=o0, n=n, a0=a0, j=j: e.scalar_tensor_tensor(out=a[:, a0:a0 + n], in0=x[:, o0 - 2 + j:o0 - 2 + j + n], scalar=cw[:, c, j:j + 1],
                                                                                                         in1=a[:, a0:a0 + n], op0=ALU.mult, op1=ALU.add), reads=[b_x, b_cw, b_a], writes=[b_a])
                S.op("act", lambda e, a=a: e.activation(out=a[:], in_=a[:], func=AF.Silu), reads=[b_a], writes=[b_a])
                if c < 24:
                    isq = c < 12
                    for bi, t0 in enumerate(range(0, S_, 512)):
                        n = min(512, S_ - t0)
                        q2, b_q2 = sq[bi % 2]
                        r, b_r = rr[bi % 2]
                        S.op("pool", lambda e, a=a, q2=q2, t0=t0, n=n: e.tensor_tensor(out=q2[:, 0:n], in0=a[:, t0:t0 + n], in1=a[:, t0:t0 + n], op=ALU.mult), reads=[b_a], writes=[b_q2])
                        ps, b_ps = self.next_ps()
                        S.op("pe", lambda e, ps=ps, q2=q2, n=n: e.matmul(out=ps[:, 0:n], lhsT=onesf[:], rhs=q2[:, 0:n], start=True, stop=True), reads=[b_onesf, b_q2], writes=[b_ps])
                        sc_, bi_ = (float(HD), float(HD) * 1e-6) if isq else (1.0, 1e-6)
                        S.op("act", lambda e, ps=ps, r=r, n=n, sc_=sc_, bi_=bi_: e.activation(out=r[:, 0:n], in_=ps[:, 0:n], func=AF.Sqrt, scale=sc_, bias=bi_), reads=[b_ps], writes=[b_r])
                        S.op("dve", lambda e, r=r, n=n: e.reciprocal(out=r[:, 0:n], in_=r[:, 0:n]), reads=[b_r], writes=[b_r])
                        S.op("dve", lambda e, a=a, r=r, t0=t0, n=n: e.tensor_tensor(out=a[:, t0:t0 + n], in0=a[:, t0:t0 + n], in1=r[:, 0:n], op=ALU.mult), reads=[b_a, b_r], writes=[b_a])
                    S.dma((self.QNT if isq else self.KNT)[c % 12], a[:], reads=[b_a])
                if c >= 12:
                    dst = self.KTOK if c < 24 else self.VTOK
                    h = c % 12
                    for t4 in range(0, NT, 4):
                        nt = min(4, NT - t4)
                        ps, b_ps = self.next_ps()
                        tt, b_tt = tk[(t4 // 4) % 2]
                        for j in range(nt):
                            S.op("pe", lambda e, ps=ps, a=a, j=j, t4=t4: e.transpose(out=ps[:, j * P:(j + 1) * P], in_=a[:, (t4 + j) * P:(t4 + j + 1) * P], identity=self.identf[:]),
                                 reads=[b_a, self.b_identf], writes=[b_ps])
                        S.op("act", lambda e, ps=ps, tt=tt, nt=nt: e.copy(out=tt[:, 0:nt, :], in_=ps[:, 0:nt * P].rearrange("p (j d) -> p j d", d=P)), reads=[b_ps], writes=[b_tt])
                        S.dma(dst[t4 * P:(t4 + nt) * P, h * HD:(h + 1) * HD].rearrange("(j p) d -> p j d", p=P), tt[:, 0:nt, :], reads=[b_tt])
            nea, b_nea = self.bcast_tile(es, "nea", self.ins["gdnp"][l], 0, 24)
            dtb, b_dtb = self.bcast_tile(es, "dtb", self.ins["gdnp"][l], 1, 24)
            S.op("act", lambda e: e.activation(out=nea[:], in_=nea[:], func=AF.Exp), reads=[b_nea], writes=[b_nea])
            S.op("dve", lambda e: e.tensor_scalar(out=nea[:], in0=nea[:], scalar1=-1.0, scalar2=None, op0=ALU.mult), reads=[b_nea], writes=[b_nea])
            ab, b_ab = self.sb(es, "ab", [P, NT, 48])
            for n0 in range(0, NT, 16):
                n1 = min(NT, n0 + 16)
                S.dma(ab[:, n0:n1, :], self.PAB[n0 * P:n1 * P, :].rearrange("(n p) c -> p n c", p=P), writes=[b_ab])
            av, bv = ab[:, :, 0:24], ab[:, :, 24:48]
            S.op("dve", lambda e: e.tensor_tensor(out=av, in0=av, in1=dtb[:].unsqueeze(1).broadcast_to([P, NT, 24]), op=ALU.add), reads=[b_ab, b_dtb], writes=[b_ab])
            S.op("act", lambda e: e.activation(out=av, in_=av, func=AF.Exp), reads=[b_ab], writes=[b_ab])
            S.op("act", lambda e: e.activation(out=av, in_=av, func=AF.Ln, bias=1.0, scale=1.0), reads=[b_ab], writes=[b_ab])
            S.op("dve", lambda e: e.tensor_tensor(out=av, in0=av, in1=nea[:].unsqueeze(1).broadcast_to([P, NT, 24]), op=ALU.mult), reads=[b_ab, b_nea], writes=[b_ab])
            S.op("act", lambda e: e.activation(out=bv, in_=bv, func=AF.Sigmoid), reads=[b_ab], writes=[b_ab])
            for n0 in range(0, NT, 16):
                n1 = min(NT, n0 + 16)
                S.dma(self.GB[n0 * P:n1 * P, :].rearrange("(n p) c -> p n c", p=P), ab[:, n0:n1, :], reads=[b_ab])
        self.phase_end()

    def p2_gdn_main(self, l):
        import contextlib
        S, cfg = self.S, self.cfg
        NCH = cfg.S // CH
        NCC = cfg.NCTX // CH
        order = [list(range(NCH)), list(range(NCC - 1, -1, -1)) + list(range(NCH - 1, NCC - 1, -1))]
        H = GH
        with contextlib.ExitStack() as es:
            msk = {}
            for d in range(2):
                for ki, nm in enumerate(("MU", "MLs", "Scs", "Ssc", "Isc")):
                    m, b_m = self.sb(es, "m_%s%d" % (nm, d), [CH, H, CH])
                    S.dma(m[:], self.ins["gmask"][d, ki], writes=[b_m])
                    msk[(nm, d)] = (m, b_m)
            i12, b_i12 = self.sb(es, "m_I", [CH, H, CH])
            S.dma(i12[:], self.ins["gmask_i"], writes=[b_i12])
            ones, b_ones = self.sb(es, "g_ones", [CH, P])
            S.op("dve", lambda e: e.memset(ones[:], 1.0), writes=[b_ones])
            Sst = [self.sb(es, "Sst%d" % d, [P, H, HD]) for d in range(2)]
            for st, b_st in Sst:
                S.op("pool", lambda e, st=st: e.memset(st[:], 0.0), writes=[b_st])
            B = []
            for d in range(2):
                nb = {}
                for nm, shp in (("kT", [P, H, CH]), ("qT", [P, H, CH]), ("ktok", [CH, H, HD]), ("vtok", [CH, H, HD]), ("gb", [CH, 48]),
                                ("eg", [CH, 24]), ("egl", [P, H]), ("Ug", [CH, H, CH]), ("Lg", [CH, H, CH]), ("E", [CH, H, CH]), ("ET", [CH, H, CH]),
                                ("decST", [CH, H, CH]), ("Rt", [CH, H, CH]), ("kdec", [CH, H, HD]), ("nwpT", [P, 4, CH]), ("vnew", [CH, 4, HD]),
                                ("osb", [CH, H, HD]), ("otmp", [CH, 4, HD])):
                    nb[nm] = self.sb(es, "g%d_%s" % (d, nm), shp)
                for al, tgt in (("decS", "E"), ("decIT", "ET"), ("Mm", "E"), ("MmT", "decST"), ("qkT", "ET"), ("diagb", "Ug"),
                                ("N0", "E"), ("P0", "decST"), ("N1", "Ug"), ("P1", "Lg"), ("kg", "ktok")):
                    nb[al] = nb[tgt]
                B.append(nb)
            A2 = [(self.tpall[:, 0:1024], [self.tp[0][1], self.tp[1][1]]), (self.tpall[:, 1024:2048], [self.tp[2][1], self.tp[3][1]])]
            a2i = [0]

            def big():
                r = A2[a2i[0] % 2]
                a2i[0] += 1
                return r

            def bc(ap, n):
                return ap.unsqueeze(2).broadcast_to([ap.shape[0], ap.shape[1], n])

            for step in range(NCH):
                for d in range(2):
                    c0 = order[d][step] * CH
                    b = B[d]
                    st, b_st = Sst[d]
                    kT, b_kT = b["kT"]; qT, b_qT = b["qT"]; ktok, b_ktok = b["ktok"]; vtok, b_vtok = b["vtok"]; gb, b_gb = b["gb"]
                    S.dma(kT[:], self.KNT[:, :, c0:c0 + CH].rearrange("h p t -> p h t"), writes=[b_kT])
                    S.dma(qT[:], self.QNT[:, :, c0:c0 + CH].rearrange("h p t -> p h t"), writes=[b_qT])
                    S.dma(ktok[:], self.KTOK[c0:c0 + CH, :].rearrange("p (h d) -> p h d", d=HD), writes=[b_ktok])
                    S.dma(vtok[:], self.VTOK[c0:c0 + CH, :].rearrange("p (h d) -> p h d", d=HD), writes=[b_vtok])
                    S.dma(gb[:], self.GB[c0:c0 + CH, :], writes=[b_gb])
                    g = gb[:, d * H:(d + 1) * H]
                    beta = gb[:, 24 + d * H:24 + (d + 1) * H]
                    MU, b_MU = msk[("MU", d)]; MLs, b_MLs = msk[("MLs", d)]
                    Scs, b_Scs = msk[("Scs", d)]; Ssc, b_Ssc = msk[("Ssc", d)]; Isc, b_Isc = msk[("Isc", d)]
                    ps, b_ps = self.next_ps(4, 8)
                    S.op("pe", lambda e, ps=ps, MU=MU, g=g: e.matmul(out=ps[0:CH, 0:H], lhsT=MU[:, 0, :], rhs=g, start=True, stop=True), reads=[b_MU, b_gb], writes=[b_ps])
                    S.op("pe", lambda e, ps=ps, MLs=MLs, g=g: e.matmul(out=ps[0:CH, H:2 * H], lhsT=MLs[:, 0, :], rhs=g, start=True, stop=True), reads=[b_MLs, b_gb], writes=[b_ps])
                    S.op("pe", lambda e, ps=ps, g=g: e.matmul(out=ps[:, 2 * H:3 * H], lhsT=ones[:], rhs=g, start=True, stop=True), reads=[b_ones, b_gb], writes=[b_ps])
                    eg, b_eg = b["eg"]; egl, b_egl = b["egl"]
                    S.op("act", lambda e, ps=ps, eg=eg: e.activation(out=eg[:], in_=ps[0:CH, 0:2 * H], func=AF.Exp), reads=[b_ps], writes=[b_eg])
                    S.op("act", lambda e, ps=ps, egl=egl: e.activation(out=egl[:], in_=ps[:, 2 * H:3 * H], func=AF.Exp), reads=[b_ps], writes=[b_egl])
                    egc, edec = eg[:, 0:H], eg[:, H:2 * H]
                    Ug, b_Ug = b["Ug"]; Lg, b_Lg = b["Lg"]
                    S.op("dve", lambda e, Ug=Ug, MU=MU, g=g: e.tensor_tensor(out=Ug[:], in0=MU[:], in1=bc(g, CH), op=ALU.mult), reads=[b_MU, b_gb], writes=[b_Ug])
                    S.op("pool", lambda e, Lg=Lg, MLs=MLs, g=g: e.tensor_tensor(out=Lg[:], in0=MLs[:], in1=bc(g, CH), op=ALU.mult), reads=[b_MLs, b_gb], writes=[b_Lg])
                    Dm, b_Dm = big()
                    DmT, b_DmT = big()
                    for h in range(H):
                        S.op("pe", lambda e, Dm=Dm, Ug=Ug, MLs=MLs, h=h: e.matmul(out=Dm[0:CH, h * CH:(h + 1) * CH], lhsT=Ug[:, h, :], rhs=MLs[:, 0, :], start=True, stop=True),
                             reads=[b_Ug, b_MLs], writes=b_Dm)
                    for h in range(H):
                        S.op("pe", lambda e, DmT=DmT, Lg=Lg, MU=MU, h=h: e.matmul(out=DmT[0:CH, h * CH:(h + 1) * CH], lhsT=Lg[:, h, :], rhs=MU[:, 0, :], start=True, stop=True),
                             reads=[b_Lg, b_MU], writes=b_DmT)
                    E, b_E = b["E"]; ET, b_ET = b["ET"]
                    fl = lambda t: t[:].rearrange("p h c -> p (h c)")
                    S.op("act", lambda e, E=E, Dm=Dm: e.activation(out=fl(E), in_=Dm[0:CH, 0:H * CH], func=AF.Exp), reads=b_Dm, writes=[b_E])
                    S.op("act", lambda e, ET=ET, DmT=DmT: e.activation(out=fl(ET), in_=DmT[0:CH, 0:H * CH], func=AF.Exp), reads=b_DmT, writes=[b_ET])
                    decS, b_decS = b["decS"]; decST, b_decST = b["decST"]; decIT, b_decIT = b["decIT"]
                    S.op("dve", lambda e, decS=decS, E=E, Scs=Scs: e.tensor_tensor(out=decS[:], in0=E[:], in1=Scs[:], op=ALU.mult), reads=[b_E, b_Scs], writes=[b_decS])
                    S.op("pool", lambda e, decST=decST, ET=ET, Ssc=Ssc: e.tensor_tensor(out=decST[:], in0=ET[:], in1=Ssc[:], op=ALU.mult), reads=[b_ET, b_Ssc], writes=[b_decST])
                    S.op("pool", lambda e, decIT=decIT, ET=ET, Isc=Isc: e.tensor_tensor(out=decIT[:], in0=ET[:], in1=Isc[:], op=ALU.mult), reads=[b_ET, b_Isc], writes=[b_decIT])
                    G, b_G = big()
                    QK, b_QK = big()
                    for h in range(H):
                        S.op("pe", lambda e, G=G, kT=kT, h=h: e.matmul(out=G[0:CH, h * CH:(h + 1) * CH], lhsT=kT[:, h, :], rhs=kT[:, h, :], start=True, stop=True), reads=[b_kT], writes=b_G)
                    for h in range(H):
                        S.op("pe", lambda e, QK=QK, kT=kT, qT=qT, h=h: e.matmul(out=QK[0:CH, h * CH:(h + 1) * CH], lhsT=kT[:, h, :], rhs=qT[:, h, :], start=True, stop=True),
                             reads=[b_kT, b_qT], writes=b_QK)
                    Mm, b_Mm = b["Mm"]; MmT, b_MmT = b["MmT"]; qkT, b_qkT = b["qkT"]
                    S.op("dve", lambda e, Mm=Mm, G=G, decS=decS: e.tensor_tensor(out=fl(Mm), in0=G[0:CH, 0:H * CH], in1=fl(decS), op=ALU.mult), reads=b_G + [b_decS], writes=[b_Mm])
                    S.op("dve", lambda e, MmT=MmT, G=G, decST=decST: e.tensor_tensor(out=fl(MmT), in0=G[0:CH, 0:H * CH], in1=fl(decST), op=ALU.mult), reads=b_G + [b_decST], writes=[b_MmT])
                    S.op("dve", lambda e, qkT=qkT, QK=QK, decIT=decIT: e.tensor_tensor(out=fl(qkT), in0=QK[0:CH, 0:H * CH], in1=fl(decIT), op=ALU.mult), reads=b_QK + [b_decIT], writes=[b_qkT])
                    diagb, b_diagb = b["diagb"]
                    S.op("pool", lambda e, diagb=diagb, beta=beta: e.tensor_tensor(out=diagb[:], in0=i12[:], in1=bc(beta, CH), op=ALU.mult), reads=[b_i12, b_gb], writes=[b_diagb])
                    Br, b_Br = big()
                    for h in range(H):
                        S.op("pe", lambda e, Br=Br, diagb=diagb, h=h: e.matmul(out=Br[0:CH, h * CH:(h + 1) * CH], lhsT=ones[:, 0:CH], rhs=diagb[:, h, :], start=True, stop=True),
                             reads=[b_ones, b_diagb], writes=b_Br)
                    Nn = [b["N0"], b["N1"]]
                    Pp = [b["P0"], b["P1"]]
                    Rt, b_Rt = b["Rt"]
                    S.op("dve", lambda e, N0=Nn[0][0], Mm=Mm, Br=Br: e.scalar_tensor_tensor(out=fl(N0), in0=fl(Mm), scalar=-1.0, in1=Br[0:CH, 0:H * CH], op0=ALU.mult, op1=ALU.mult),
                         reads=[b_Mm] + b_Br, writes=[Nn[0][1]])
                    S.op("dve", lambda e, P0=Pp[0][0], MmT=MmT, beta=beta: e.scalar_tensor_tensor(out=P0[:], in0=MmT[:], scalar=-1.0, in1=bc(beta, CH), op0=ALU.mult, op1=ALU.mult),
                         reads=[b_MmT, b_gb], writes=[Pp[0][1]])
                    S.op("pool", lambda e, Rt=Rt, P0=Pp[0][0]: e.tensor_tensor(out=Rt[:], in0=P0[:], in1=i12[:], op=ALU.add), reads=[Pp[0][1], b_i12], writes=[b_Rt])
                    for j in range(1, 6):
                        Np, b_Np = Nn[(j - 1) % 2]
                        Pq, b_Pq = Pp[(j - 1) % 2]
                        Nc, b_Nc = Nn[j % 2]
                        Pc, b_Pc = Pp[j % 2]
                        nps, b_nps = big()
                        for h in range(H):
                            S.op("pe", lambda e, nps=nps, Pq=Pq, Np=Np, h=h: e.matmul(out=nps[0:CH, h * CH:(h + 1) * CH], lhsT=Pq[:, h, :], rhs=Np[:, h, :], start=True, stop=True),
                                 reads=[b_Pq, b_Np], writes=b_nps)
                        if j < 5:
                            pps, b_pps = big()
                            for h in range(H):
                                S.op("pe", lambda e, pps=pps, Pq=Pq, Np=Np, h=h: e.matmul(out=pps[0:CH, h * CH:(h + 1) * CH], lhsT=Np[:, h, :], rhs=Pq[:, h, :], start=True, stop=True),
                                     reads=[b_Pq, b_Np], writes=b_pps)
                        S.op("act", lambda e, Nc=Nc, nps=nps: e.copy(out=fl(Nc), in_=nps[0:CH, 0:H * CH]), reads=b_nps, writes=[b_Nc])
                        if j < 5:
                            S.op("dve", lambda e, Pc=Pc, pps=pps: e.tensor_copy(out=fl(Pc), in_=pps[0:CH, 0:H * CH]), reads=b_pps, writes=[b_Pc])
                        rps, b_rps = big()
                        for h in range(H):
                            S.op("pe", lambda e, rps=rps, Nc=Nc, Rt=Rt, h=h: e.matmul(out=rps[0:CH, h * CH:(h + 1) * CH], lhsT=Nc[:, h, :], rhs=Rt[:, h, :], start=True, stop=True),
                                 reads=[b_Nc, b_Rt], writes=b_rps)
                        S.op("dve", lambda e, Rt=Rt, rps=rps: e.tensor_tensor(out=fl(Rt), in0=fl(Rt), in1=rps[0:CH, 0:H * CH], op=ALU.add), reads=[b_Rt] + b_rps, writes=[b_Rt])
                    kg, b_kg = b["kg"]; kdec, b_kdec = b["kdec"]; osb, b_osb = b["osb"]
                    S.op("pool", lambda e, kdec=kdec, ktok=ktok, edec=edec: e.tensor_tensor(out=kdec[:], in0=ktok[:], in1=bc(edec, HD), op=ALU.mult), reads=[b_ktok, b_eg], writes=[b_kdec])
                    S.op("dve", lambda e, kg=kg, ktok=ktok, egc=egc: e.tensor_tensor(out=kg[:], in0=ktok[:], in1=bc(egc, HD), op=ALU.mult), reads=[b_ktok, b_eg], writes=[b_kg])
                    nwpT, b_nwpT = b["nwpT"]; vnew, b_vnew = b["vnew"]; otmp, b_otmp = b["otmp"]
                    for hg in range(0, H, 4):
                        wps, b_wps = self.next_ps(4, 8)
                        for hh in range(4):
                            h = hg + hh
                            S.op("pe", lambda e, wps=wps, kg=kg, Rt=Rt, h=h, hh=hh: e.matmul(out=wps[:, hh * CH:(hh + 1) * CH], lhsT=kg[:, h, :], rhs=Rt[:, h, :], start=True, stop=True),
                                 reads=[b_kg, b_Rt], writes=[b_wps])
                        S.op("act", lambda e, nwpT=nwpT, wps=wps: e.activation(out=nwpT[:].rearrange("p h c -> p (h c)"), in_=wps[:, 0:4 * CH], func=AF.Copy, scale=-1.0), reads=[b_wps], writes=[b_nwpT])
                        vps, b_vps = self.next_ps(4, 8)
                        for hh in range(4):
                            h = hg + hh
                            S.op("pe", lambda e, vps=vps, Rt=Rt, vtok=vtok, h=h, hh=hh: e.matmul(out=vps[0:CH, hh * HD:(hh + 1) * HD], lhsT=Rt[:, h, :], rhs=vtok[:, h, :], start=True, stop=False),
                                 reads=[b_Rt, b_vtok], writes=[b_vps])
                            S.op("pe", lambda e, vps=vps, nwpT=nwpT, st=st, h=h, hh=hh: e.matmul(out=vps[0:CH, hh * HD:(hh + 1) * HD], lhsT=nwpT[:, hh, :], rhs=st[:, h, :], start=False, stop=True),
                                 reads=[b_nwpT, b_st], writes=[b_vps])
                        S.op("dve", lambda e, vnew=vnew, vps=vps, beta=beta, hg=hg: e.tensor_tensor(out=vnew[:], in0=vps[0:CH, :].rearrange("p (h d) -> p h d", d=HD), in1=bc(beta[:, hg:hg + 4], HD), op=ALU.mult),
                             reads=[b_vps, b_gb], writes=[b_vnew])
                        o1, b_o1 = self.next_ps(4, 8)
                        for hh in range(4):
                            h = hg + hh
                            S.op("pe", lambda e, o1=o1, qT=qT, st=st, h=h, hh=hh: e.matmul(out=o1[0:CH, hh * HD:(hh + 1) * HD], lhsT=qT[:, h, :], rhs=st[:, h, :], start=True, stop=True),
                                 reads=[b_qT, b_st], writes=[b_o1])
                        S.op("dve", lambda e, otmp=otmp, o1=o1, egc=egc, hg=hg: e.tensor_tensor(out=otmp[:], in0=o1[0:CH, :].rearrange("p (h d) -> p h d", d=HD), in1=bc(egc[:, hg:hg + 4], HD), op=ALU.mult),
                             reads=[b_o1, b_eg], writes=[b_otmp])
                        o2, b_o2 = self.next_ps(4, 8)
                        for hh in range(4):
                            h = hg + hh
                            S.op("pe", lambda e, o2=o2, qkT=qkT, vnew=vnew, h=h, hh=hh: e.matmul(out=o2[0:CH, hh * HD:(hh + 1) * HD], lhsT=qkT[:, h, :], rhs=vnew[:, hh, :], start=True, stop=True),
                                 reads=[b_qkT, b_vnew], writes=[b_o2])
                        S.op("dve", lambda e, osb=osb, otmp=otmp, o2=o2, hg=hg: e.tensor_tensor(out=osb[:, hg:hg + 4, :], in0=otmp[:], in1=o2[0:CH, :].rearrange("p (h d) -> p h d", d=HD), op=ALU.add),
                             reads=[b_otmp, b_o2], writes=[b_osb])
                        su, b_su = self.next_ps(4, 8)
                        for hh in range(4):
                            h = hg + hh
                            S.op("pe", lambda e, su=su, kdec=kdec, vnew=vnew, h=h, hh=hh: e.matmul(out=su[:, hh * HD:(hh + 1) * HD], lhsT=kdec[:, h, :], rhs=vnew[:, hh, :], start=True, stop=True),
                                 reads=[b_kdec, b_vnew], writes=[b_su])
                        S.op("pool", lambda e, st=st, egl=egl, hg=hg: e.tensor_tensor(out=st[:, hg:hg + 4, :], in0=st[:, hg:hg + 4, :], in1=bc(egl[:, hg:hg + 4], HD), op=ALU.mult),
                             reads=[b_st, b_egl], writes=[b_st])
                        S.op("dve", lambda e, st=st, su=su, hg=hg: e.tensor_tensor(out=st[:, hg:hg + 4, :], in0=st[:, hg:hg + 4, :], in1=su[:, :].rearrange("p (h d) -> p h d", d=HD), op=ALU.add),
                             reads=[b_st, b_su], writes=[b_st])
                    S.dma(self.OG[d][c0:c0 + CH, :].rearrange("p (h d) -> p h d", d=HD), osb[:], reads=[b_osb])
        self.phase_end()

    def p2_gdn_gate(self, l):
        import contextlib
        S, cfg = self.S, self.cfg
        with contextlib.ExitStack() as es:
            gw, b_gw = self.bcast_tile(es, "gnw", self.ins["gdnp2"][l], 0, HD)
            o0 = [self.sb(es, "go0_%d" % i, [P, GH, HD]) for i in range(2)]
            o1 = [self.sb(es, "go1_%d" % i, [P, GH, HD]) for i in range(2)]
            z = [self.sb(es, "gz_%d" % i, [P, GH, HD]) for i in range(2)]
            sq, b_sq = self.sb(es, "gsq", [P, GH, HD])
            ss, b_ss = self.sb(es, "gss", [P, GH])
            for t in range(cfg.NT):
                a, b_a = o0[t % 2]
                bb, b_bb = o1[t % 2]
                zz, b_zz = z[t % 2]
                S.dma(a[:], self.OG[0][t * P:(t + 1) * P, :].rearrange("p (h d) -> p h d", d=HD), writes=[b_a])
                S.dma(bb[:], self.OG[1][t * P:(t + 1) * P, :].rearrange("p (h d) -> p h d", d=HD), writes=[b_bb])
                S.dma(zz[:], self.PZ[t * P:(t + 1) * P, :].rearrange("p (h d) -> p h d", d=HD), writes=[b_zz])
                S.op("dve", lambda e, a=a, bb=bb: e.tensor_tensor(out=a[:], in0=a[:], in1=bb[:], op=ALU.add), reads=[b_a, b_bb], writes=[b_a])
                S.op("pool", lambda e, a=a: e.tensor_tensor(out=sq[:], in0=a[:], in1=a[:], op=ALU.mult), reads=[b_a], writes=[b_sq])
                S.op("dve", lambda e: e.tensor_reduce(out=ss[:], in_=sq[:], axis=AX.X, op=ALU.add), reads=[b_sq], writes=[b_ss])
                S.op("act", lambda e: e.activation(out=ss[:], in_=ss[:], func=AF.Sqrt, scale=1.0 / HD, bias=1e-6), reads=[b_ss], writes=[b_ss])
                S.op("dve", lambda e: e.reciprocal(out=ss[:], in_=ss[:]), reads=[b_ss], writes=[b_ss])
                S.op("act", lambda e, zz=zz: e.activation(out=zz[:], in_=zz[:], func=AF.Silu), reads=[b_zz], writes=[b_zz])
                S.op("dve", lambda e, a=a: e.tensor_tensor(out=a[:], in0=a[:], in1=ss[:].unsqueeze(2).broadcast_to([P, GH, HD]), op=ALU.mult), reads=[b_a, b_ss], writes=[b_a])
                S.op("pool", lambda e, a=a: e.tensor_tensor(out=a[:], in0=a[:], in1=gw[:].unsqueeze(1).broadcast_to([P, GH, HD]), op=ALU.mult), reads=[b_a, b_gw], writes=[b_a])
                S.op("dve", lambda e, a=a, zz=zz: e.tensor_tensor(out=a[:], in0=a[:], in1=zz[:], op=ALU.mult), reads=[b_a, b_zz], writes=[b_a])
                S.dma(self.MIX[t * P:(t + 1) * P, 0:1536].rearrange("p (h d) -> p h d", d=HD), a[:], reads=[b_a])
        self.phase_end()

    def p6_moe(self, l, dbg=''):
        import contextlib
        S, cfg = self.S, self.cfg
        DFF = cfg.DFF
        FC = DFF // P
        TB = 2
        w1v = self.W1B[l].rearrange("e (c p) f -> e p c f", p=P)
        w3v = self.W3B[l].rearrange("e (c p) f -> e p c f", p=P)
        w2v = self.W2B[l].rearrange("e (c p) n -> e p c n", p=P)
        with contextlib.ExitStack() as es:
            wr, b_wr = self.sb(es, "wr", [P, KD, 36])
            S.dma(wr[:], self.ins["wr"][l].rearrange("(c p) n -> p c n", p=P), writes=[b_wr])
            rb, b_rb = self.bcast_tile(es, "rb", self.ins["rb"][l], 0, 36)
            ht = [self.sb(es, "m_h%d" % i, [P, D]) for i in range(1)]
            hTf, b_hTf = self.sb(es, "m_hTf", [P, KD, P])
            hTb, b_hTb = self.sb(es, "m_hTb", [P, KD, TB * P], BF16)
            yacc, b_yacc = self.sb(es, "m_yacc", [P, TB, D])
            W1, b_W1 = self.sb(es, "m_W1", [P, KD, DFF], BF16)
            W3, b_W3 = self.sb(es, "m_W3", [P, KD, DFF], BF16)
            W2, b_W2 = self.sb(es, "m_W2", [P, FC, D], BF16)
            gates, b_gates = self.sb(es, "m_gates", [P, TB, NE])
            sm = {nm: self.sb(es, "m_" + nm, shp) for nm, shp in (("lg", [P, 36]), ("gmax", [P, 1]), ("ngmax", [P, 1]), ("ohg", [P, 4]), ("ex", [P, 4]), ("se", [P, 1]),
                                                                 ("les", [P, 8]), ("m1", [P, 1]), ("oh1", [P, 8]), ("le2", [P, 8]), ("m2", [P, 1]), ("oh2", [P, 8]),
                                                                 ("dd", [P, 1]), ("w1", [P, 1]), ("w2", [P, 1]), ("g8", [P, 8]))}
            s1 = [self.sb(es, "m_s1_%d" % i, [P, TB * P]) for i in range(2)]
            gT, b_gT = self.sb(es, "m_gT", [P, FC, TB * P], BF16)
            nblk = (cfg.NT + TB - 1) // TB
            for bi in range(nblk):
                tiles = list(range(bi * TB, min(cfg.NT, (bi + 1) * TB)))
                ntok = len(tiles) * P
                for j, t in enumerate(tiles):
                    h, b_h = ht[0]
                    S.dma(h[:], self.HM[t * P:(t + 1) * P, :], writes=[b_h])
                    for c4 in range(0, KD, 4):
                        ps, b_ps = self.next_ps()
                        for c in range(c4, c4 + 4):
                            S.op("pe", lambda e, ps=ps, h=h, c=c, c4=c4: e.transpose(out=ps[:, (c - c4) * P:(c - c4 + 1) * P], in_=h[:, c * P:(c + 1) * P], identity=self.identf[:]),
                                 reads=[b_h, self.b_identf], writes=[b_ps])
                        S.op("act", lambda e, ps=ps, c4=c4: e.copy(out=hTf[:, c4:c4 + 4, :], in_=ps[:].rearrange("p (c t) -> p c t", c=4)), reads=[b_ps], writes=[b_hTf])
                        S.op("dve", lambda e, c4=c4, j=j: e.tensor_copy(out=hTb[:, c4:c4 + 4, j * P:(j + 1) * P], in_=hTf[:, c4:c4 + 4, :]), reads=[b_hTf], writes=[b_hTb])
                    ps, b_ps = self.next_ps()
                    for c in range(KD if 'w' not in dbg else 0):
                        S.op("pe", lambda e, ps=ps, c=c: e.matmul(out=ps[:, 0:36], lhsT=hTf[:, c, :], rhs=wr[:, c, :], start=(c == 0), stop=(c == KD - 1)), reads=[b_hTf, b_wr], writes=[b_ps])
                    if 'r' in dbg:
                        continue
                    V = {k_: v_[0] for k_, v_ in sm.items()}
                    Bf = {k_: v_[1] for k_, v_ in sm.items()}

                    def dv(fn, r, w):
                        S.op("dve", fn, reads=[Bf[x] if isinstance(x, str) else x for x in r], writes=[Bf[x] if isinstance(x, str) else x for x in w])
                    dv(lambda e, ps=ps: e.tensor_tensor(out=V["lg"][:], in0=ps[:, 0:36], in1=rb[:], op=ALU.add), [b_ps, b_rb], ["lg"])
                    dv(lambda e: e.tensor_reduce(out=V["gmax"][:], in_=V["lg"][:, 0:4], axis=AX.X, op=ALU.max), ["lg"], ["gmax"])
                    dv(lambda e: e.tensor_scalar(out=V["ohg"][:], in0=V["lg"][:, 0:4], scalar1=V["gmax"][:, 0:1], scalar2=None, op0=ALU.is_ge), ["lg", "gmax"], ["ohg"])
                    dv(lambda e: e.tensor_scalar(out=V["ngmax"][:], in0=V["gmax"][:], scalar1=-1.0, scalar2=None, op0=ALU.mult), ["gmax"], ["ngmax"])
                    S.op("act", lambda e: e.activation(out=V["ex"][:], in_=V["lg"][:, 0:4], func=AF.Exp, bias=V["ngmax"][:, 0:1], scale=1.0), reads=[Bf["lg"], Bf["ngmax"]], writes=[Bf["ex"]])
                    dv(lambda e: e.tensor_reduce(out=V["se"][:], in_=V["ex"][:], axis=AX.X, op=ALU.add), ["ex"], ["se"])
                    dv(lambda e: e.reciprocal(out=V["se"][:], in_=V["se"][:]), ["se"], ["se"])
                    dv(lambda e: e.tensor_scalar(out=V["les"][:], in0=V["lg"][:, 4:12], scalar1=V["ohg"][:, 0:1], scalar2=None, op0=ALU.mult), ["lg", "ohg"], ["les"])
                    for g in range(1, 4):
                        dv(lambda e, g=g: e.scalar_tensor_tensor(out=V["les"][:], in0=V["lg"][:, 4 + 8 * g:12 + 8 * g], scalar=V["ohg"][:, g:g + 1], in1=V["les"][:], op0=ALU.mult, op1=ALU.add),
                           ["lg", "ohg", "les"], ["les"])
                    dv(lambda e: e.tensor_reduce(out=V["m1"][:], in_=V["les"][:], axis=AX.X, op=ALU.max), ["les"], ["m1"])
                    dv(lambda e: e.tensor_scalar(out=V["oh1"][:], in0=V["les"][:], scalar1=V["m1"][:, 0:1], scalar2=None, op0=ALU.is_ge), ["les", "m1"], ["oh1"])
                    dv(lambda e: e.scalar_tensor_tensor(out=V["le2"][:], in0=V["oh1"][:], scalar=-1e30, in1=V["les"][:], op0=ALU.mult, op1=ALU.add), ["oh1", "les"], ["le2"])
                    dv(lambda e: e.tensor_reduce(out=V["m2"][:], in_=V["le2"][:], axis=AX.X, op=ALU.max), ["le2"], ["m2"])
                    dv(lambda e: e.tensor_scalar(out=V["oh2"][:], in0=V["le2"][:], scalar1=V["m2"][:, 0:1], scalar2=None, op0=ALU.is_ge), ["le2", "m2"], ["oh2"])
                    dv(lambda e: e.tensor_tensor(out=V["dd"][:], in0=V["m2"][:], in1=V["m1"][:], op=ALU.subtract), ["m1", "m2"], ["dd"])
                    S.op("act", lambda e: e.activation(out=V["dd"][:], in_=V["dd"][:], func=AF.Exp), reads=[Bf["dd"]], writes=[Bf["dd"]])
                    dv(lambda e: e.tensor_scalar(out=V["w1"][:], in0=V["dd"][:], scalar1=1.0, scalar2=None, op0=ALU.add), ["dd"], ["w1"])
                    dv(lambda e: e.reciprocal(out=V["w1"][:], in_=V["w1"][:]), ["w1"], ["w1"])
                    dv(lambda e: e.tensor_tensor(out=V["w2"][:], in0=V["dd"][:], in1=V["w1"][:], op=ALU.mult), ["dd", "w1"], ["w2"])
                    dv(lambda e: e.tensor_tensor(out=V["w1"][:], in0=V["w1"][:], in1=V["se"][:], op=ALU.mult), ["w1", "se"], ["w1"])
                    dv(lambda e: e.tensor_tensor(out=V["w2"][:], in0=V["w2"][:], in1=V["se"][:], op=ALU.mult), ["w2", "se"], ["w2"])
                    dv(lambda e: e.tensor_scalar(out=V["g8"][:], in0=V["oh1"][:], scalar1=V["w1"][:, 0:1], scalar2=None, op0=ALU.mult), ["oh1", "w1"], ["g8"])
                    dv(lambda e: e.scalar_tensor_tensor(out=V["g8"][:], in0=V["oh2"][:], scalar=V["w2"][:, 0:1], in1=V["g8"][:], op0=ALU.mult, op1=ALU.add), ["oh2", "w2", "g8"], ["g8"])
                    for g in range(4):
                        dv(lambda e, g=g, j=j: e.tensor_scalar(out=gates[:, j, 8 * g:8 * g + 8], in0=V["g8"][:], scalar1=V["ohg"][:, g:g + 1], scalar2=None, op0=ALU.mult), ["g8", "ohg"], [b_gates])
                for ex in range(0 if 'x' not in dbg else NE, NE):
                    S.dma(W1[:], w1v[ex], writes=[b_W1])
                    S.dma(W3[:], w3v[ex], writes=[b_W3])
                    S.dma(W2[:], w2v[ex], writes=[b_W2])
                    for fc in range(FC):
                        p1, b_p1 = self.next_ps()
                        p3, b_p3 = self.next_ps()
                        for c in range(KD):
                            S.op("pe", lambda e, p1=p1, c=c, fc=fc, ntok=ntok: e.matmul(out=p1[:, 0:ntok], lhsT=W1[:, c, fc * P:(fc + 1) * P], rhs=hTb[:, c, 0:ntok], start=(c == 0), stop=(c == KD - 1)),
                                 reads=[b_W1, b_hTb], writes=[b_p1])
                        for c in range(KD):
                            S.op("pe", lambda e, p3=p3, c=c, fc=fc, ntok=ntok: e.matmul(out=p3[:, 0:ntok], lhsT=W3[:, c, fc * P:(fc + 1) * P], rhs=hTb[:, c, 0:ntok], start=(c == 0), stop=(c == KD - 1)),
                                 reads=[b_W3, b_hTb], writes=[b_p3])
                        ss_, b_ss = s1[fc % 2]
                        S.op("act", lambda e, ss_=ss_, p1=p1, ntok=ntok: e.activation(out=ss_[:, 0:ntok], in_=p1[:, 0:ntok], func=AF.Silu), reads=[b_p1], writes=[b_ss])
                        S.op("dve", lambda e, ss_=ss_, p3=p3, fc=fc, ntok=ntok: e.tensor_tensor(out=gT[:, fc, 0:ntok], in0=p3[:, 0:ntok], in1=ss_[:, 0:ntok], op=ALU.mult), reads=[b_p3, b_ss], writes=[b_gT])
                    for j in range(len(tiles)):
                        for n0 in range(0, D, 512):
                            py, b_py = self.next_ps()
                            for fc in range(FC):
                                S.op("pe", lambda e, py=py, fc=fc, j=j, n0=n0: e.matmul(out=py[:], lhsT=gT[:, fc, j * P:(j + 1) * P], rhs=W2[:, fc, n0:n0 + 512], start=(fc == 0), stop=(fc == FC - 1)),
                                     reads=[b_gT, b_W2], writes=[b_py])
                            if ex == 0:
                                S.op("dve", lambda e, py=py, j=j, n0=n0, ex=ex: e.tensor_scalar(out=yacc[:, j, n0:n0 + 512], in0=py[:], scalar1=gates[:, j, ex:ex + 1], scalar2=None, op0=ALU.mult),
                                     reads=[b_py, b_gates], writes=[b_yacc])
                            else:
                                S.op("dve", lambda e, py=py, j=j, n0=n0, ex=ex: e.scalar_tensor_tensor(out=yacc[:, j, n0:n0 + 512], in0=py[:], scalar=gates[:, j, ex:ex + 1], in1=yacc[:, j, n0:n0 + 512],
                                                                                                     op0=ALU.mult, op1=ALU.add), reads=[b_py, b_gates, b_yacc], writes=[b_yacc])
                for j, t in enumerate(tiles):
                    S.dma(self.Y1[t * P:(t + 1) * P, :], yacc[:, j, :], reads=[b_yacc])
        self.phase_end()
        if 'n' not in dbg:
            self.ln_pass(l, 2, self.Y1, make_h=False)


def build_program(cfg):
    import contextlib
    k = K(cfg)
    L, T, NCTX, DFF = cfg.L, cfg.T, cfg.NCTX, cfg.DFF
    k.inp("x", [T, D]); k.inp("ctx", [NCTX, D]); k.inp("cc", [P, KD, 2])
    k.inp("w_mod", [L, D, 6 * D]); k.inp("bm", [L, 2, 6 * D]); k.inp("w_in", [L, D, NIN]); k.inp("w_out", [L, D, D])
    k.inp("fn_w", [L, 1024, 1024]); k.inp("lnp", [L, 2, 2, D]); k.inp("qknw", [L, 2, HD]); k.inp("rope", [T, HD])
    k.inp("convw", [L, P, 36, 5]); k.inp("gdnp", [L, 2, 24]); k.inp("gdnp2", [L, 2, HD])
    k.inp("gmask", [2, 5, CH, GH, CH]); k.inp("gmask_i", [CH, GH, CH])
    k.inp("dft_ch", [P, 256], BF16); k.inp("dft_c", [T, T], BF16); k.inp("dft_ns", [T, T], BF16)
    k.inp("dftc_c", [NCTX, NCTX], BF16); k.inp("dftc_ns", [NCTX, NCTX], BF16)
    k.inp("wr", [L, D, 36]); k.inp("rb", [L, 2, 36])
    k.inp("w1", [L, NE * D, DFF]); k.inp("w3", [L, NE * D, DFF]); k.inp("w2", [L, NE * DFF, D])
    k.inp("identf", [P, P]); k.inp("sel", [2, 2 * P])
    _ns = _moe_geom(cfg)[-1]
    k.inp("ustr", [P, P]); k.inp("slotpos", [P, _ns]); k.inp("pidx", [P, 1]); k.inp("iot8", [P, 8])
    out = k.nc.dram_tensor("out", [T, D], F32, kind="ExternalOutput").ap()
    k.alloc_scratch()
    k.alloc_moe()
    with contextlib.ExitStack() as es:
        k.consts(es)
        k.p0_mods()
        k.load_x()
        for l in range(L):
            last = l == L - 1
            k.cast_w(k.ins["w_in"][l], k.WINB[l], D, NIN)
            k.p1_proj(l)
            k.p2_gdn_prep(l)
            k.p2_gdn_main(l)
            k.p2_gdn_gate(l)
            k.p3_fnet(l, ctx_out=not last)
            k.p4_attn(l, ctx_out=not last)
            k.cast_w(k.ins["w_out"][l], k.WOUTB[l], D, D)
            k.p5_out(l)
            k.cast_moe(l)
            k.p6_moe_routed(l)
        st = [k.sb(es, "fo%d" % i, [P, D]) for i in range(2)]
        for t in range(cfg.NTC, cfg.NT):
            s, b_s = st[t % 2]
            k.S.dma(s[:], k.XRES[t * P:(t + 1) * P, :], writes=[b_s])
            k.S.dma(out[(t - cfg.NTC) * P:(t - cfg.NTC + 1) * P, :], s[:], reads=[b_s])
        k.finish()
    return k


def host_consts(cfg):
    c = {}
    c["identf"] = np.eye(P, dtype=np.float32)
    sel = np.zeros((2, 2 * P), np.float32)
    sel[0, :P] = 1
    sel[1, P:] = 1
    c["sel"] = sel
    m = np.arange(128)
    ang = 2 * np.pi * np.outer(m, m) / 128
    c["dft_ch"] = (np.concatenate([np.cos(ang), np.sin(ang)], 1) / np.sqrt(128)).astype(ml_dtypes.bfloat16)
    for nm, n in (("dft", cfg.T), ("dftc", cfg.NCTX)):
        t = np.arange(n, dtype=np.int64)
        a = 2 * np.pi * (np.outer(t, t) % n) / n
        c[nm + "_c"] = (np.cos(a) / np.sqrt(n)).astype(ml_dtypes.bfloat16)
        c[nm + "_ns"] = (-np.sin(a) / np.sqrt(n)).astype(ml_dtypes.bfloat16)
    rows = cfg.T // 64
    row = np.repeat(np.arange(rows, dtype=np.float32), 64)
    col = np.tile(np.arange(64, dtype=np.float32), rows)
    inv = (10000.0 ** (-np.arange(0, 64, 2, dtype=np.float32) / 64)).astype(np.float32)
    ang = np.concatenate([row[:, None] * inv, col[:, None] * inv], -1)
    c["rope"] = np.concatenate([np.cos(ang), np.sin(ang)], -1).astype(np.float32)
    t = np.arange(CH)
    mk = np.zeros((2, 5, CH, CH), np.float32)
    mk[0, 0] = t[:, None] <= t[None, :]; mk[0, 1] = t[:, None] > t[None, :]; mk[0, 2] = t[:, None] > t[None, :]
    mk[0, 3] = t[None, :] > t[:, None]; mk[0, 4] = t[None, :] >= t[:, None]
    mk[1, 0] = t[:, None] >= t[None, :]; mk[1, 1] = t[:, None] < t[None, :]; mk[1, 2] = t[:, None] < t[None, :]
    mk[1, 3] = t[None, :] < t[:, None]; mk[1, 4] = t[None, :] <= t[:, None]
    c["gmask"] = np.repeat(mk[:, :, :, None, :], GH, 3).copy()
    c["gmask_i"] = np.repeat(np.eye(CH, dtype=np.float32)[:, None, :], GH, 1).copy()
    ns = _moe_geom(cfg)[-1]
    tt = np.arange(P)
    c["ustr"] = (tt[:, None] < tt[None, :]).astype(np.float32)
    c["slotpos"] = np.tile((np.arange(ns, dtype=np.float32) * P)[None, :], (P, 1))
    c["pidx"] = np.arange(P, dtype=np.float32)[:, None].copy()
    c["iot8"] = np.tile(np.arange(8, dtype=np.float32)[None, :], (P, 1))
    return c


def host_inputs(cfg, inp, b, consts):
    L = cfg.L
    m = dict(consts)
    m["x"] = np.ascontiguousarray(inp["x"][b])
    m["ctx"] = np.ascontiguousarray(inp["ctx"][b])
    m["cc"] = np.ascontiguousarray(np.stack([inp["c"][b], inp["c_ctx"]], -1).reshape(KD, P, 2).transpose(1, 0, 2))
    m["w_mod"] = inp["w_mod"]
    m["bm"] = np.ascontiguousarray(np.stack([inp["b_mod"], inp["b_mod"]], 1))
    m["w_in"] = inp["w_in"]
    m["w_out"] = inp["w_out"]
    m["fn_w"] = inp["fn_w"]
    m["lnp"] = np.ascontiguousarray(np.stack([np.stack([inp["ln1_g"], inp["ln1_b"]], 1), np.stack([inp["ln2_g"], inp["ln2_b"]], 1)], 1))
    m["qknw"] = np.ascontiguousarray(np.stack([inp["q_norm"], inp["k_norm"]], 1))
    m["convw"] = np.ascontiguousarray(inp["gdn_conv"].reshape(L, 5, 36, P).transpose(0, 3, 2, 1))
    m["gdnp"] = np.ascontiguousarray(np.stack([inp["gdn_a_log"].reshape(L, 24), inp["gdn_dt_bias"].reshape(L, 24)], 1))
    m["gdnp2"] = np.ascontiguousarray(np.stack([inp["gdn_norm"], inp["gdn_norm"]], 1))
    m["wr"] = np.ascontiguousarray(np.concatenate([inp["router_g"], inp["router_e"]], -1))
    rbv = np.concatenate([inp["router_g_b"], inp["router_e_b"]], -1)
    m["rb"] = np.ascontiguousarray(np.stack([rbv, rbv], 1))
    m["w1"] = inp["w1"].reshape(L, NE * D, cfg.DFF)
    m["w3"] = inp["w3"].reshape(L, NE * D, cfg.DFF)
    m["w2"] = inp["w2"].reshape(L, NE * cfg.DFF, D)
    return m


_PROG = {}


def kernel(**inputs):
    inp = {k_: np.asarray(v) for k_, v in inputs.items()}
    B, T, _ = inp["x"].shape
    NCTX = inp["ctx"].shape[1]
    DFF = inp["w1"].shape[-1]
    cfg = Cfg(T=T, NCTX=NCTX, DFF=DFF, L=inp["w_mod"].shape[0])
    key = (T, NCTX, DFF, cfg.L)
    if key not in _PROG:
        _PROG[key] = build_program(cfg)
    k = _PROG[key]
    consts = host_consts(cfg)
    in_maps = [host_inputs(cfg, inp, b, consts) for b in range(B)]
    res = run_bass_kernel_spmd(k.nc, in_maps, core_ids=list(range(B)))
    return np.stack([res.results[b]["out"] for b in range(B)], 0).astype(np.float32)


U32 = mybir.dt.uint32


def _dma_custom(self, q, fn, reads, writes):
    d = "d%d" % self.dma_rr
    self.dma_rr = (self.dma_rr + 1) % self.n_dma
    if self.cnt[d] > 0 and self.waited.get((q, d), 0) < self.cnt[d]:
        self.prog[q].append(("wait", self.sem[d], self.cnt[d]))
        self.waited[(q, d)] = self.cnt[d]
    self._deps(q, reads, writes)
    self.cnt[d] += 16
    c = self.cnt[d]
    self.prog[q].append(("ins", fn, self.sem[d], 16))
    for b in reads:
        b.r[d] = c
    for b in writes:
        b.w = (d, c)
        b.r = {}


Sched.dma_custom = _dma_custom


def _moe_geom(cfg):
    DFF = cfg.DFF
    FC = DFF // P
    NPC1 = max(1, (KD * DFF * 2) // 16384)
    CPP = KD // NPC1
    NPC2 = max(1, (FC * D * 2) // 16384)
    FPP = FC // NPC2
    NS = (2 * cfg.S) // P + NE
    return FC, NPC1, CPP, NPC2, FPP, NS


def _alloc_moe(self):
    cfg = self.cfg
    FC, NPC1, CPP, NPC2, FPP, NS = _moe_geom(cfg)
    self.W1R = [[self.scratch("w1r%d_%d" % (l, i), [NE * P, CPP * cfg.DFF], BF16)[0] for i in range(NPC1)] for l in range(cfg.L)]
    self.W3R = [[self.scratch("w3r%d_%d" % (l, i), [NE * P, CPP * cfg.DFF], BF16)[0] for i in range(NPC1)] for l in range(cfg.L)]
    self.W2R = [[self.scratch("w2r%d_%d" % (l, i), [NE * P, FPP * D], BF16)[0] for i in range(NPC2)] for l in range(cfg.L)]
    HD2 = D // 2
    self.HS = [self.scratch("hs%d" % i, [NS * P, HD2])[0] for i in range(2)]
    self.YS = [self.scratch("ys%d" % i, [NS * P, HD2])[0] for i in range(2)]


def _cast_moe(self, l):
    import contextlib
    S, cfg = self.S, self.cfg
    DFF = cfg.DFF
    FC, NPC1, CPP, NPC2, FPP, NS = _moe_geom(cfg)
    items = []
    for src, dstl in ((self.ins["w1"][l], self.W1R[l]), (self.ins["w3"][l], self.W3R[l])):
        for e in range(NE):
            for c4 in range(0, KD, 4):
                s_ = src[e * D + c4 * P:e * D + (c4 + 4) * P, :].rearrange("(c p) f -> p c f", p=P)
                pc, off = c4 // CPP, (c4 % CPP) * DFF
                d_ = dstl[pc][e * P:(e + 1) * P, off:off + 4 * DFF].rearrange("p (c f) -> p c f", c=4)
                items.append((s_, d_, [P, 4, DFF]))
    for e in range(NE):
        for fc in range(FC):
            for n0 in range(0, D, 2048):
                s_ = self.ins["w2"][l][e * DFF + fc * P:e * DFF + (fc + 1) * P, n0:n0 + 2048]
                pc, off = fc // FPP, (fc % FPP) * D + n0
                d_ = self.W2R[l][pc][e * P:(e + 1) * P, off:off + 2048]
                items.append((s_, d_, [P, 2048]))
    with contextlib.ExitStack() as es:
        st = [self.sb(es, "cm_f%d" % i, [P, 2048]) for i in range(3)]
        sbf = [self.sb(es, "cm_b%d" % i, [P, 2048], BF16) for i in range(3)]
        engs = ["dve", "pool", "act"]
        for i, (s_, d_, shp) in enumerate(items):
            f, b_f = st[i % 3]
            o, b_o = sbf[i % 3]
            n = int(np.prod(shp[1:]))
            fv = f[:, 0:n] if len(shp) == 2 else f[:, 0:n].rearrange("p (c f) -> p c f", c=shp[1])
            ov = o[:, 0:n] if len(shp) == 2 else o[:, 0:n].rearrange("p (c f) -> p c f", c=shp[1])
            S.dma(fv, s_, writes=[b_f])
            en = engs[i % 3]
            if en == "act":
                S.op("act", lambda e, f=f, o=o, n=n: e.copy(out=o[:, 0:n], in_=f[:, 0:n]), reads=[b_f], writes=[b_o])
            else:
                S.op(en, lambda e, f=f, o=o, n=n: e.tensor_copy(out=o[:, 0:n], in_=f[:, 0:n]), reads=[b_f], writes=[b_o])
            S.dma(d_, ov, reads=[b_o])
    self.phase_end()


def _p6_moe_routed(self, l):
    import contextlib
    S, cfg = self.S, self.cfg
    DFF, NT = cfg.DFF, cfg.NT
    FC, NPC1, CPP, NPC2, FPP, NS = _moe_geom(cfg)
    IOA = bass.IndirectOffsetOnAxis
    with contextlib.ExitStack() as es:
        wr, b_wr = self.sb(es, "wr", [P, KD, 36])
        S.dma(wr[:], self.ins["wr"][l].rearrange("(c p) n -> p c n", p=P), writes=[b_wr])
        rb, b_rb = self.bcast_tile(es, "rb", self.ins["rb"][l], 0, 36)
        ustr, b_ustr = self.sb(es, "ustr", [P, P])
        S.dma(ustr[:], self.ins["ustr"], writes=[b_ustr])
        onesf, b_onesf = self.sb(es, "r_ones", [P, P])
        S.op("dve", lambda e: e.memset(onesf[:], 1.0), writes=[b_onesf])
        slotpos, b_slotpos = self.sb(es, "slotpos", [P, NS])
        S.dma(slotpos[:], self.ins["slotpos"], writes=[b_slotpos])
        pidx, b_pidx = self.sb(es, "pidx", [P, 1])
        S.dma(pidx[:], self.ins["pidx"], writes=[b_pidx])
        iot, b_iot = self.sb(es, "iot", [P, 8])
        S.dma(iot[:], self.ins["iot8"], writes=[b_iot])
        h, b_h = self.sb(es, "r_h", [P, D])
        hTf, b_hTf = self.sb(es, "r_hTf", [P, KD, P])
        OH, b_OH = self.sb(es, "r_OH", [P, NT, 2, NE])
        G12, b_G12 = self.G12, self.b_G12
        RANK, b_RANK = self.sb(es, "r_rank", [P, NT, 2])
        carry, b_carry = self.sb(es, "r_carry", [P, NE])
        S.op("dve", lambda e: e.memset(carry[:], 0.0), writes=[b_carry])
        tmp, b_tmp = self.sb(es, "r_tmp", [P, NE])
        sm = {nm: self.sb(es, "r_" + nm, shp) for nm, shp in (("lg", [P, 36]), ("gmax", [P, 1]), ("ngmax", [P, 1]), ("ohg", [P, 4]), ("ex", [P, 4]), ("se", [P, 1]),
                                                             ("les", [P, 8]), ("m1", [P, 1]), ("oh1", [P, 8]), ("le2", [P, 8]), ("m2", [P, 1]), ("oh2", [P, 8]),
                                                             ("dd", [P, 1]), ("w1", [P, 1]), ("w2", [P, 1]))}
        V = {k_: v_[0] for k_, v_ in sm.items()}
        Bf = {k_: v_[1] for k_, v_ in sm.items()}

        def dv(fn, r, w):
            S.op("dve", fn, reads=[Bf[x] if isinstance(x, str) else x for x in r], writes=[Bf[x] if isinstance(x, str) else x for x in w])
        for t in range(NT):
            S.dma(h[:], self.HM[t * P:(t + 1) * P, :], writes=[b_h])
            for c4 in range(0, KD, 4):
                ps, b_ps = self.next_ps()
                for c in range(c4, c4 + 4):
                    S.op("pe", lambda e, ps=ps, c=c, c4=c4: e.transpose(out=ps[:, (c - c4) * P:(c - c4 + 1) * P], in_=h[:, c * P:(c + 1) * P], identity=self.identf[:]),
                         reads=[b_h, self.b_identf], writes=[b_ps])
                S.op("act", lambda e, ps=ps, c4=c4: e.copy(out=hTf[:, c4:c4 + 4, :], in_=ps[:].rearrange("p (c t) -> p c t", c=4)), reads=[b_ps], writes=[b_hTf])
            ps, b_ps = self.next_ps()
            for c in range(KD):
                S.op("pe", lambda e, ps=ps, c=c: e.matmul(out=ps[:, 0:36], lhsT=hTf[:, c, :], rhs=wr[:, c, :], start=(c == 0), stop=(c == KD - 1)), reads=[b_hTf, b_wr], writes=[b_ps])
            dv(lambda e, ps=ps: e.tensor_tensor(out=V["lg"][:], in0=ps[:, 0:36], in1=rb[:], op=ALU.add), [b_ps, b_rb], ["lg"])
            dv(lambda e: e.tensor_reduce(out=V["gmax"][:], in_=V["lg"][:, 0:4], axis=AX.X, op=ALU.max), ["lg"], ["gmax"])
            dv(lambda e: e.tensor_scalar(out=V["ohg"][:], in0=V["lg"][:, 0:4], scalar1=V["gmax"][:, 0:1], scalar2=None, op0=ALU.is_ge), ["lg", "gmax"], ["ohg"])
            dv(lambda e: e.tensor_scalar(out=V["ngmax"][:], in0=V["gmax"][:], scalar1=-1.0, scalar2=None, op0=ALU.mult), ["gmax"], ["ngmax"])
            S.op("act", lambda e: e.activation(out=V["ex"][:], in_=V["lg"][:, 0:4], func=AF.Exp, bias=V["ngmax"][:, 0:1], scale=1.0), reads=[Bf["lg"], Bf["ngmax"]], writes=[Bf["ex"]])
            dv(lambda e: e.tensor_reduce(out=V["se"][:], in_=V["ex"][:], axis=AX.X, op=ALU.add), ["ex"], ["se"])
            dv(lambda e: e.reciprocal(out=V["se"][:], in_=V["se"][:]), ["se"], ["se"])
            dv(lambda e: e.tensor_scalar(out=V["les"][:], in0=V["lg"][:, 4:12], scalar1=V["ohg"][:, 0:1], scalar2=None, op0=ALU.mult), ["lg", "ohg"], ["les"])
            for g in range(1, 4):
                dv(lambda e, g=g: e.scalar_tensor_tensor(out=V["les"][:], in0=V["lg"][:, 4 + 8 * g:12 + 8 * g], scalar=V["ohg"][:, g:g + 1], in1=V["les"][:], op0=ALU.mult, op1=ALU.add),
                   ["lg", "ohg", "les"], ["les"])
            dv(lambda e: e.tensor_reduce(out=V["m1"][:], in_=V["les"][:], axis=AX.X, op=ALU.max), ["les"], ["m1"])
            dv(lambda e: e.tensor_scalar(out=V["oh1"][:], in0=V["les"][:], scalar1=V["m1"][:, 0:1], scalar2=None, op0=ALU.is_ge), ["les", "m1"], ["oh1"])
            dv(lambda e: e.scalar_tensor_tensor(out=V["le2"][:], in0=V["oh1"][:], scalar=-1e30, in1=V["les"][:], op0=ALU.mult, op1=ALU.add), ["oh1", "les"], ["le2"])
            dv(lambda e: e.tensor_reduce(out=V["m2"][:], in_=V["le2"][:], axis=AX.X, op=ALU.max), ["le2"], ["m2"])
            dv(lambda e: e.tensor_scalar(out=V["oh2"][:], in0=V["le2"][:], scalar1=V["m2"][:, 0:1], scalar2=None, op0=ALU.is_ge), ["le2", "m2"], ["oh2"])
            dv(lambda e: e.tensor_tensor(out=V["dd"][:], in0=V["m2"][:], in1=V["m1"][:], op=ALU.subtract), ["m1", "m2"], ["dd"])
            S.op("act", lambda e: e.activation(out=V["dd"][:], in_=V["dd"][:], func=AF.Exp), reads=[Bf["dd"]], writes=[Bf["dd"]])
            dv(lambda e: e.tensor_scalar(out=V["w1"][:], in0=V["dd"][:], scalar1=1.0, scalar2=None, op0=ALU.add), ["dd"], ["w1"])
            dv(lambda e: e.reciprocal(out=V["w1"][:], in_=V["w1"][:]), ["w1"], ["w1"])
            dv(lambda e: e.tensor_tensor(out=V["w2"][:], in0=V["dd"][:], in1=V["w1"][:], op=ALU.mult), ["dd", "w1"], ["w2"])
            dv(lambda e, t=t: e.tensor_tensor(out=G12[:, t, 0:1], in0=V["w1"][:], in1=V["se"][:], op=ALU.mult), ["w1", "se"], [b_G12])
            dv(lambda e, t=t: e.tensor_tensor(out=G12[:, t, 1:2], in0=V["w2"][:], in1=V["se"][:], op=ALU.mult), ["w2", "se"], [b_G12])
            for kk, ohn in ((0, "oh1"), (1, "oh2")):
                for g in range(4):
                    dv(lambda e, g=g, t=t, kk=kk, ohn=ohn: e.tensor_scalar(out=OH[:, t, kk, 8 * g:8 * g + 8], in0=V[ohn][:], scalar1=V["ohg"][:, g:g + 1], scalar2=None, op0=ALU.mult),
                       [ohn, "ohg"], [b_OH])
                ps2, b_ps2 = self.next_ps()
                S.op("pe", lambda e, ps2=ps2, t=t, kk=kk: e.matmul(out=ps2[:, 0:NE], lhsT=ustr[:], rhs=OH[:, t, kk, :], start=True, stop=True), reads=[b_ustr, b_OH], writes=[b_ps2])
                S.op("pe", lambda e, ps2=ps2, t=t, kk=kk: e.matmul(out=ps2[:, NE:2 * NE], lhsT=onesf[:], rhs=OH[:, t, kk, :], start=True, stop=True), reads=[b_onesf, b_OH], writes=[b_ps2])
                dv(lambda e, ps2=ps2: e.tensor_tensor(out=tmp[:], in0=ps2[:, 0:NE], in1=carry[:], op=ALU.add), [b_ps2, b_carry], [b_tmp])
                dv(lambda e, t=t, kk=kk: e.tensor_tensor(out=tmp[:], in0=tmp[:], in1=OH[:, t, kk, :], op=ALU.mult), [b_tmp, b_OH], [b_tmp])
                dv(lambda e, t=t, kk=kk: e.tensor_reduce(out=RANK[:, t, kk:kk + 1], in_=tmp[:], axis=AX.X, op=ALU.add), [b_tmp], [b_RANK])
                dv(lambda e, ps2=ps2: e.tensor_tensor(out=carry[:], in0=carry[:], in1=ps2[:, NE:2 * NE], op=ALU.add), [b_carry, b_ps2], [b_carry])
        ci, b_ci = self.sb(es, "r_ci", [P, NE], I32)
        pad, b_pad = self.sb(es, "r_pad", [P, NE])
        padT, b_padT = self.sb(es, "r_padT", [NE, P])
        base, b_base = self.sb(es, "r_base", [P, NE])
        bend, b_bend = self.sb(es, "r_bend", [P, NE])
        dv(lambda e: e.tensor_scalar(out=carry[:], in0=carry[:], scalar1=127.0, scalar2=None, op0=ALU.add), [b_carry], [b_carry])
        dv(lambda e: e.tensor_copy(out=ci[:], in_=carry[:]), [b_carry], [b_ci])
        dv(lambda e: e.tensor_scalar(out=ci[:], in0=ci[:], scalar1=7, scalar2=7, op0=ALU.arith_shift_right, op1=ALU.logical_shift_left), [b_ci], [b_ci])
        dv(lambda e: e.tensor_copy(out=pad[:], in_=ci[:]), [b_ci], [b_pad])
        ps, b_ps = self.next_ps()
        S.op("pe", lambda e, ps=ps: e.transpose(out=ps[0:NE, 0:P], in_=pad[:], identity=self.identf[:]), reads=[b_pad, self.b_identf], writes=[b_ps])
        S.op("act", lambda e, ps=ps: e.copy(out=padT[:], in_=ps[0:NE, 0:P]), reads=[b_ps], writes=[b_padT])
        ps, b_ps = self.next_ps()
        S.op("pe", lambda e, ps=ps: e.matmul(out=ps[:, 0:NE], lhsT=padT[:], rhs=ustr[0:NE, 0:NE], start=True, stop=True), reads=[b_padT, b_ustr], writes=[b_ps])
        dv(lambda e, ps=ps: e.tensor_copy(out=base[:], in_=ps[:, 0:NE]), [b_ps], [b_base])
        dv(lambda e: e.tensor_tensor(out=bend[:], in0=base[:], in1=pad[:], op=ALU.add), [b_base, b_pad], [b_bend])
        big3, b_big3 = self.sb(es, "r_big3", [P, max(NS, 2 * NT), NE])
        posf, b_posf = self.sb(es, "r_posf", [P, NT, 2])
        OHv = OH[:].rearrange("p t k e -> p (t k) e")
        dv(lambda e: e.tensor_tensor(out=big3[:, 0:2 * NT, :], in0=OHv, in1=base[:].unsqueeze(1).broadcast_to([P, 2 * NT, NE]), op=ALU.mult), [b_OH, b_base], [b_big3])
        dv(lambda e: e.tensor_reduce(out=posf[:].rearrange("p t k -> p (t k)"), in_=big3[:, 0:2 * NT, :], axis=AX.X, op=ALU.add), [b_big3], [b_posf])
        dv(lambda e: e.tensor_tensor(out=posf[:], in0=posf[:], in1=RANK[:], op=ALU.add), [b_posf, b_RANK], [b_posf])
        POSI, b_POSI = self.POSI, self.b_POSI
        dv(lambda e: e.tensor_scalar(out=posf[:], in0=posf[:], scalar1=0.0, scalar2=float(NS * P - 1), op0=ALU.max, op1=ALU.min), [b_posf], [b_posf])
        dv(lambda e: e.tensor_copy(out=POSI[:], in_=posf[:]), [b_posf], [b_POSI])
        esl, b_esl = self.sb(es, "r_esl", [P, NS])
        dv(lambda e: e.tensor_tensor(out=big3[:, 0:NS, :], in0=slotpos[:].unsqueeze(2).broadcast_to([P, NS, NE]), in1=bend[:].unsqueeze(1).broadcast_to([P, NS, NE]), op=ALU.is_ge),
           [b_slotpos, b_bend], [b_big3])
        dv(lambda e: e.tensor_reduce(out=esl[:], in_=big3[:, 0:NS, :], axis=AX.X, op=ALU.add), [b_big3], [b_esl])
        dv(lambda e: e.tensor_scalar(out=esl[:], in0=esl[:], scalar1=0.0, scalar2=float(NE - 1), op0=ALU.max, op1=ALU.min), [b_esl], [b_esl])
        dv(lambda e: e.tensor_scalar(out=esl[:], in0=esl[:], scalar1=float(P), scalar2=None, op0=ALU.mult), [b_esl], [b_esl])
        dv(lambda e: e.tensor_scalar(out=esl[:], in0=esl[:], scalar1=pidx[:, 0:1], scalar2=None, op0=ALU.add), [b_esl, b_pidx], [b_esl])
        IDXI, b_IDXI = self.IDXI, self.b_IDXI
        dv(lambda e: e.tensor_copy(out=IDXI[:], in_=esl[:]), [b_esl], [b_IDXI])
        hs2 = [self.sb(es, "r_hs%d" % i, [P, D]) for i in range(2)]
        for t in range(NT):
            hh, b_hh = hs2[t % 2]
            S.dma(hh[:], self.HM[t * P:(t + 1) * P, :], writes=[b_hh])
            for kk in range(2):
                off = IOA(ap=POSI[:, t, kk:kk + 1].bitcast(U32), axis=0)
                for hf in range(2):
                    S.dma_custom("pool", lambda eng, hh=hh, off=off, hf=hf: eng.indirect_dma_start(out=self.HS[hf], out_offset=off, in_=hh[:, hf * (D // 2):(hf + 1) * (D // 2)], in_offset=None),
                                 [b_hh, b_POSI], [])
    self.phase_end()
    with contextlib.ExitStack() as es:
        W1, b_W1 = self.sb(es, "s_W1", [P, KD * DFF], BF16)
        W3, b_W3 = self.sb(es, "s_W3", [P, KD * DFF], BF16)
        W2, b_W2 = self.sb(es, "s_W2", [P, FC * D], BF16)
        hs = [self.sb(es, "s_hs%d" % i, [P, D]) for i in range(2)]
        hT, b_hT = self.sb(es, "s_hT", [P, KD, P], BF16)
        s1, b_s1 = self.sb(es, "s_s1", [P, DFF])
        gg, b_gg = self.sb(es, "s_g", [P, DFF])
        gT, b_gT = self.sb(es, "s_gT", [P, FC, P], BF16)
        ysb = [self.sb(es, "s_y%d" % i, [P, D]) for i in range(2)]
        IDXI, b_IDXI = self.IDXI, self.b_IDXI
        PC1W, PC2W = CPP * DFF, FPP * D
        for s in range(NS):
            off = IOA(ap=IDXI[:, s:s + 1].bitcast(U32), axis=0)
            for pc in range(NPC1):
                S.dma_custom("pool", lambda eng, pc=pc, off=off: eng.indirect_dma_start(out=W1[:, pc * PC1W:(pc + 1) * PC1W], out_offset=None, in_=self.W1R[l][pc], in_offset=off), [b_IDXI], [b_W1])
                S.dma_custom("pool", lambda eng, pc=pc, off=off: eng.indirect_dma_start(out=W3[:, pc * PC1W:(pc + 1) * PC1W], out_offset=None, in_=self.W3R[l][pc], in_offset=off), [b_IDXI], [b_W3])
            for pc in range(NPC2):
                S.dma_custom("pool", lambda eng, pc=pc, off=off: eng.indirect_dma_start(out=W2[:, pc * PC2W:(pc + 1) * PC2W], out_offset=None, in_=self.W2R[l][pc], in_offset=off), [b_IDXI], [b_W2])
            x, b_x = hs[s % 2]
            for hf in range(2):
                S.dma(x[:, hf * (D // 2):(hf + 1) * (D // 2)], self.HS[hf][s * P:(s + 1) * P, :], writes=[b_x])
            for c4 in range(0, KD, 4):
                ps, b_ps = self.next_ps()
                for c in range(c4, c4 + 4):
                    S.op("pe", lambda e, ps=ps, x=x, c=c, c4=c4: e.transpose(out=ps[:, (c - c4) * P:(c - c4 + 1) * P], in_=x[:, c * P:(c + 1) * P], identity=self.identf[:]),
                         reads=[b_x, self.b_identf], writes=[b_ps])
                if (c4 // 4) % 2 == 0:
                    S.op("act", lambda e, ps=ps, c4=c4: e.copy(out=hT[:, c4:c4 + 4, :], in_=ps[:].rearrange("p (c t) -> p c t", c=4)), reads=[b_ps], writes=[b_hT])
                else:
                    S.op("dve", lambda e, ps=ps, c4=c4: e.tensor_copy(out=hT[:, c4:c4 + 4, :], in_=ps[:].rearrange("p (c t) -> p c t", c=4)), reads=[b_ps], writes=[b_hT])
            p1, b_p1 = self.next_ps()
            p3, b_p3 = self.next_ps()
            for c in range(KD):
                S.op("pe", lambda e, p1=p1, c=c: e.matmul(out=p1[:, 0:DFF], lhsT=hT[:, c, :], rhs=W1[:, c * DFF:(c + 1) * DFF], start=(c == 0), stop=(c == KD - 1)), reads=[b_hT, b_W1], writes=[b_p1])
            for c in range(KD):
                S.op("pe", lambda e, p3=p3, c=c: e.matmul(out=p3[:, 0:DFF], lhsT=hT[:, c, :], rhs=W3[:, c * DFF:(c + 1) * DFF], start=(c == 0), stop=(c == KD - 1)), reads=[b_hT, b_W3], writes=[b_p3])
            S.op("act", lambda e, p1=p1: e.activation(out=s1[:], in_=p1[:, 0:DFF], func=AF.Silu), reads=[b_p1], writes=[b_s1])
            S.op("dve", lambda e, p3=p3: e.tensor_tensor(out=gg[:], in0=p3[:, 0:DFF], in1=s1[:], op=ALU.mult), reads=[b_p3, b_s1], writes=[b_gg])
            ps, b_ps = self.next_ps()
            for fc in range(FC):
                S.op("pe", lambda e, ps=ps, fc=fc: e.transpose(out=ps[:, fc * P:(fc + 1) * P], in_=gg[:, fc * P:(fc + 1) * P], identity=self.identf[:]), reads=[b_gg, self.b_identf], writes=[b_ps])
            S.op("act", lambda e, ps=ps: e.copy(out=gT[:], in_=ps[:, 0:FC * P].rearrange("p (c t) -> p c t", c=FC)), reads=[b_ps], writes=[b_gT])
            y, b_y = ysb[s % 2]
            for ni, n0 in enumerate(range(0, D, 512)):
                py, b_py = self.next_ps()
                for fc in range(FC):
                    S.op("pe", lambda e, py=py, fc=fc, n0=n0: e.matmul(out=py[:], lhsT=gT[:, fc, :], rhs=W2[:, fc * D + n0:fc * D + n0 + 512], start=(fc == 0), stop=(fc == FC - 1)),
                         reads=[b_gT, b_W2], writes=[b_py])
                if ni % 2 == 0:
                    S.op("act", lambda e, py=py, y=y, n0=n0: e.copy(out=y[:, n0:n0 + 512], in_=py[:]), reads=[b_py], writes=[b_y])
                else:
                    S.op("dve", lambda e, py=py, y=y, n0=n0: e.tensor_copy(out=y[:, n0:n0 + 512], in_=py[:]), reads=[b_py], writes=[b_y])
            for hf in range(2):
                S.dma(self.YS[hf][s * P:(s + 1) * P, :], y[:, hf * (D // 2):(hf + 1) * (D // 2)], reads=[b_y])
    self.phase_end()
    with contextlib.ExitStack() as es:
        ya = [self.sb(es, "u_a%d" % i, [P, D]) for i in range(2)]
        yb = [self.sb(es, "u_b%d" % i, [P, D]) for i in range(2)]
        POSI, b_POSI = self.POSI, self.b_POSI
        G12, b_G12 = self.G12, self.b_G12
        for t in range(NT):
            a, b_a = ya[t % 2]
            b2, b_b2 = yb[t % 2]
            offa = IOA(ap=POSI[:, t, 0:1].bitcast(U32), axis=0)
            offb = IOA(ap=POSI[:, t, 1:2].bitcast(U32), axis=0)
            for hf in range(2):
                S.dma_custom("pool", lambda eng, a=a, offa=offa, hf=hf: eng.indirect_dma_start(out=a[:, hf * (D // 2):(hf + 1) * (D // 2)], out_offset=None, in_=self.YS[hf], in_offset=offa), [b_POSI], [b_a])
                S.dma_custom("pool", lambda eng, b2=b2, offb=offb, hf=hf: eng.indirect_dma_start(out=b2[:, hf * (D // 2):(hf + 1) * (D // 2)], out_offset=None, in_=self.YS[hf], in_offset=offb), [b_POSI], [b_b2])
            S.op("dve", lambda e, a=a, t=t: e.tensor_scalar(out=a[:], in0=a[:], scalar1=G12[:, t, 0:1], scalar2=None, op0=ALU.mult), reads=[b_a, b_G12], writes=[b_a])
            S.op("dve", lambda e, a=a, b2=b2, t=t: e.scalar_tensor_tensor(out=a[:], in0=b2[:], scalar=G12[:, t, 1:2], in1=a[:], op0=ALU.mult, op1=ALU.add), reads=[b_a, b_b2, b_G12], writes=[b_a])
            S.dma(self.Y1[t * P:(t + 1) * P, :], a[:], reads=[b_a])
    self.phase_end()
    self.ln_pass(l, 2, self.Y1, make_h=False)


K.alloc_moe = _alloc_moe
K.cast_moe = _cast_moe
K.p6_moe_routed = _p6_moe_routed
```

```python
import numpy as np
import ml_dtypes
import concourse.bass as bass
import concourse.mybir as mybir
from concourse.bass_utils import run_bass_kernel_spmd

F32 = mybir.dt.float32
BF16 = mybir.dt.bfloat16
I32 = mybir.dt.int32
AF = mybir.ActivationFunctionType
ALU = mybir.AluOpType
AX = mybir.AxisListType

P = 128


class Buf:
    __slots__ = ("name", "w", "r")

    def __init__(self, name):
        self.name = name
        self.w = None
        self.r = {}


class Sched:
    COMPUTE = ("pe", "act", "dve", "pool")
    LIMIT = 24000

    def __init__(self, nc, n_dma=16):
        self.nc = nc
        self.eng = {"pe": nc.tensor, "act": nc.scalar, "dve": nc.vector, "pool": nc.gpsimd, "sp": nc.sync}
        self.prog = {e: [] for e in self.eng}
        self.n_dma = n_dma
        self.dma_rr = 0
        self.bufs = []
        self.epoch = 0
        self._new_epoch()

    def _new_epoch(self):
        self.sem = {}
        self.cnt = {}
        for p in list(self.COMPUTE) + ["d%d" % i for i in range(self.n_dma)]:
            self.sem[p] = self.nc.alloc_semaphore(name="s_%s_%d" % (p, self.epoch))
            self.cnt[p] = 0
        self.waited = {}
        for b in self.bufs:
            b.w = None
            b.r = {}
        self.epoch += 1

    def buf(self, name):
        b = Buf(name)
        self.bufs.append(b)
        return b

    def barrier(self):
        for e in self.eng:
            for p, c in self.cnt.items():
                if c > 0 and self.waited.get((e, p), 0) < c:
                    self.prog[e].append(("wait", self.sem[p], c))
                    self.waited[(e, p)] = c

    def _maybe_epoch(self):
        pass

    def _deps(self, e, reads, writes):
        need = {}
        for b in reads:
            if b.w is not None:
                need[b.w[0]] = max(need.get(b.w[0], 0), b.w[1])
        for b in writes:
            if b.w is not None:
                need[b.w[0]] = max(need.get(b.w[0], 0), b.w[1])
            for p, c in b.r.items():
                need[p] = max(need.get(p, 0), c)
        for p, c in need.items():
            if p == e == "pe":
                continue
            if self.waited.get((e, p), 0) < c:
                self.prog[e].append(("wait", self.sem[p], c))
                self.waited[(e, p)] = c

    def op(self, e, fn, reads=(), writes=()):
        self._maybe_epoch()
        self._deps(e, reads, writes)
        self.cnt[e] += 1
        c = self.cnt[e]
        self.prog[e].append(("ins", fn, self.sem[e], 1))
        for b in reads:
            b.r[e] = c
        for b in writes:
            b.w = (e, c)
            b.r = {}

    def dma(self, out, in_, reads=(), writes=(), q="sp", **kw):
        self._maybe_epoch()
        if q == "sp" and len(reads) > 0 and len(writes) == 0:
            q = "act"
            self.st_rr = (getattr(self, "st_rr", 0) + 1) % 8
            d = "d%d" % (8 + self.st_rr)
        else:
            d = "d%d" % self.dma_rr
            self.dma_rr = (self.dma_rr + 1) % 8
        if self.cnt[d] > 0 and self.waited.get((q, d), 0) < self.cnt[d]:
            self.prog[q].append(("wait", self.sem[d], self.cnt[d]))
            self.waited[(q, d)] = self.cnt[d]
        self._deps(q, reads, writes)
        self.cnt[d] += 16
        c = self.cnt[d]
        self.prog[q].append(("ins", lambda eng: eng.dma_start(out=out, in_=in_, **kw), self.sem[d], 16))
        for b in reads:
            b.r[d] = c
        for b in writes:
            b.w = (d, c)
            b.r = {}

    def finish(self):
        for e in self.eng:
            for p, c in self.cnt.items():
                if c > 0:
                    self.prog[e].append(("wait", self.sem[p], c))

    def emit(self, block):
        def mk(e):
            def body(eng):
                for it in self.prog[e]:
                    if it[0] == "wait":
                        eng.wait_ge(it[1], it[2])
                    else:
                        it[1](eng).then_inc(it[2], it[3])
            return body
        block.tensor(mk("pe"))
        block.scalar(mk("act"))
        block.vector(mk("dve"))
        block.gpsimd(mk("pool"))
        block.sync(mk("sp"))


D = 4096
KD = D // P
HD = 128
GH = 12
NIN = 9776
ALPHA = 4 ** 0.25
CH = 64
NE = 32
NG = 4
EPG = 8


class Cfg:
    def __init__(self, T=8192, NCTX=256, DFF=512, L=2):
        self.T, self.NCTX, self.DFF, self.L = T, NCTX, DFF, L
        self.S = T + NCTX
        self.NT = self.S // P
        self.NTC = NCTX // P


class K:
    def __init__(self, cfg):
        self.cfg = cfg
        self.nc = bass.Bass("TRN2", target_bir_lowering=False)
        self.S = Sched(self.nc)
        self.uid = 0
        self.ins = {}

    def inp(self, name, shape, dt=F32):
        t = self.nc.dram_tensor(name, list(shape), dt, kind="ExternalInput").ap()
        self.ins[name] = t
        return t

    def scratch(self, name, shape, dt=F32):
        return self.nc.dram_tensor(name, list(shape), dt, kind="Internal").ap(), self.S.buf(name)

    def sb(self, es, name, shape, dt=F32):
        self.uid += 1
        t = es.enter_context(self.nc.sbuf_tensor("%s_%d" % (name, self.uid), list(shape), dt))
        return t, self.S.buf(name)

    def ps(self, es, name, shape, dt=F32):
        self.uid += 1
        t = es.enter_context(self.nc.psum_tensor("%s_%d" % (name, self.uid), list(shape), dt))
        return t, self.S.buf(name)

    def phase_end(self):
        self.S.barrier()

    def bcast_tile(self, es, name, rows_dram, r, n, add=0.0, dt=F32):
        S = self.S
        out, b_out = self.sb(es, name, [P, n], dt)
        for j in range(0, n, 512):
            w = min(512, n - j)
            rows, b_rows = self.rowstg[self.rs_i % 2]
            self.rs_i += 1
            S.dma(rows[:, 0:w], rows_dram[:, j:j + w], writes=[b_rows])
            ps, b_ps = self.next_ps()
            S.op("pe", lambda e, ps=ps, rows=rows, w=w: e.matmul(out=ps[:, 0:w], lhsT=self.sel[:, r * P:(r + 1) * P],
                                                               rhs=rows[:, 0:w], start=True, stop=True),
                 reads=[b_rows, self.b_sel], writes=[b_ps])
            S.op("act", lambda e, ps=ps, j=j, w=w: e.activation(out=out[:, j:j + w], in_=ps[:, 0:w], func=AF.Identity,
                                                              bias=float(add), scale=1.0),
                 reads=[b_ps], writes=[b_out])
        return out, b_out

    def modcols(self, es, name, rows_dram, add=0.0):
        S = self.S
        out, b_out = self.sb(es, name, [P, KD, 2])
        for j in range(0, D, 512):
            rows, b_rows = self.rowstg[self.rs_i % 2]
            self.rs_i += 1
            S.dma(rows[:, :], rows_dram[:, j:j + 512], writes=[b_rows])
            ps, b_ps = self.next_ps()
            for c in range(4):
                S.op("pe", lambda e, ps=ps, rows=rows, c=c: e.transpose(out=ps[:, 2 * c:2 * c + 2], in_=rows[:, c * P:(c + 1) * P],
                                                                     identity=self.identf[0:2, 0:2]),
                     reads=[b_rows, self.b_identf], writes=[b_ps])
            c0 = j // P
            S.op("act", lambda e, ps=ps, c0=c0: e.activation(out=out[:, c0:c0 + 4, :], in_=ps[:, 0:8].rearrange("p (c r) -> p c r", r=2),
                                                            func=AF.Identity, bias=float(add), scale=1.0),
                 reads=[b_ps], writes=[b_out])
        return out, b_out

    def consts(self, es):
        S = self.S
        self.identf, self.b_identf = self.sb(es, "identf", [P, P])
        self.identb, self.b_identb = self.sb(es, "identb", [P, P], BF16)
        self.sel, self.b_sel = self.sb(es, "sel", [2, 2 * P])
        S.dma(self.identf[:], self.ins["identf"], writes=[self.b_identf])
        S.dma(self.sel[:], self.ins["sel"], writes=[self.b_sel])
        S.op("dve", lambda e: e.tensor_copy(out=self.identb[:], in_=self.identf[:]), reads=[self.b_identf],
             writes=[self.b_identb])
        _fc, _n1, _cpp, _n2, _fpp, _ns = _moe_geom(self.cfg)
        self.G12, self.b_G12 = self.sb(es, "G12", [P, self.cfg.NT, 2])
        self.POSI, self.b_POSI = self.sb(es, "POSI", [P, self.cfg.NT, 2], I32)
        self.IDXI, self.b_IDXI = self.sb(es, "IDXI", [P, _ns], I32)
        self.rowstg = [self.sb(es, "rowstg%d" % i, [2, 512]) for i in range(2)]
        self.rs_i = 0
        self.tpall, _ = self.ps(es, "tpall", [P, 8 * 512])
        self.tp = [(self.tpall[:, i * 512:(i + 1) * 512], self.S.buf("tp%d" % i)) for i in range(8)]
        self.tp_i = 0

    def next_ps(self, lo=0, hi=8):
        p = lo + self.tp_i % (hi - lo)
        self.tp_i += 1
        return self.tp[p]

    def cast_w(self, src, dst, rows, cols):
        import contextlib
        S = self.S
        with contextlib.ExitStack() as es:
            CW = 2048
            st = [self.sb(es, "cw_f%d" % i, [P, CW]) for i in range(3)]
            sbf = [self.sb(es, "cw_b%d" % i, [P, CW], BF16) for i in range(3)]
            i = 0
            engs = ["dve", "pool", "act"]
            for r in range(0, rows, P):
                for c in range(0, cols, CW):
                    w = min(CW, cols - c)
                    s = i % 3
                    f, bf_ = st[s]
                    o, bo = sbf[s]
                    S.dma(f[:, 0:w], src[r:r + P, c:c + w], writes=[bf_])
                    en = engs[i % 3]
                    if en == "act":
                        S.op("act", lambda e, f=f, o=o, w=w: e.copy(out=o[:, 0:w], in_=f[:, 0:w]), reads=[bf_], writes=[bo])
                    else:
                        S.op(en, lambda e, f=f, o=o, w=w: e.tensor_copy(out=o[:, 0:w], in_=f[:, 0:w]), reads=[bf_],
                             writes=[bo])
                    S.dma(dst[r:r + P, c:c + w], o[:, 0:w], reads=[bo])
                    i += 1
        self.phase_end()

    def p0_mods(self):
        import contextlib
        S, cfg = self.S, self.cfg
        cc, w_mod, bm = self.ins["cc"], self.ins["w_mod"], self.ins["bm"]
        self.MODROW, _ = self.scratch("modrow", [cfg.L, 2, 6 * D])
        with contextlib.ExitStack() as es:
            sc, b_sc = self.sb(es, "sc", [P, KD, 2])
            S.dma(sc[:], cc, writes=[b_sc])
            S.op("act", lambda e: e.activation(out=sc[:], in_=sc[:], func=AF.Silu), reads=[b_sc], writes=[b_sc])
            NB = 512
            wm = [self.sb(es, "wm%d" % i, [P, KD, NB]) for i in range(2)]
            bmt = [self.sb(es, "bmt%d" % i, [2, NB]) for i in range(2)]
            mr = [self.sb(es, "mr%d" % i, [2, NB]) for i in range(2)]
            it = 0
            for l in range(cfg.L):
                for n in range(0, 6 * D, NB):
                    s = it % 2
                    it += 1
                    w, b_w = wm[s]
                    bt, b_bt = bmt[s]
                    m, b_m = mr[s]
                    S.dma(w[:], w_mod[l].rearrange("(c p) n -> p c n", p=P)[:, :, n:n + NB], writes=[b_w])
                    S.dma(bt[:], bm[l, :, n:n + NB], writes=[b_bt])
                    ps, b_ps = self.next_ps()
                    for c in range(KD):
                        S.op("pe", lambda e, ps=ps, w=w, c=c: e.matmul(out=ps[0:2, :], lhsT=sc[:, c, :], rhs=w[:, c, :],
                                                                      start=(c == 0), stop=(c == KD - 1)),
                             reads=[b_sc, b_w], writes=[b_ps])
                    S.op("dve", lambda e, ps=ps, m=m, bt=bt: e.tensor_tensor(out=m[:], in0=ps[0:2, :], in1=bt[:], op=ALU.add),
                         reads=[b_ps, b_bt], writes=[b_m])
                    S.dma(self.MODROW[l, :, n:n + NB], m[:], reads=[b_m])
        self.phase_end()

    def gemm(self, src_rows, Kdim, wb, blocks, mod=None, tiles=None):
        import contextlib
        S, cfg = self.S, self.cfg
        KC = Kdim // P
        tiles = list(range(cfg.NT)) if tiles is None else tiles
        wview = wb.rearrange("(c p) n -> p c n", p=P)
        with contextlib.ExitStack() as es:
            xt = [self.sb(es, "g_xt%d" % i, [P, Kdim]) for i in range(2)]
            aT = [self.sb(es, "g_aT%d" % i, [P, KC, 512], BF16) for i in range(2)]
            wt = [self.sb(es, "g_w%d" % i, [P, KC, 512], BF16) for i in range(2)]
            xi = 0
            wi = 0
            for bi, t0 in enumerate(range(0, len(tiles), 4)):
                tb = tiles[t0:t0 + 4]
                a, b_a = aT[bi % 2]
                for j, t in enumerate(tb):
                    x, b_x = xt[xi % 2]
                    xi += 1
                    S.dma(x[:], src_rows(t), writes=[b_x])
                    for c4 in range(0, KC, 4):
                        ps, b_ps = self.next_ps()
                        for c in range(c4, c4 + 4):
                            S.op("pe", lambda e, ps=ps, x=x, c=c, c4=c4: e.transpose(
                                out=ps[:, (c - c4) * P:(c - c4 + 1) * P], in_=x[:, c * P:(c + 1) * P], identity=self.identf[:]),
                                reads=[b_x, self.b_identf], writes=[b_ps])
                        if mod is not None:
                            (msc, b_msc), (msh, b_msh) = mod
                            r = 1 if t < cfg.NTC else 0
                            for c in range(c4, c4 + 4):
                                S.op("act", lambda e, ps=ps, c=c, c4=c4, j=j, r=r, a=a: e.activation(
                                    out=a[:, c, j * P:(j + 1) * P], in_=ps[:, (c - c4) * P:(c - c4 + 1) * P], func=AF.Identity,
                                    scale=msc[:, c, r:r + 1], bias=msh[:, c, r:r + 1]), reads=[b_ps, b_msc, b_msh], writes=[b_a])
                        else:
                            eng = "act" if (c4 // 4) % 2 == 0 else "dve"
                            dst = a[:, c4:c4 + 4, j * P:(j + 1) * P]
                            src = ps[:].rearrange("p (c t) -> p c t", c=4)
                            if eng == "act":
                                S.op("act", lambda e, dst=dst, src=src: e.copy(out=dst, in_=src), reads=[b_ps], writes=[b_a])
                            else:
                                S.op("dve", lambda e, dst=dst, src=src: e.tensor_copy(out=dst, in_=src), reads=[b_ps], writes=[b_a])
                ntok = len(tb) * P
                for (c0, ncols, mode, sink) in blocks:
                    w, b_w = wt[wi % 2]
                    wi += 1
                    S.dma(w[:, :, 0:ncols], wview[:, :, c0:c0 + ncols], writes=[b_w])
                    if mode == "A":
                        for j, t in enumerate(tb):
                            ps, b_ps = self.next_ps()
                            for c in range(KC):
                                S.op("pe", lambda e, ps=ps, a=a, w=w, c=c, j=j, ncols=ncols: e.matmul(
                                    out=ps[:, 0:ncols], lhsT=a[:, c, j * P:(j + 1) * P], rhs=w[:, c, 0:ncols],
                                    start=(c == 0), stop=(c == KC - 1)), reads=[b_a, b_w], writes=[b_ps])
                            sink(ps, b_ps, t, c0, ncols)
                    else:
                        for cs in range(0, ncols, P):
                            ps, b_ps = self.next_ps()
                            for c in range(KC):
                                S.op("pe", lambda e, ps=ps, a=a, w=w, c=c, cs=cs, ntok=ntok: e.matmul(
                                    out=ps[:, 0:ntok], lhsT=w[:, c, cs:cs + P], rhs=a[:, c, 0:ntok],
                                    start=(c == 0), stop=(c == KC - 1)), reads=[b_a, b_w], writes=[b_ps])
                            sink(ps, b_ps, tb[0], ntok, c0 + cs)
        self.phase_end()

    def p1_proj(self, l):
        import contextlib
        S, cfg = self.S, self.cfg
        with contextlib.ExitStack() as es:
            msc = self.modcols(es, "m_s1", self.MODROW[l, :, D:2 * D], add=1.0)
            msh = self.modcols(es, "m_sh1", self.MODROW[l, :, 0:D])
            stg = [self.sb(es, "p1_stg%d" % i, [P, 512]) for i in range(3)]
            stgb = [self.sb(es, "p1_stgb%d" % i, [P, 512], BF16) for i in range(2)]
            cnt = [0]

            def sinkA(dst, off):
                def f(ps, b_ps, t, c0, ncols):
                    s, b_s = stg[cnt[0] % 3]
                    cnt[0] += 1
                    S.op("act", lambda e: e.copy(out=s[:, 0:ncols], in_=ps[:, 0:ncols]), reads=[b_ps], writes=[b_s])
                    S.dma(dst[t * P:(t + 1) * P, c0 - off:c0 - off + ncols], s[:, 0:ncols], reads=[b_s])
                return f

            def sinkB_f32(ps, b_ps, t0, ntok, col0):
                s, b_s = stg[cnt[0] % 3]
                cnt[0] += 1
                S.op("act", lambda e: e.copy(out=s[:, 0:ntok], in_=ps[:, 0:ntok]), reads=[b_ps], writes=[b_s])
                S.dma(self.QKVT[col0 // P, :, t0 * P:t0 * P + ntok], s[:, 0:ntok], reads=[b_s])

            def sinkB_bf(ps, b_ps, t0, ntok, col0):
                s, b_s = stgb[cnt[0] % 2]
                cnt[0] += 1
                S.op("dve", lambda e: e.tensor_copy(out=s[:, 0:ntok], in_=ps[:, 0:ntok]), reads=[b_ps], writes=[b_s])
                S.dma(self.FT[(col0 - 6192) // P, :, t0 * P:t0 * P + ntok], s[:, 0:ntok], reads=[b_s])

            blocks = []
            for c0 in range(0, 4608, 512):
                blocks.append((c0, 512, "B", sinkB_f32))
            for c0 in range(4608, 6144, 512):
                blocks.append((c0, 512, "A", sinkA(self.PZ, 4608)))
            blocks.append((6144, 48, "A", sinkA(self.PAB, 6144)))
            for c0 in range(6192, 7216, 512):
                blocks.append((c0, 512, "B", sinkB_bf))
            for c0 in range(7216, 8752, 512):
                blocks.append((c0, 512, "A", sinkA(self.PQ, 7216)))
            blocks.append((8752, 512, "A", sinkA(self.PK, 8752)))
            blocks.append((9264, 512, "A", sinkA(self.PV, 9264)))
            self.gemm(lambda t: self.XRES[t * P:(t + 1) * P, :], D, self.WINB[l], blocks, mod=(msc, msh))

    def alloc_scratch(self):
        cfg = self.cfg
        S_ = cfg.S
        self.XRES, _ = self.scratch("xres", [S_, D])
        self.WINB = [self.scratch("winb%d" % l, [D, NIN], BF16)[0] for l in range(cfg.L)]
        self.WOUTB = [self.scratch("woutb%d" % l, [D, D], BF16)[0] for l in range(cfg.L)]
        self.QKVT, _ = self.scratch("qkvt", [36, P, S_])
        self.FT, _ = self.scratch("ft", [8, P, S_], BF16)
        self.PZ, _ = self.scratch("pz", [S_, 1536])
        self.PAB, _ = self.scratch("pab", [S_, 48])
        self.PQ, _ = self.scratch("pq", [S_, 1536])
        self.PK, _ = self.scratch("pk", [S_, 512])
        self.PV, _ = self.scratch("pv", [S_, 512])
        self.VB, _ = self.scratch("vb", [S_, 512], BF16)
        self.QT, _ = self.scratch("qt", [12, P, S_], BF16)
        self.KT, _ = self.scratch("kt", [4, P, S_], BF16)
        self.YT, _ = self.scratch("yt", [8, P, S_], BF16)
        self.QNT, _ = self.scratch("qnt", [12, P, S_])
        self.KNT, _ = self.scratch("knt", [12, P, S_])
        self.KTOK, _ = self.scratch("ktok", [S_, 1536])
        self.VTOK, _ = self.scratch("vtok", [S_, 1536])
        self.GB, _ = self.scratch("gb", [S_, 48])
        self.OG = [self.scratch("og%d" % d, [S_, 1536])[0] for d in range(2)]
        self.MIX, _ = self.scratch("mix", [S_, D])
        self.Y1, _ = self.scratch("y1", [S_, D])
        self.HM, _ = self.scratch("hm", [S_, D])

    def load_x(self):
        import contextlib
        S, cfg = self.S, self.cfg
        with contextlib.ExitStack() as es:
            st = [self.sb(es, "lx%d" % i, [P, D]) for i in range(3)]
            for t in range(cfg.NT):
                s, b_s = st[t % 3]
                src = self.ins["ctx"][t * P:(t + 1) * P, :] if t < cfg.NTC else self.ins["x"][(t - cfg.NTC) * P:(t - cfg.NTC + 1) * P, :]
                S.dma(s[:], src, writes=[b_s])
                S.dma(self.XRES[t * P:(t + 1) * P, :], s[:], reads=[b_s])
        self.phase_end()

    def dump(self, name, src, shape, dt=F32):
        import contextlib
        S = self.S
        out = self.nc.dram_tensor(name, list(shape), dt, kind="ExternalOutput").ap()
        rows, cols = shape
        with contextlib.ExitStack() as es:
            st = [self.sb(es, "dmp%d" % i, [P, cols], dt) for i in range(2)]
            for i, r in enumerate(range(0, rows, P)):
                n = min(P, rows - r)
                s, b_s = st[i % 2]
                S.dma(s[0:n, :], src[r:r + n, :], writes=[b_s])
                S.dma(out[r:r + n, :], s[0:n, :], reads=[b_s])
        self.phase_end()

    def finish(self):
        self.S.finish()
        with self.nc.Block() as block:
            self.S.emit(block)

    def p4_attn(self, l, ctx_out=True):
        import contextlib
        S, cfg = self.S, self.cfg
        NT, NTC = cfg.NT, cfg.NTC
        with contextlib.ExitStack() as es:
            qw, b_qw = self.bcast_tile(es, "qw", self.ins["qknw"][l], 0, HD)
            kw, b_kw = self.bcast_tile(es, "kw", self.ins["qknw"][l], 1, HD)
            S.op("dve", lambda e: e.tensor_scalar(out=qw[:], in0=qw[:], scalar1=float(HD ** -0.5), scalar2=None, op0=ALU.mult),
                 reads=[b_qw], writes=[b_qw])
            bufs = {}
            for nm, H in (("q", 12), ("k", 4)):
                bufs[nm] = dict(
                    x=[self.sb(es, nm + "x%d" % i, [P, H, HD]) for i in range(2)],
                    sq=self.sb(es, nm + "sq", [P, H, HD]), ss=self.sb(es, nm + "ss", [P, H]),
                    xn=self.sb(es, nm + "xn", [P, H, HD]), xr=self.sb(es, nm + "xr", [P, H, HD]),
                    t1=self.sb(es, nm + "t1", [P, H, 64]), t2=self.sb(es, nm + "t2", [P, H, 64]),
                    xT=[self.sb(es, nm + "xT%d" % i, [P, H, P], BF16) for i in range(2)])
            vx = [self.sb(es, "vx%d" % i, [P, 512]) for i in range(2)]
            vb = [self.sb(es, "vb%d" % i, [P, 512], BF16) for i in range(2)]
            rp = [self.sb(es, "rp%d" % i, [P, HD]) for i in range(2)]
            for t in range(NT):
                lat = t >= NTC
                r, b_r = rp[t % 2]
                if lat:
                    S.dma(r[:], self.ins["rope"][(t - NTC) * P:(t - NTC + 1) * P, :], writes=[b_r])
                v, b_v = vx[t % 2]
                vbb, b_vb = vb[t % 2]
                S.dma(v[:], self.PV[t * P:(t + 1) * P, :], writes=[b_v])
                S.op("pool", lambda e, v=v, vbb=vbb: e.tensor_copy(out=vbb[:], in_=v[:]), reads=[b_v], writes=[b_vb])
                S.dma(self.VB[t * P:(t + 1) * P, :], vbb[:], reads=[b_vb])
                for nm, H, src, w, b_w, dstT in (("q", 12, self.PQ, qw, b_qw, self.QT), ("k", 4, self.PK, kw, b_kw, self.KT)):
                    B = bufs[nm]
                    x, b_x = B["x"][t % 2]
                    sq, b_sq = B["sq"]
                    ss, b_ss = B["ss"]
                    xn, b_xn = B["xn"]
                    xr, b_xr = B["xr"]
                    t1, b_t1 = B["t1"]
                    t2, b_t2 = B["t2"]
                    xT, b_xT = B["xT"][t % 2]
                    S.dma(x[:], src[t * P:(t + 1) * P, :].rearrange("p (h d) -> p h d", d=HD), writes=[b_x])
                    S.op("pool", lambda e, x=x, sq=sq: e.tensor_tensor(out=sq[:], in0=x[:], in1=x[:], op=ALU.mult), reads=[b_x], writes=[b_sq])
                    S.op("dve", lambda e, sq=sq, ss=ss: e.tensor_reduce(out=ss[:], in_=sq[:], axis=AX.X, op=ALU.add), reads=[b_sq], writes=[b_ss])
                    S.op("act", lambda e, ss=ss: e.activation(out=ss[:], in_=ss[:], func=AF.Sqrt, scale=1.0 / HD, bias=1e-6), reads=[b_ss], writes=[b_ss])
                    S.op("dve", lambda e, ss=ss: e.reciprocal(out=ss[:], in_=ss[:]), reads=[b_ss], writes=[b_ss])
                    S.op("dve", lambda e, x=x, ss=ss, xn=xn, H=H: e.tensor_tensor(out=xn[:], in0=x[:], in1=ss[:].unsqueeze(2).broadcast_to([P, H, HD]), op=ALU.mult),
                         reads=[b_x, b_ss], writes=[b_xn])
                    fin = xr if lat else xn
                    b_fin = b_xr if lat else b_xn
                    S.op("pool", lambda e, xn=xn, w=w, H=H: e.tensor_tensor(out=xn[:], in0=xn[:], in1=w[:].unsqueeze(1).broadcast_to([P, H, HD]), op=ALU.mult),
                         reads=[b_xn, b_w], writes=[b_xn])
                    if lat:
                        cosb = r[:, 0:64].unsqueeze(1).broadcast_to([P, H, 64])
                        sinb = r[:, 64:128].unsqueeze(1).broadcast_to([P, H, 64])
                        x1, x2 = xn[:, :, 0:64], xn[:, :, 64:128]
                        S.op("dve", lambda e, t1=t1, x1=x1, cosb=cosb: e.tensor_tensor(out=t1[:], in0=x1, in1=cosb, op=ALU.mult), reads=[b_xn, b_r], writes=[b_t1])
                        S.op("pool", lambda e, t2=t2, x2=x2, sinb=sinb: e.tensor_tensor(out=t2[:], in0=x2, in1=sinb, op=ALU.mult), reads=[b_xn, b_r], writes=[b_t2])
                        S.op("dve", lambda e, xr=xr, t1=t1, t2=t2: e.tensor_tensor(out=xr[:, :, 0:64], in0=t1[:], in1=t2[:], op=ALU.subtract), reads=[b_t1, b_t2], writes=[b_xr])
                        S.op("pool", lambda e, t1=t1, x2=x2, cosb=cosb: e.tensor_tensor(out=t1[:], in0=x2, in1=cosb, op=ALU.mult), reads=[b_xn, b_r, b_xr], writes=[b_t1])
                        S.op("dve", lambda e, t2=t2, x1=x1, sinb=sinb: e.tensor_tensor(out=t2[:], in0=x1, in1=sinb, op=ALU.mult), reads=[b_xn, b_r, b_xr], writes=[b_t2])
                        S.op("pool", lambda e, xr=xr, t1=t1, t2=t2: e.tensor_tensor(out=xr[:, :, 64:128], in0=t1[:], in1=t2[:], op=ALU.add), reads=[b_t1, b_t2], writes=[b_xr])
                    for h4 in range(0, H, 4):
                        ps, b_ps = self.next_ps()
                        for h in range(h4, h4 + 4):
                            S.op("pe", lambda e, ps=ps, fin=fin, h=h, h4=h4: e.transpose(out=ps[:, (h - h4) * P:(h - h4 + 1) * P], in_=fin[:, h, :], identity=self.identf[:]),
                                 reads=[b_fin, self.b_identf], writes=[b_ps])
                        S.op("act", lambda e, ps=ps, xT=xT, h4=h4: e.copy(out=xT[:, h4:h4 + 4, :], in_=ps[:].rearrange("p (h t) -> p h t", h=4)),
                             reads=[b_ps], writes=[b_xT])
                    S.dma(dstT[:, :, t * P:(t + 1) * P].rearrange("h p t -> p h t"), xT[:], reads=[b_xT])
        self.phase_end()
        with contextlib.ExitStack() as es:
            ones, b_ones = self.sb(es, "a_ones", [P, P], BF16)
            S.op("dve", lambda e: e.memset(ones[:], 1.0), writes=[b_ones])
            ktg = [self.sb(es, "ktg%d" % i, [P, cfg.S], BF16) for i in range(2)]
            vg = [self.sb(es, "vg%d" % i, [P, NT, HD], BF16) for i in range(2)]
            qt = [self.sb(es, "qt%d" % i, [P, 512], BF16) for i in range(2)]
            pT = [self.sb(es, "pT%d" % i, [P, 512], BF16) for i in range(3)]
            rinv = self.sb(es, "rinv", [P, 512])
            osb = [self.sb(es, "osb%d" % i, [P, 512]) for i in range(2)]
            ostg = [self.sb(es, "ostg%d" % i, [P, 4, HD]) for i in range(2)]
            qblocks = [(q0, min(512, cfg.S - q0), 0, NT) for q0 in range(cfg.NCTX, cfg.S, 512)]
            if ctx_out:
                qblocks += [(q0, min(512, cfg.NCTX - q0), 0, NTC) for q0 in range(0, cfg.NCTX, 512)]
            ui = 0
            for g in range(4):
                kt, b_kt = ktg[g % 2]
                vv, b_vv = vg[g % 2]
                S.dma(kt[:], self.KT[g], writes=[b_kt])
                for n0 in range(0, NT, 16):
                    n1 = min(NT, n0 + 16)
                    S.dma(vv[:, n0:n1, :], self.VB[n0 * P:n1 * P, g * HD:(g + 1) * HD].rearrange("(n p) d -> p n d", p=P), writes=[b_vv])
                for (q0, nq, s0, s1) in qblocks:
                    for hh in range(3):
                        h = g * 3 + hh
                        q, b_q = qt[ui % 2]
                        oT, b_oT = self.tp[4 + 2 * (ui % 2)]
                        rs, b_rs = self.tp[5 + 2 * (ui % 2)]
                        o, b_o = osb[ui % 2]
                        og, b_og = ostg[ui % 2]
                        ui += 1
                        S.dma(q[:, 0:nq], self.QT[h, :, q0:q0 + nq], writes=[b_q])
                        for s in range(s0, s1):
                            st, b_st = self.next_ps(0, 4)
                            pt, b_pt = pT[s % 3]
                            S.op("pe", lambda e, st=st, kt=kt, q=q, s=s, nq=nq: e.matmul(out=st[:, 0:nq], lhsT=kt[:, s * P:(s + 1) * P], rhs=q[:, 0:nq], start=True, stop=True),
                                 reads=[b_kt, b_q], writes=[b_st])
                            S.op("act", lambda e, st=st, pt=pt, nq=nq: e.activation(out=pt[:, 0:nq], in_=st[:, 0:nq], func=AF.Exp), reads=[b_st], writes=[b_pt])
                            S.op("pe", lambda e, oT=oT, vv=vv, pt=pt, s=s, nq=nq, s0=s0, s1=s1: e.matmul(out=oT[:, 0:nq], lhsT=vv[:, s, :], rhs=pt[:, 0:nq], start=(s == s0), stop=(s == s1 - 1)),
                                 reads=[b_vv, b_pt], writes=[b_oT])
                            S.op("pe", lambda e, rs=rs, pt=pt, nq=nq, s=s, s0=s0, s1=s1: e.matmul(out=rs[:, 0:nq], lhsT=ones[:], rhs=pt[:, 0:nq], start=(s == s0), stop=(s == s1 - 1)),
                                 reads=[b_ones, b_pt], writes=[b_rs])
                        ri, b_ri = rinv
                        S.op("dve", lambda e, ri=ri, rs=rs, nq=nq: e.reciprocal(out=ri[:, 0:nq], in_=rs[:, 0:nq]), reads=[b_rs], writes=[b_ri])
                        S.op("dve", lambda e, o=o, oT=oT, ri=ri, nq=nq: e.tensor_tensor(out=o[:, 0:nq], in0=oT[:, 0:nq], in1=ri[:, 0:nq], op=ALU.mult),
                             reads=[b_oT, b_ri], writes=[b_o])
                        ps, b_ps = self.next_ps(0, 4)
                        for j in range(nq // P):
                            S.op("pe", lambda e, ps=ps, o=o, j=j: e.transpose(out=ps[:, j * P:(j + 1) * P], in_=o[:, j * P:(j + 1) * P], identity=self.identf[:]),
                                 reads=[b_o, self.b_identf], writes=[b_ps])
                        nj = nq // P
                        S.op("act", lambda e, ps=ps, og=og, nj=nj: e.copy(out=og[:, 0:nj, :], in_=ps[:, 0:nj * P].rearrange("p (j d) -> p j d", d=HD)),
                             reads=[b_ps], writes=[b_og])
                        S.dma(self.MIX[q0:q0 + nq, 2560 + h * HD:2560 + (h + 1) * HD].rearrange("(j p) d -> p j d", p=P), og[:, 0:nj, :], reads=[b_og])
        self.phase_end()

    def p3_fnet(self, l, ctx_out=True):
        import contextlib
        S, cfg = self.S, self.cfg
        NT, NTC = cfg.NT, cfg.NTC
        segs = [(NTC, NT, self.ins["dft_c"], self.ins["dft_ns"], cfg.T)]
        if ctx_out:
            segs.append((0, NTC, self.ins["dftc_c"], self.ins["dftc_ns"], cfg.NCTX))
        with contextlib.ExitStack() as es:
            dch, b_dch = self.sb(es, "dch", [P, 256], BF16)
            S.dma(dch[:], self.ins["dft_ch"], writes=[b_dch])
            xcs, b_xcs = self.sb(es, "xcs", [P, NT, 2, 256], BF16)
            ftt = [self.sb(es, "ftt%d" % i, [P, 2, 512], BF16) for i in range(2)]
            tab = [self.sb(es, "tab%d" % i, [P, 2, 512], BF16) for i in range(3)]
            ysb = [self.sb(es, "ysb%d" % i, [P, 512], BF16) for i in range(2)]
            it = 0
            for gp in range(4):
                for t0 in range(0, NT, 4):
                    nt = min(4, NT - t0)
                    f, b_f = ftt[(t0 // 4) % 2]
                    S.dma(f[:, :, 0:nt * P], self.FT[2 * gp:2 * gp + 2, :, t0 * P:(t0 + nt) * P].rearrange("g p t -> p g t"), writes=[b_f])
                    for j in range(nt):
                        ps, b_ps = self.next_ps(0, 4)
                        for gi in range(2):
                            S.op("pe", lambda e, ps=ps, f=f, gi=gi, j=j: e.matmul(out=ps[:, gi * 256:(gi + 1) * 256], lhsT=f[:, gi, j * P:(j + 1) * P], rhs=dch[:],
                                                                                 start=True, stop=True), reads=[b_f, b_dch], writes=[b_ps])
                        S.op("act", lambda e, ps=ps, t=t0 + j: e.copy(out=xcs[:, t, :, :], in_=ps[:].rearrange("p (g c) -> p g c", g=2)),
                             reads=[b_ps], writes=[b_xcs])
                for (ta, tb_, dc, dns, n) in segs:
                    for k0 in range(0, n, 512):
                        nk = min(512, n - k0)
                        acc = [self.tp[4 + 2 * (it % 2)], self.tp[5 + 2 * (it % 2)]]
                        for ti, t in enumerate(range(ta, tb_)):
                            tb2, b_tab = tab[ti % 3]
                            S.dma(tb2[:, 0, 0:nk], dc[ti * P:(ti + 1) * P, k0:k0 + nk], writes=[b_tab])
                            S.dma(tb2[:, 1, 0:nk], dns[ti * P:(ti + 1) * P, k0:k0 + nk], writes=[b_tab])
                            for gi in range(2):
                                a_, b_a = acc[gi]
                                S.op("pe", lambda e, a_=a_, t=t, gi=gi, tb2=tb2, nk=nk, ti=ti: e.matmul(out=a_[:, 0:nk], lhsT=xcs[:, t, gi, 0:128], rhs=tb2[:, 0, 0:nk],
                                                                                                    start=(ti == 0), stop=False), reads=[b_xcs, b_tab], writes=[b_a])
                                S.op("pe", lambda e, a_=a_, t=t, gi=gi, tb2=tb2, nk=nk, last=(t == tb_ - 1): e.matmul(out=a_[:, 0:nk], lhsT=xcs[:, t, gi, 128:256], rhs=tb2[:, 1, 0:nk],
                                                                                                                  start=False, stop=last), reads=[b_xcs, b_tab], writes=[b_a])
                        for gi in range(2):
                            a_, b_a = acc[gi]
                            y, b_y = ysb[gi]
                            S.op("act" if gi == 0 else "dve", (lambda e, y=y, a_=a_, nk=nk: e.copy(out=y[:, 0:nk], in_=a_[:, 0:nk])) if gi == 0 else
                                 (lambda e, y=y, a_=a_, nk=nk: e.tensor_copy(out=y[:, 0:nk], in_=a_[:, 0:nk])), reads=[b_a], writes=[b_y])
                            S.dma(self.YT[2 * gp + gi, :, ta * P + k0:ta * P + k0 + nk], y[:, 0:nk], reads=[b_y])
                        it += 1
        self.phase_end()
        with contextlib.ExitStack() as es:
            fwf, b_fwf = self.sb(es, "fwf", [P, 8, 1024])
            fwb, b_fwb = self.sb(es, "fwb", [P, 8, 1024], BF16)
            S.dma(fwf[:], self.ins["fn_w"][l].rearrange("(g p) n -> p g n", p=P), writes=[b_fwf])
            S.op("dve", lambda e: e.tensor_copy(out=fwb[:], in_=fwf[:]), reads=[b_fwf], writes=[b_fwb])
            y8 = [self.sb(es, "y8_%d" % i, [P, 8, P], BF16) for i in range(2)]
            fo = [self.sb(es, "fo%d" % i, [P, 1024]) for i in range(2)]
            tiles = range(NT) if ctx_out else range(NTC, NT)
            for t in tiles:
                y, b_y = y8[t % 2]
                o, b_o = fo[t % 2]
                S.dma(y[:], self.YT[:, :, t * P:(t + 1) * P].rearrange("g p t -> p g t"), writes=[b_y])
                for half in range(2):
                    ps, b_ps = self.next_ps()
                    for g in range(8):
                        S.op("pe", lambda e, ps=ps, y=y, g=g, half=half: e.matmul(out=ps[:], lhsT=y[:, g, :], rhs=fwb[:, g, half * 512:(half + 1) * 512],
                                                                                 start=(g == 0), stop=(g == 7)), reads=[b_y, b_fwb], writes=[b_ps])
                    S.op("act", lambda e, ps=ps, o=o, half=half: e.copy(out=o[:, half * 512:(half + 1) * 512], in_=ps[:]), reads=[b_ps], writes=[b_o])
                S.dma(self.MIX[t * P:(t + 1) * P, 1536:2560], o[:], reads=[b_o])
        self.phase_end()

    def ln_pass(self, l, which, ysrc, make_h):
        import contextlib
        S, cfg = self.S, self.cfg
        gcol = 2 if which == 1 else 5
        lnrows = self.ins["lnp"][l, which - 1]
        for r, tiles in ((0, range(cfg.NTC, cfg.NT)), (1, range(0, cfg.NTC))):
            with contextlib.ExitStack() as es:
                gt, b_gt = self.bcast_tile(es, "ln_gate", self.MODROW[l, :, gcol * D:(gcol + 1) * D], r, D)
                lg, b_lg = self.bcast_tile(es, "ln_g", lnrows, 0, D)
                lb, b_lb = self.bcast_tile(es, "ln_b", lnrows, 1, D)
                if make_h:
                    s2, b_s2 = self.bcast_tile(es, "ln_s2", self.MODROW[l, :, 4 * D:5 * D], r, D, add=1.0)
                    sh2, b_sh2 = self.bcast_tile(es, "ln_sh2", self.MODROW[l, :, 3 * D:4 * D], r, D)
                xs = [self.sb(es, "ln_x%d" % i, [P, D]) for i in range(2)]
                ys = [self.sb(es, "ln_y%d" % i, [P, D]) for i in range(2)]
                st, b_st = self.sb(es, "ln_st", [P, 8, 6])
                mv, b_mv = self.sb(es, "ln_mv", [P, 2])
                rstd, b_rstd = self.sb(es, "ln_rstd", [P, 1])
                nmr, b_nmr = self.sb(es, "ln_nmr", [P, 1])
                for i, t in enumerate(tiles):
                    x, b_x = xs[i % 2]
                    y, b_y = ys[i % 2]
                    S.dma(x[:], self.XRES[t * P:(t + 1) * P, :], writes=[b_x])
                    S.dma(y[:], ysrc[t * P:(t + 1) * P, :], writes=[b_y])
                    S.op("pool", lambda e, y=y: e.tensor_tensor(out=y[:], in0=y[:], in1=gt[:], op=ALU.mult), reads=[b_y, b_gt], writes=[b_y])
                    S.op("dve", lambda e, x=x, y=y: e.scalar_tensor_tensor(out=x[:], in0=x[:], scalar=float(ALPHA), in1=y[:], op0=ALU.mult, op1=ALU.add),
                         reads=[b_x, b_y], writes=[b_x])
                    for c in range(8):
                        S.op("dve", lambda e, x=x, c=c: e.bn_stats(out=st[:, c, :], in_=x[:, c * 512:(c + 1) * 512]), reads=[b_x], writes=[b_st])
                    S.op("dve", lambda e: e.bn_aggr(out=mv[:], in_=st[:].rearrange("p c s -> p (c s)")), reads=[b_st], writes=[b_mv])
                    S.op("act", lambda e: e.activation(out=rstd[:], in_=mv[:, 1:2], func=AF.Sqrt, bias=1e-5, scale=1.0), reads=[b_mv], writes=[b_rstd])
                    S.op("dve", lambda e: e.reciprocal(out=rstd[:], in_=rstd[:]), reads=[b_rstd], writes=[b_rstd])
                    S.op("dve", lambda e: e.scalar_tensor_tensor(out=nmr[:], in0=mv[:, 0:1], scalar=-1.0, in1=rstd[:], op0=ALU.mult, op1=ALU.mult),
                         reads=[b_mv, b_rstd], writes=[b_nmr])
                    S.op("act", lambda e, x=x: e.activation(out=x[:], in_=x[:], func=AF.Identity, scale=rstd[:, 0:1], bias=nmr[:, 0:1]),
                         reads=[b_x, b_rstd, b_nmr], writes=[b_x])
                    S.op("pool", lambda e, x=x: e.tensor_tensor(out=x[:], in0=x[:], in1=lg[:], op=ALU.mult), reads=[b_x, b_lg], writes=[b_x])
                    S.op("dve", lambda e, x=x: e.tensor_tensor(out=x[:], in0=x[:], in1=lb[:], op=ALU.add), reads=[b_x, b_lb], writes=[b_x])
                    S.dma(self.XRES[t * P:(t + 1) * P, :], x[:], reads=[b_x])
                    if make_h:
                        S.op("pool", lambda e, x=x, y=y: e.tensor_tensor(out=y[:], in0=x[:], in1=s2[:], op=ALU.mult), reads=[b_x, b_s2], writes=[b_y])
                        S.op("dve", lambda e, y=y: e.tensor_tensor(out=y[:], in0=y[:], in1=sh2[:], op=ALU.add), reads=[b_y, b_sh2], writes=[b_y])
                        S.dma(self.HM[t * P:(t + 1) * P, :], y[:], reads=[b_y])
            self.phase_end()

    def p5_out(self, l):
        import contextlib
        S = self.S
        with contextlib.ExitStack() as es:
            stg = [self.sb(es, "p5_stg%d" % i, [P, 512]) for i in range(3)]
            cnt = [0]

            def sink(ps, b_ps, t, c0, ncols):
                s, b_s = stg[cnt[0] % 3]
                cnt[0] += 1
                S.op("act", lambda e: e.copy(out=s[:, 0:ncols], in_=ps[:, 0:ncols]), reads=[b_ps], writes=[b_s])
                S.dma(self.Y1[t * P:(t + 1) * P, c0:c0 + ncols], s[:, 0:ncols], reads=[b_s])
            blocks = [(c0, 512, "A", sink) for c0 in range(0, D, 512)]
            self.gemm(lambda t: self.MIX[t * P:(t + 1) * P, :], D, self.WOUTB[l], blocks)
        self.ln_pass(l, 1, self.Y1, make_h=True)

    def p2_gdn_prep(self, l):
        import contextlib
        S, cfg = self.S, self.cfg
        NT, NTC, T, NCTX, S_ = cfg.NT, cfg.NTC, cfg.T, cfg.NCTX, cfg.S
        with contextlib.ExitStack() as es:
            cw, b_cw = self.sb(es, "cw", [P, 36, 5])
            S.dma(cw[:], self.ins["convw"][l], writes=[b_cw])
            onesf, b_onesf = self.sb(es, "onesf", [P, P])
            S.op("dve", lambda e: e.memset(onesf[:], 1.0), writes=[b_onesf])
            xin = [self.sb(es, "xin%d" % i, [P, S_ + 8]) for i in range(2)]
            for x, b_x in xin:
                S.op("pool", lambda e, x=x: e.memset(x[:], 0.0), writes=[b_x])
            acc = [self.sb(es, "cacc%d" % i, [P, S_]) for i in range(2)]
            sq = [self.sb(es, "csq%d" % i, [P, 512]) for i in range(2)]
            rr = [self.sb(es, "crr%d" % i, [P, 512]) for i in range(2)]
            tk = [self.sb(es, "ctk%d" % i, [P, 4, P]) for i in range(2)]
            OC, OL = 2, NCTX + 6
            for c in range(36):
                x, b_x = xin[c % 2]
                a, b_a = acc[c % 2]
                S.dma(x[:, OC:OC + NCTX], self.QKVT[c, :, 0:NCTX], writes=[b_x])
                S.dma(x[:, OL:OL + T], self.QKVT[c, :, NCTX:S_], writes=[b_x])
                for (o0, n, a0) in ((OC, NCTX, 0), (OL, T, NCTX)):
                    S.op("dve", lambda e, x=x, a=a, c=c, o0=o0, n=n, a0=a0: e.tensor_scalar(out=a[:, a0:a0 + n], in0=x[:, o0 - 2:o0 - 2 + n], scalar1=cw[:, c, 0:1], scalar2=None, op0=ALU.mult),
                         reads=[b_x, b_cw], writes=[b_a])
                    for j in range(1, 5):
                        S.op("dve", lambda e, x=x, a=a, c=c, o0=o0, n=n, a0=a0, j=j: e.scalar_tensor_tensor(out=a[:, a0:a0 + n], in0=x[:, o0 - 2 + j:o0 - 2 + j + n], scalar=cw[:, c, j:j + 1],
                                                                                                         in1=a[:, a0:a0 + n], op0=ALU.mult, op1=ALU.add), reads=[b_x, b_cw, b_a], writes=[b_a])
                S.op("act", lambda e, a=a: e.activation(out=a[:], in_=a[:], func=AF.Silu), reads=[b_a], writes=[b_a])
                if c < 24:
                    isq = c < 12
                    for bi, t0 in enumerate(range(0, S_, 512)):
                        n = min(512, S_ - t0)
                        q2, b_q2 = sq[bi % 2]
                        r, b_r = rr[bi % 2]
                        S.op("pool", lambda e, a=a, q2=q2, t0=t0, n=n: e.tensor_tensor(out=q2[:, 0:n], in0=a[:, t0:t0 + n], in1=a[:, t0:t0 + n], op=ALU.mult), reads=[b_a], writes=[b_q2])
                        ps, b_ps = self.next_ps()
                        S.op("pe", lambda e, ps=ps, q2=q2, n=n: e.matmul(out=ps[:, 0:n], lhsT=onesf[:], rhs=q2[:, 0:n], start=True, stop=True), reads=[b_onesf, b_q2], writes=[b_ps])
                        sc_, bi_ = (float(HD), float(HD) * 1e-6) if isq else (1.0, 1e-6)
                        S.op("act", lambda e, ps=ps, r=r, n=n, sc_=sc_, bi_=bi_: e.activation(out=r[:, 0:n], in_=ps[:, 0:n], func=AF.Sqrt, scale=sc_, bias=bi_), reads=[b_ps], writes=[b_r])
                        S.op("dve", lambda e, r=r, n=n: e.reciprocal(out=r[:, 0:n], in_=r[:, 0:n]), reads=[b_r], writes=[b_r])
                        S.op("dve", lambda e, a=a, r=r, t0=t0, n=n: e.tensor_tensor(out=a[:, t0:t0 + n], in0=a[:, t0:t0 + n], in1=r[:, 0:n], op=ALU.mult), reads=[b_a, b_r], writes=[b_a])
                    S.dma((self.QNT if isq else self.KNT)[c % 12], a[:], reads=[b_a])
                if c >= 12:
                    dst = self.KTOK if c < 24 else self.VTOK
                    h = c % 12
                    for t4 in range(0, NT, 4):
                        nt = min(4, NT - t4)
                        ps, b_ps = self.next_ps()
                        tt, b_tt = tk[(t4 // 4) % 2]
                        for j in range(nt):
                            S.op("pe", lambda e, ps=ps, a=a, j=j, t4=t4: e.transpose(out=ps[:, j * P:(j + 1) * P], in_=a[:, (t4 + j) * P:(t4 + j + 1) * P], identity=self.identf[:]),
                                 reads=[b_a, self.b_identf], writes=[b_ps])
                        S.op("act", lambda e, ps=ps, tt=tt, nt=nt: e.copy(out=tt[:, 0:nt, :], in_=ps[:, 0:nt * P].rearrange("p (j d) -> p j d", d=P)), reads=[b_ps], writes=[b_tt])
                        S.dma(dst[t4 * P:(t4 + nt) * P, h * HD:(h + 1) * HD].rearrange("(j p) d -> p j d", p=P), tt[:, 0:nt, :], reads=[b_tt])
            nea, b_nea = self.bcast_tile(es, "nea", self.ins["gdnp"][l], 0, 24)
            dtb, b_dtb = self.bcast_tile(es, "dtb", self.ins["gdnp"][l], 1, 24)
            S.op("act", lambda e: e.activation(out=nea[:], in_=nea[:], func=AF.Exp), reads=[b_nea], writes=[b_nea])
            S.op("dve", lambda e: e.tensor_scalar(out=nea[:], in0=nea[:], scalar1=-1.0, scalar2=None, op0=ALU.mult), reads=[b_nea], writes=[b_nea])
            ab, b_ab = self.sb(es, "ab", [P, NT, 48])
            for n0 in range(0, NT, 16):
                n1 = min(NT, n0 + 16)
                S.dma(ab[:, n0:n1, :], self.PAB[n0 * P:n1 * P, :].rearrange("(n p) c -> p n c", p=P), writes=[b_ab])
            av, bv = ab[:, :, 0:24], ab[:, :, 24:48]
            S.op("dve", lambda e: e.tensor_tensor(out=av, in0=av, in1=dtb[:].unsqueeze(1).broadcast_to([P, NT, 24]), op=ALU.add), reads=[b_ab, b_dtb], writes=[b_ab])
            S.op("act", lambda e: e.activation(out=av, in_=av, func=AF.Exp), reads=[b_ab], writes=[b_ab])
            S.op("act", lambda e: e.activation(out=av, in_=av, func=AF.Ln, bias=1.0, scale=1.0), reads=[b_ab], writes=[b_ab])
            S.op("dve", lambda e: e.tensor_tensor(out=av, in0=av, in1=nea[:].unsqueeze(1).broadcast_to([P, NT, 24]), op=ALU.mult), reads=[b_ab, b_nea], writes=[b_ab])
            S.op("act", lambda e: e.activation(out=bv, in_=bv, func=AF.Sigmoid), reads=[b_ab], writes=[b_ab])
            for n0 in range(0, NT, 16):
                n1 = min(NT, n0 + 16)
                S.dma(self.GB[n0 * P:n1 * P, :].rearrange("(n p) c -> p n c", p=P), ab[:, n0:n1, :], reads=[b_ab])
        self.phase_end()

    def p2_gdn_main(self, l):
        import contextlib
        S, cfg = self.S, self.cfg
        NCH = cfg.S // CH
        NCC = cfg.NCTX // CH
        order = [list(range(NCH)), list(range(NCC - 1, -1, -1)) + list(range(NCH - 1, NCC - 1, -1))]
        H = GH
        with contextlib.ExitStack() as es:
            msk = {}
            for d in range(2):
                for ki, nm in enumerate(("MU", "MLs", "Scs", "Ssc", "Isc")):
                    m, b_m = self.sb(es, "m_%s%d" % (nm, d), [CH, H, CH])
                    S.dma(m[:], self.ins["gmask"][d, ki], writes=[b_m])
                    msk[(nm, d)] = (m, b_m)
            i12, b_i12 = self.sb(es, "m_I", [CH, H, CH])
            S.dma(i12[:], self.ins["gmask_i"], writes=[b_i12])
            ones, b_ones = self.sb(es, "g_ones", [CH, P])
            S.op("dve", lambda e: e.memset(ones[:], 1.0), writes=[b_ones])
            Sst = [self.sb(es, "Sst%d" % d, [P, H, HD]) for d in range(2)]
            for st, b_st in Sst:
                S.op("pool", lambda e, st=st: e.memset(st[:], 0.0), writes=[b_st])
            B = []
            for d in range(2):
                nb = {}
                for nm, shp in (("kT", [P, H, CH]), ("qT", [P, H, CH]), ("ktok", [CH, H, HD]), ("vtok", [CH, H, HD]), ("gb", [CH, 48]),
                                ("eg", [CH, 24]), ("egl", [P, H]), ("Ug", [CH, H, CH]), ("Lg", [CH, H, CH]), ("E", [CH, H, CH]), ("ET", [CH, H, CH]),
                                ("decST", [CH, H, CH]), ("Rt", [CH, H, CH]), ("kdec", [CH, H, HD]), ("nwpT", [P, 4, CH]), ("vnew", [CH, 4, HD]),
                                ("osb", [CH, H, HD]), ("otmp", [CH, 4, HD])):
                    nb[nm] = self.sb(es, "g%d_%s" % (d, nm), shp)
                for al, tgt in (("decS", "E"), ("decIT", "ET"), ("Mm", "E"), ("MmT", "decST"), ("qkT", "ET"), ("diagb", "Ug"),
                                ("N0", "E"), ("P0", "decST"), ("N1", "Ug"), ("P1", "Lg"), ("kg", "ktok")):
                    nb[al] = nb[tgt]
                B.append(nb)
            A2 = [(self.tpall[:, 0:1024], [self.tp[0][1], self.tp[1][1]]), (self.tpall[:, 1024:2048], [self.tp[2][1], self.tp[3][1]])]
            a2i = [0]

            def big():
                r = A2[a2i[0] % 2]
                a2i[0] += 1
                return r

            def bc(ap, n):
                return ap.unsqueeze(2).broadcast_to([ap.shape[0], ap.shape[1], n])

            for step in range(NCH):
                for d in range(2):
                    c0 = order[d][step] * CH
                    b = B[d]
                    st, b_st = Sst[d]
                    kT, b_kT = b["kT"]; qT, b_qT = b["qT"]; ktok, b_ktok = b["ktok"]; vtok, b_vtok = b["vtok"]; gb, b_gb = b["gb"]
                    S.dma(kT[:], self.KNT[:, :, c0:c0 + CH].rearrange("h p t -> p h t"), writes=[b_kT])
                    S.dma(qT[:], self.QNT[:, :, c0:c0 + CH].rearrange("h p t -> p h t"), writes=[b_qT])
                    S.dma(ktok[:], self.KTOK[c0:c0 + CH, :].rearrange("p (h d) -> p h d", d=HD), writes=[b_ktok])
                    S.dma(vtok[:], self.VTOK[c0:c0 + CH, :].rearrange("p (h d) -> p h d", d=HD), writes=[b_vtok])
                    S.dma(gb[:], self.GB[c0:c0 + CH, :], writes=[b_gb])
                    g = gb[:, d * H:(d + 1) * H]
                    beta = gb[:, 24 + d * H:24 + (d + 1) * H]
                    MU, b_MU = msk[("MU", d)]; MLs, b_MLs = msk[("MLs", d)]
                    Scs, b_Scs = msk[("Scs", d)]; Ssc, b_Ssc = msk[("Ssc", d)]; Isc, b_Isc = msk[("Isc", d)]
                    ps, b_ps = self.next_ps(4, 8)
                    S.op("pe", lambda e, ps=ps, MU=MU, g=g: e.matmul(out=ps[0:CH, 0:H], lhsT=MU[:, 0, :], rhs=g, start=True, stop=True), reads=[b_MU, b_gb], writes=[b_ps])
                    S.op("pe", lambda e, ps=ps, MLs=MLs, g=g: e.matmul(out=ps[0:CH, H:2 * H], lhsT=MLs[:, 0, :], rhs=g, start=True, stop=True), reads=[b_MLs, b_gb], writes=[b_ps])
                    S.op("pe", lambda e, ps=ps, g=g: e.matmul(out=ps[:, 2 * H:3 * H], lhsT=ones[:], rhs=g, start=True, stop=True), reads=[b_ones, b_gb], writes=[b_ps])
                    eg, b_eg = b["eg"]; egl, b_egl = b["egl"]
                    S.op("act", lambda e, ps=ps, eg=eg: e.activation(out=eg[:], in_=ps[0:CH, 0:2 * H], func=AF.Exp), reads=[b_ps], writes=[b_eg])
                    S.op("act", lambda e, ps=ps, egl=egl: e.activation(out=egl[:], in_=ps[:, 2 * H:3 * H], func=AF.Exp), reads=[b_ps], writes=[b_egl])
                    egc, edec = eg[:, 0:H], eg[:, H:2 * H]
                    Ug, b_Ug = b["Ug"]; Lg, b_Lg = b["Lg"]
                    S.op("dve", lambda e, Ug=Ug, MU=MU, g=g: e.tensor_tensor(out=Ug[:], in0=MU[:], in1=bc(g, CH), op=ALU.mult), reads=[b_MU, b_gb], writes=[b_Ug])
                    S.op("pool", lambda e, Lg=Lg, MLs=MLs, g=g: e.tensor_tensor(out=Lg[:], in0=MLs[:], in1=bc(g, CH), op=ALU.mult), reads=[b_MLs, b_gb], writes=[b_Lg])
                    Dm, b_Dm = big()
                    DmT, b_DmT = big()
                    for h in range(H):
                        S.op("pe", lambda e, Dm=Dm, Ug=Ug, MLs=MLs, h=h: e.matmul(out=Dm[0:CH, h * CH:(h + 1) * CH], lhsT=Ug[:, h, :], rhs=MLs[:, 0, :], start=True, stop=True),
                             reads=[b_Ug, b_MLs], writes=b_Dm)
                    for h in range(H):
                        S.op("pe", lambda e, DmT=DmT, Lg=Lg, MU=MU, h=h: e.matmul(out=DmT[0:CH, h * CH:(h + 1) * CH], lhsT=Lg[:, h, :], rhs=MU[:, 0, :], start=True, stop=True),
                             reads=[b_Lg, b_MU], writes=b_DmT)
                    E, b_E = b["E"]; ET, b_ET = b["ET"]
                    fl = lambda t: t[:].rearrange("p h c -> p (h c)")
                    S.op("act", lambda e, E=E, Dm=Dm: e.activation(out=fl(E), in_=Dm[0:CH, 0:H * CH], func=AF.Exp), reads=b_Dm, writes=[b_E])
                    S.op("act", lambda e, ET=ET, DmT=DmT: e.activation(out=fl(ET), in_=DmT[0:CH, 0:H * CH], func=AF.Exp), reads=b_DmT, writes=[b_ET])
                    decS, b_decS = b["decS"]; decST, b_decST = b["decST"]; decIT, b_decIT = b["decIT"]
                    S.op("dve", lambda e, decS=decS, E=E, Scs=Scs: e.tensor_tensor(out=decS[:], in0=E[:], in1=Scs[:], op=ALU.mult), reads=[b_E, b_Scs], writes=[b_decS])
                    S.op("pool", lambda e, decST=decST, ET=ET, Ssc=Ssc: e.tensor_tensor(out=decST[:], in0=ET[:], in1=Ssc[:], op=ALU.mult), reads=[b_ET, b_Ssc], writes=[b_decST])
                    S.op("pool", lambda e, decIT=decIT, ET=ET, Isc=Isc: e.tensor_tensor(out=decIT[:], in0=ET[:], in1=Isc[:], op=ALU.mult), reads=[b_ET, b_Isc], writes=[b_decIT])
                    G, b_G = big()
                    QK, b_QK = big()
                    for h in range(H):
                        S.op("pe", lambda e, G=G, kT=kT, h=h: e.matmul(out=G[0:CH, h * CH:(h + 1) * CH], lhsT=kT[:, h, :], rhs=kT[:, h, :], start=True, stop=True), reads=[b_kT], writes=b_G)
                    for h in range(H):
                        S.op("pe", lambda e, QK=QK, kT=kT, qT=qT, h=h: e.matmul(out=QK[0:CH, h * CH:(h + 1) * CH], lhsT=kT[:, h, :], rhs=qT[:, h, :], start=True, stop=True),
                             reads=[b_kT, b_qT], writes=b_QK)
                    Mm, b_Mm = b["Mm"]; MmT, b_MmT = b["MmT"]; qkT, b_qkT = b["qkT"]
                    S.op("dve", lambda e, Mm=Mm, G=G, decS=decS: e.tensor_tensor(out=fl(Mm), in0=G[0:CH, 0:H * CH], in1=fl(decS), op=ALU.mult), reads=b_G + [b_decS], writes=[b_Mm])
                    S.op("dve", lambda e, MmT=MmT, G=G, decST=decST: e.tensor_tensor(out=fl(MmT), in0=G[0:CH, 0:H * CH], in1=fl(decST), op=ALU.mult), reads=b_G + [b_decST], writes=[b_MmT])
                    S.op("dve", lambda e, qkT=qkT, QK=QK, decIT=decIT: e.tensor_tensor(out=fl(qkT), in0=QK[0:CH, 0:H * CH], in1=fl(decIT), op=ALU.mult), reads=b_QK + [b_decIT], writes=[b_qkT])
                    diagb, b_diagb = b["diagb"]
                    S.op("pool", lambda e, diagb=diagb, beta=beta: e.tensor_tensor(out=diagb[:], in0=i12[:], in1=bc(beta, CH), op=ALU.mult), reads=[b_i12, b_gb], writes=[b_diagb])
                    Br, b_Br = big()
                    for h in range(H):
                        S.op("pe", lambda e, Br=Br, diagb=diagb, h=h: e.matmul(out=Br[0:CH, h * CH:(h + 1) * CH], lhsT=ones[:, 0:CH], rhs=diagb[:, h, :], start=True, stop=True),
                             reads=[b_ones, b_diagb], writes=b_Br)
                    Nn = [b["N0"], b["N1"]]
                    Pp = [b["P0"], b["P1"]]
                    Rt, b_Rt = b["Rt"]
                    S.op("dve", lambda e, N0=Nn[0][0], Mm=Mm, Br=Br: e.scalar_tensor_tensor(out=fl(N0), in0=fl(Mm), scalar=-1.0, in1=Br[0:CH, 0:H * CH], op0=ALU.mult, op1=ALU.mult),
                         reads=[b_Mm] + b_Br, writes=[Nn[0][1]])
                    S.op("dve", lambda e, P0=Pp[0][0], MmT=MmT, beta=beta: e.scalar_tensor_tensor(out=P0[:], in0=MmT[:], scalar=-1.0, in1=bc(beta, CH), op0=ALU.mult, op1=ALU.mult),
                         reads=[b_MmT, b_gb], writes=[Pp[0][1]])
                    S.op("pool", lambda e, Rt=Rt, P0=Pp[0][0]: e.tensor_tensor(out=Rt[:], in0=P0[:], in1=i12[:], op=ALU.add), reads=[Pp[0][1], b_i12], writes=[b_Rt])
                    for j in range(1, 6):
                        Np, b_Np = Nn[(j - 1) % 2]
                        Pq, b_Pq = Pp[(j - 1) % 2]
                        Nc, b_Nc = Nn[j % 2]
                        Pc, b_Pc = Pp[j % 2]
                        nps, b_nps = big()
                        for h in range(H):
                            S.op("pe", lambda e, nps=nps, Pq=Pq, Np=Np, h=h: e.matmul(out=nps[0:CH, h * CH:(h + 1) * CH], lhsT=Pq[:, h, :], rhs=Np[:, h, :], start=True, stop=True),
                                 reads=[b_Pq, b_Np], writes=b_nps)
                        if j < 5:
                            pps, b_pps = big()
                            for h in range(H):
                                S.op("pe", lambda e, pps=pps, Pq=Pq, Np=Np, h=h: e.matmul(out=pps[0:CH, h * CH:(h + 1) * CH], lhsT=Np[:, h, :], rhs=Pq[:, h, :], start=True, stop=True),
                                     reads=[b_Pq, b_Np], writes=b_pps)
                        S.op("act", lambda e, Nc=Nc, nps=nps: e.copy(out=fl(Nc), in_=nps[0:CH, 0:H * CH]), reads=b_nps, writes=[b_Nc])
                        if j < 5:
                            S.op("dve", lambda e, Pc=Pc, pps=pps: e.tensor_copy(out=fl(Pc), in_=pps[0:CH, 0:H * CH]), reads=b_pps, writes=[b_Pc])
                        rps, b_rps = big()
                        for h in range(H):
                            S.op("pe", lambda e, rps=rps, Nc=Nc, Rt=Rt, h=h: e.matmul(out=rps[0:CH, h * CH:(h + 1) * CH], lhsT=Nc[:, h, :], rhs=Rt[:, h, :], start=True, stop=True),
                                 reads=[b_Nc, b_Rt], writes=b_rps)
                        S.op("dve", lambda e, Rt=Rt, rps=rps: e.tensor_tensor(out=fl(Rt), in0=fl(Rt), in1=rps[0:CH, 0:H * CH], op=ALU.add), reads=[b_Rt] + b_rps, writes=[b_Rt])
                    kg, b_kg = b["kg"]; kdec, b_kdec = b["kdec"]; osb, b_osb = b["osb"]
                    S.op("pool", lambda e, kdec=kdec, ktok=ktok, edec=edec: e.tensor_tensor(out=kdec[:], in0=ktok[:], in1=bc(edec, HD), op=ALU.mult), reads=[b_ktok, b_eg], writes=[b_kdec])
                    S.op("dve", lambda e, kg=kg, ktok=ktok, egc=egc: e.tensor_tensor(out=kg[:], in0=ktok[:], in1=bc(egc, HD), op=ALU.mult), reads=[b_ktok, b_eg], writes=[b_kg])
                    nwpT, b_nwpT = b["nwpT"]; vnew, b_vnew = b["vnew"]; otmp, b_otmp = b["otmp"]
                    for hg in range(0, H, 4):
                        wps, b_wps = self.next_ps(4, 8)
                        for hh in range(4):
                            h = hg + hh
                            S.op("pe", lambda e, wps=wps, kg=kg, Rt=Rt, h=h, hh=hh: e.matmul(out=wps[:, hh * CH:(hh + 1) * CH], lhsT=kg[:, h, :], rhs=Rt[:, h, :], start=True, stop=True),
                                 reads=[b_kg, b_Rt], writes=[b_wps])
                        S.op("act", lambda e, nwpT=nwpT, wps=wps: e.activation(out=nwpT[:].rearrange("p h c -> p (h c)"), in_=wps[:, 0:4 * CH], func=AF.Copy, scale=-1.0), reads=[b_wps], writes=[b_nwpT])
                        vps, b_vps = self.next_ps(4, 8)
                        for hh in range(4):
                            h = hg + hh
                            S.op("pe", lambda e, vps=vps, Rt=Rt, vtok=vtok, h=h, hh=hh: e.matmul(out=vps[0:CH, hh * HD:(hh + 1) * HD], lhsT=Rt[:, h, :], rhs=vtok[:, h, :], start=True, stop=False),
                                 reads=[b_Rt, b_vtok], writes=[b_vps])
                            S.op("pe", lambda e, vps=vps, nwpT=nwpT, st=st, h=h, hh=hh: e.matmul(out=vps[0:CH, hh * HD:(hh + 1) * HD], lhsT=nwpT[:, hh, :], rhs=st[:, h, :], start=False, stop=True),
                                 reads=[b_nwpT, b_st], writes=[b_vps])
                        S.op("dve", lambda e, vnew=vnew, vps=vps, beta=beta, hg=hg: e.tensor_tensor(out=vnew[:], in0=vps[0:CH, :].rearrange("p (h d) -> p h d", d=HD), in1=bc(beta[:, hg:hg + 4], HD), op=ALU.mult),
                             reads=[b_vps, b_gb], writes=[b_vnew])
                        o1, b_o1 = self.next_ps(4, 8)
                        for hh in range(4):
                            h = hg + hh
                            S.op("pe", lambda e, o1=o1, qT=qT, st=st, h=h, hh=hh: e.matmul(out=o1[0:CH, hh * HD:(hh + 1) * HD], lhsT=qT[:, h, :], rhs=st[:, h, :], start=True, stop=True),
                                 reads=[b_qT, b_st], writes=[b_o1])
                        S.op("dve", lambda e, otmp=otmp, o1=o1, egc=egc, hg=hg: e.tensor_tensor(out=otmp[:], in0=o1[0:CH, :].rearrange("p (h d) -> p h d", d=HD), in1=bc(egc[:, hg:hg + 4], HD), op=ALU.mult),
                             reads=[b_o1, b_eg], writes=[b_otmp])
                        o2, b_o2 = self.next_ps(4, 8)
                        for hh in range(4):
                            h = hg + hh
                            S.op("pe", lambda e, o2=o2, qkT=qkT, vnew=vnew, h=h, hh=hh: e.matmul(out=o2[0:CH, hh * HD:(hh + 1) * HD], lhsT=qkT[:, h, :], rhs=vnew[:, hh, :], start=True, stop=True),
                                 reads=[b_qkT, b_vnew], writes=[b_o2])
                        S.op("dve", lambda e, osb=osb, otmp=otmp, o2=o2, hg=hg: e.tensor_tensor(out=osb[:, hg:hg + 4, :], in0=otmp[:], in1=o2[0:CH, :].rearrange("p (h d) -> p h d", d=HD), op=ALU.add),
                             reads=[b_otmp, b_o2], writes=[b_osb])
                        su, b_su = self.next_ps(4, 8)
                        for hh in range(4):
                            h = hg + hh
                            S.op("pe", lambda e, su=su, kdec=kdec, vnew=vnew, h=h, hh=hh: e.matmul(out=su[:, hh * HD:(hh + 1) * HD], lhsT=kdec[:, h, :], rhs=vnew[:, hh, :], start=True, stop=True),
                                 reads=[b_kdec, b_vnew], writes=[b_su])
                        S.op("pool", lambda e, st=st, egl=egl, hg=hg: e.tensor_tensor(out=st[:, hg:hg + 4, :], in0=st[:, hg:hg + 4, :], in1=bc(egl[:, hg:hg + 4], HD), op=ALU.mult),
                             reads=[b_st, b_egl], writes=[b_st])
                        S.op("dve", lambda e, st=st, su=su, hg=hg: e.tensor_tensor(out=st[:, hg:hg + 4, :], in0=st[:, hg:hg + 4, :], in1=su[:, :].rearrange("p (h d) -> p h d", d=HD), op=ALU.add),
                             reads=[b_st, b_su], writes=[b_st])
                    S.dma(self.OG[d][c0:c0 + CH, :].rearrange("p (h d) -> p h d", d=HD), osb[:], reads=[b_osb])
        self.phase_end()

    def p2_gdn_gate(self, l):
        import contextlib
        S, cfg = self.S, self.cfg
        with contextlib.ExitStack() as es:
            gw, b_gw = self.bcast_tile(es, "gnw", self.ins["gdnp2"][l], 0, HD)
            o0 = [self.sb(es, "go0_%d" % i, [P, GH, HD]) for i in range(2)]
            o1 = [self.sb(es, "go1_%d" % i, [P, GH, HD]) for i in range(2)]
            z = [self.sb(es, "gz_%d" % i, [P, GH, HD]) for i in range(2)]
            sq, b_sq = self.sb(es, "gsq", [P, GH, HD])
            ss, b_ss = self.sb(es, "gss", [P, GH])
            for t in range(cfg.NT):
                a, b_a = o0[t % 2]
                bb, b_bb = o1[t % 2]
                zz, b_zz = z[t % 2]
                S.dma(a[:], self.OG[0][t * P:(t + 1) * P, :].rearrange("p (h d) -> p h d", d=HD), writes=[b_a])
                S.dma(bb[:], self.OG[1][t * P:(t + 1) * P, :].rearrange("p (h d) -> p h d", d=HD), writes=[b_bb])
                S.dma(zz[:], self.PZ[t * P:(t + 1) * P, :].rearrange("p (h d) -> p h d", d=HD), writes=[b_zz])
                S.op("dve", lambda e, a=a, bb=bb: e.tensor_tensor(out=a[:], in0=a[:], in1=bb[:], op=ALU.add), reads=[b_a, b_bb], writes=[b_a])
                S.op("pool", lambda e, a=a: e.tensor_tensor(out=sq[:], in0=a[:], in1=a[:], op=ALU.mult), reads=[b_a], writes=[b_sq])
                S.op("dve", lambda e: e.tensor_reduce(out=ss[:], in_=sq[:], axis=AX.X, op=ALU.add), reads=[b_sq], writes=[b_ss])
                S.op("act", lambda e: e.activation(out=ss[:], in_=ss[:], func=AF.Sqrt, scale=1.0 / HD, bias=1e-6), reads=[b_ss], writes=[b_ss])
                S.op("dve", lambda e: e.reciprocal(out=ss[:], in_=ss[:]), reads=[b_ss], writes=[b_ss])
                S.op("act", lambda e, zz=zz: e.activation(out=zz[:], in_=zz[:], func=AF.Silu), reads=[b_zz], writes=[b_zz])
                S.op("dve", lambda e, a=a: e.tensor_tensor(out=a[:], in0=a[:], in1=ss[:].unsqueeze(2).broadcast_to([P, GH, HD]), op=ALU.mult), reads=[b_a, b_ss], writes=[b_a])
                S.op("pool", lambda e, a=a: e.tensor_tensor(out=a[:], in0=a[:], in1=gw[:].unsqueeze(1).broadcast_to([P, GH, HD]), op=ALU.mult), reads=[b_a, b_gw], writes=[b_a])
                S.op("dve", lambda e, a=a, zz=zz: e.tensor_tensor(out=a[:], in0=a[:], in1=zz[:], op=ALU.mult), reads=[b_a, b_zz], writes=[b_a])
                S.dma(self.MIX[t * P:(t + 1) * P, 0:1536].rearrange("p (h d) -> p h d", d=HD), a[:], reads=[b_a])
        self.phase_end()

    def p6_moe(self, l, dbg=''):
        import contextlib
        S, cfg = self.S, self.cfg
        DFF = cfg.DFF
        FC = DFF // P
        TB = 2
        w1v = self.W1B[l].rearrange("e (c p) f -> e p c f", p=P)
        w3v = self.W3B[l].rearrange("e (c p) f -> e p c f", p=P)
        w2v = self.W2B[l].rearrange("e (c p) n -> e p c n", p=P)
        with contextlib.ExitStack() as es:
            wr, b_wr = self.sb(es, "wr", [P, KD, 36])
            S.dma(wr[:], self.ins["wr"][l].rearrange("(c p) n -> p c n", p=P), writes=[b_wr])
            rb, b_rb = self.bcast_tile(es, "rb", self.ins["rb"][l], 0, 36)
            ht = [self.sb(es, "m_h%d" % i, [P, D]) for i in range(1)]
            hTf, b_hTf = self.sb(es, "m_hTf", [P, KD, P])
            hTb, b_hTb = self.sb(es, "m_hTb", [P, KD, TB * P], BF16)
            yacc, b_yacc = self.sb(es, "m_yacc", [P, TB, D])
            W1, b_W1 = self.sb(es, "m_W1", [P, KD, DFF], BF16)
            W3, b_W3 = self.sb(es, "m_W3", [P, KD, DFF], BF16)
            W2, b_W2 = self.sb(es, "m_W2", [P, FC, D], BF16)
            gates, b_gates = self.sb(es, "m_gates", [P, TB, NE])
            sm = {nm: self.sb(es, "m_" + nm, shp) for nm, shp in (("lg", [P, 36]), ("gmax", [P, 1]), ("ngmax", [P, 1]), ("ohg", [P, 4]), ("ex", [P, 4]), ("se", [P, 1]),
                                                                 ("les", [P, 8]), ("m1", [P, 1]), ("oh1", [P, 8]), ("le2", [P, 8]), ("m2", [P, 1]), ("oh2", [P, 8]),
                                                                 ("dd", [P, 1]), ("w1", [P, 1]), ("w2", [P, 1]), ("g8", [P, 8]))}
            s1 = [self.sb(es, "m_s1_%d" % i, [P, TB * P]) for i in range(2)]
            gT, b_gT = self.sb(es, "m_gT", [P, FC, TB * P], BF16)
            nblk = (cfg.NT + TB - 1) // TB
            for bi in range(nblk):
                tiles = list(range(bi * TB, min(cfg.NT, (bi + 1) * TB)))
                ntok = len(tiles) * P
                for j, t in enumerate(tiles):
                    h, b_h = ht[0]
                    S.dma(h[:], self.HM[t * P:(t + 1) * P, :], writes=[b_h])
                    for c4 in range(0, KD, 4):
                        ps, b_ps = self.next_ps()
                        for c in range(c4, c4 + 4):
                            S.op("pe", lambda e, ps=ps, h=h, c=c, c4=c4: e.transpose(out=ps[:, (c - c4) * P:(c - c4 + 1) * P], in_=h[:, c * P:(c + 1) * P], identity=self.identf[:]),
                                 reads=[b_h, self.b_identf], writes=[b_ps])
                        S.op("act", lambda e, ps=ps, c4=c4: e.copy(out=hTf[:, c4:c4 + 4, :], in_=ps[:].rearrange("p (c t) -> p c t", c=4)), reads=[b_ps], writes=[b_hTf])
                        S.op("dve", lambda e, c4=c4, j=j: e.tensor_copy(out=hTb[:, c4:c4 + 4, j * P:(j + 1) * P], in_=hTf[:, c4:c4 + 4, :]), reads=[b_hTf], writes=[b_hTb])
                    ps, b_ps = self.next_ps()
                    for c in range(KD if 'w' not in dbg else 0):
                        S.op("pe", lambda e, ps=ps, c=c: e.matmul(out=ps[:, 0:36], lhsT=hTf[:, c, :], rhs=wr[:, c, :], start=(c == 0), stop=(c == KD - 1)), reads=[b_hTf, b_wr], writes=[b_ps])
                    if 'r' in dbg:
                        continue
                    V = {k_: v_[0] for k_, v_ in sm.items()}
                    Bf = {k_: v_[1] for k_, v_ in sm.items()}

                    def dv(fn, r, w):
                        S.op("dve", fn, reads=[Bf[x] if isinstance(x, str) else x for x in r], writes=[Bf[x] if isinstance(x, str) else x for x in w])
                    dv(lambda e, ps=ps: e.tensor_tensor(out=V["lg"][:], in0=ps[:, 0:36], in1=rb[:], op=ALU.add), [b_ps, b_rb], ["lg"])
                    dv(lambda e: e.tensor_reduce(out=V["gmax"][:], in_=V["lg"][:, 0:4], axis=AX.X, op=ALU.max), ["lg"], ["gmax"])
                    dv(lambda e: e.tensor_scalar(out=V["ohg"][:], in0=V["lg"][:, 0:4], scalar1=V["gmax"][:, 0:1], scalar2=None, op0=ALU.is_ge), ["lg", "gmax"], ["ohg"])
                    dv(lambda e: e.tensor_scalar(out=V["ngmax"][:], in0=V["gmax"][:], scalar1=-1.0, scalar2=None, op0=ALU.mult), ["gmax"], ["ngmax"])
                    S.op("act", lambda e: e.activation(out=V["ex"][:], in_=V["lg"][:, 0:4], func=AF.Exp, bias=V["ngmax"][:, 0:1], scale=1.0), reads=[Bf["lg"], Bf["ngmax"]], writes=[Bf["ex"]])
                    dv(lambda e: e.tensor_reduce(out=V["se"][:], in_=V["ex"][:], axis=AX.X, op=ALU.add), ["ex"], ["se"])
                    dv(lambda e: e.reciprocal(out=V["se"][:], in_=V["se"][:]), ["se"], ["se"])
                    dv(lambda e: e.tensor_scalar(out=V["les"][:], in0=V["lg"][:, 4:12], scalar1=V["ohg"][:, 0:1], scalar2=None, op0=ALU.mult), ["lg", "ohg"], ["les"])
                    for g in range(1, 4):
                        dv(lambda e, g=g: e.scalar_tensor_tensor(out=V["les"][:], in0=V["lg"][:, 4 + 8 * g:12 + 8 * g], scalar=V["ohg"][:, g:g + 1], in1=V["les"][:], op0=ALU.mult, op1=ALU.add),
                           ["lg", "ohg", "les"], ["les"])
                    dv(lambda e: e.tensor_reduce(out=V["m1"][:], in_=V["les"][:], axis=AX.X, op=ALU.max), ["les"], ["m1"])
                    dv(lambda e: e.tensor_scalar(out=V["oh1"][:], in0=V["les"][:], scalar1=V["m1"][:, 0:1], scalar2=None, op0=ALU.is_ge), ["les", "m1"], ["oh1"])
                    dv(lambda e: e.scalar_tensor_tensor(out=V["le2"][:], in0=V["oh1"][:], scalar=-1e30, in1=V["les"][:], op0=ALU.mult, op1=ALU.add), ["oh1", "les"], ["le2"])
                    dv(lambda e: e.tensor_reduce(out=V["m2"][:], in_=V["le2"][:], axis=AX.X, op=ALU.max), ["le2"], ["m2"])
                    dv(lambda e: e.tensor_scalar(out=V["oh2"][:], in0=V["le2"][:], scalar1=V["m2"][:, 0:1], scalar2=None, op0=ALU.is_ge), ["le2", "m2"], ["oh2"])
                    dv(lambda e: e.tensor_tensor(out=V["dd"][:], in0=V["m2"][:], in1=V["m1"][:], op=ALU.subtract), ["m1", "m2"], ["dd"])
                    S.op("act", lambda e: e.activation(out=V["dd"][:], in_=V["dd"][:], func=AF.Exp), reads=[Bf["dd"]], writes=[Bf["dd"]])
                    dv(lambda e: e.tensor_scalar(out=V["w1"][:], in0=V["dd"][:], scalar1=1.0, scalar2=None, op0=ALU.add), ["dd"], ["w1"])
                    dv(lambda e: e.reciprocal(out=V["w1"][:], in_=V["w1"][:]), ["w1"], ["w1"])
                    dv(lambda e: e.tensor_tensor(out=V["w2"][:], in0=V["dd"][:], in1=V["w1"][:], op=ALU.mult), ["dd", "w1"], ["w2"])
                    dv(lambda e: e.tensor_tensor(out=V["w1"][:], in0=V["w1"][:], in1=V["se"][:], op=ALU.mult), ["w1", "se"], ["w1"])
                    dv(lambda e: e.tensor_tensor(out=V["w2"][:], in0=V["w2"][:], in1=V["se"][:], op=ALU.mult), ["w2", "se"], ["w2"])
                    dv(lambda e: e.tensor_scalar(out=V["g8"][:], in0=V["oh1"][:], scalar1=V["w1"][:, 0:1], scalar2=None, op0=ALU.mult), ["oh1", "w1"], ["g8"])
                    dv(lambda e: e.scalar_tensor_tensor(out=V["g8"][:], in0=V["oh2"][:], scalar=V["w2"][:, 0:1], in1=V["g8"][:], op0=ALU.mult, op1=ALU.add), ["oh2", "w2", "g8"], ["g8"])
                    for g in range(4):
                        dv(lambda e, g=g, j=j: e.tensor_scalar(out=gates[:, j, 8 * g:8 * g + 8], in0=V["g8"][:], scalar1=V["ohg"][:, g:g + 1], scalar2=None, op0=ALU.mult), ["g8", "ohg"], [b_gates])
                for ex in range(0 if 'x' not in dbg else NE, NE):
                    S.dma(W1[:], w1v[ex], writes=[b_W1])
                    S.dma(W3[:], w3v[ex], writes=[b_W3])
                    S.dma(W2[:], w2v[ex], writes=[b_W2])
                    for fc in range(FC):
                        p1, b_p1 = self.next_ps()
                        p3, b_p3 = self.next_ps()
                        for c in range(KD):
                            S.op("pe", lambda e, p1=p1, c=c, fc=fc, ntok=ntok: e.matmul(out=p1[:, 0:ntok], lhsT=W1[:, c, fc * P:(fc + 1) * P], rhs=hTb[:, c, 0:ntok], start=(c == 0), stop=(c == KD - 1)),
                                 reads=[b_W1, b_hTb], writes=[b_p1])
                        for c in range(KD):
                            S.op("pe", lambda e, p3=p3, c=c, fc=fc, ntok=ntok: e.matmul(out=p3[:, 0:ntok], lhsT=W3[:, c, fc * P:(fc + 1) * P], rhs=hTb[:, c, 0:ntok], start=(c == 0), stop=(c == KD - 1)),
                                 reads=[b_W3, b_hTb], writes=[b_p3])
                        ss_, b_ss = s1[fc % 2]
                        S.op("act", lambda e, ss_=ss_, p1=p1, ntok=ntok: e.activation(out=ss_[:, 0:ntok], in_=p1[:, 0:ntok], func=AF.Silu), reads=[b_p1], writes=[b_ss])
                        S.op("dve", lambda e, ss_=ss_, p3=p3, fc=fc, ntok=ntok: e.tensor_tensor(out=gT[:, fc, 0:ntok], in0=p3[:, 0:ntok], in1=ss_[:, 0:ntok], op=ALU.mult), reads=[b_p3, b_ss], writes=[b_gT])
                    for j in range(len(tiles)):
                        for n0 in range(0, D, 512):
                            py, b_py = self.next_ps()
                            for fc in range(FC):
                                S.op("pe", lambda e, py=py, fc=fc, j=j, n0=n0: e.matmul(out=py[:], lhsT=gT[:, fc, j * P:(j + 1) * P], rhs=W2[:, fc, n0:n0 + 512], start=(fc == 0), stop=(fc == FC - 1)),
                                     reads=[b_gT, b_W2], writes=[b_py])
                            if ex == 0:
                                S.op("dve", lambda e, py=py, j=j, n0=n0, ex=ex: e.tensor_scalar(out=yacc[:, j, n0:n0 + 512], in0=py[:], scalar1=gates[:, j, ex:ex + 1], scalar2=None, op0=ALU.mult),
                                     reads=[b_py, b_gates], writes=[b_yacc])
                            else:
                                S.op("dve", lambda e, py=py, j=j, n0=n0, ex=ex: e.scalar_tensor_tensor(out=yacc[:, j, n0:n0 + 512], in0=py[:], scalar=gates[:, j, ex:ex + 1], in1=yacc[:, j, n0:n0 + 512],
                                                                                                     op0=ALU.mult, op1=ALU.add), reads=[b_py, b_gates, b_yacc], writes=[b_yacc])
                for j, t in enumerate(tiles):
                    S.dma(self.Y1[t * P:(t + 1) * P, :], yacc[:, j, :], reads=[b_yacc])
        self.phase_end()
        if 'n' not in dbg:
            self.ln_pass(l, 2, self.Y1, make_h=False)


def build_program(cfg):
    import contextlib
    k = K(cfg)
    L, T, NCTX, DFF = cfg.L, cfg.T, cfg.NCTX, cfg.DFF
    k.inp("x", [T, D]); k.inp("ctx", [NCTX, D]); k.inp("cc", [P, KD, 2])
    k.inp("w_mod", [L, D, 6 * D]); k.inp("bm", [L, 2, 6 * D]); k.inp("w_in", [L, D, NIN]); k.inp("w_out", [L, D, D])
    k.inp("fn_w", [L, 1024, 1024]); k.inp("lnp", [L, 2, 2, D]); k.inp("qknw", [L, 2, HD]); k.inp("rope", [T, HD])
    k.inp("convw", [L, P, 36, 5]); k.inp("gdnp", [L, 2, 24]); k.inp("gdnp2", [L, 2, HD])
    k.inp("gmask", [2, 5, CH, GH, CH]); k.inp("gmask_i", [CH, GH, CH])
    k.inp("dft_ch", [P, 256], BF16); k.inp("dft_c", [T, T], BF16); k.inp("dft_ns", [T, T], BF16)
    k.inp("dftc_c", [NCTX, NCTX], BF16); k.inp("dftc_ns", [NCTX, NCTX], BF16)
    k.inp("wr", [L, D, 36]); k.inp("rb", [L, 2, 36])
    k.inp("w1", [L, NE * D, DFF]); k.inp("w3", [L, NE * D, DFF]); k.inp("w2", [L, NE * DFF, D])
    k.inp("identf", [P, P]); k.inp("sel", [2, 2 * P])
    _ns = _moe_geom(cfg)[-1]
    k.inp("ustr", [P, P]); k.inp("slotpos", [P, _ns]); k.inp("pidx", [P, 1]); k.inp("iot8", [P, 8])
    out = k.nc.dram_tensor("out", [T, D], F32, kind="ExternalOutput").ap()
    k.alloc_scratch()
    k.alloc_moe()
    with contextlib.ExitStack() as es:
        k.consts(es)
        k.p0_mods()
        k.load_x()
        for l in range(L):
            last = l == L - 1
            k.cast_w(k.ins["w_in"][l], k.WINB[l], D, NIN)
            k.p1_proj(l)
            k.p2_gdn_prep(l)
            k.p2_gdn_main(l)
            k.p2_gdn_gate(l)
            k.p3_fnet(l, ctx_out=not last)
            k.p4_attn(l, ctx_out=not last)
            k.cast_w(k.ins["w_out"][l], k.WOUTB[l], D, D)
            k.p5_out(l)
            k.cast_moe(l)
            k.p6_moe_routed(l)
        st = [k.sb(es, "fo%d" % i, [P, D]) for i in range(2)]
        for t in range(cfg.NTC, cfg.NT):
            s, b_s = st[t % 2]
            k.S.dma(s[:], k.XRES[t * P:(t + 1) * P, :], writes=[b_s])
            k.S.dma(out[(t - cfg.NTC) * P:(t - cfg.NTC + 1) * P, :], s[:], reads=[b_s])
        k.finish()
    return k


def host_consts(cfg):
    c = {}
    c["identf"] = np.eye(P, dtype=np.float32)
    sel = np.zeros((2, 2 * P), np.float32)
    sel[0, :P] = 1
    sel[1, P:] = 1
    c["sel"] = sel
    m = np.arange(128)
    ang = 2 * np.pi * np.outer(m, m) / 128
    c["dft_ch"] = (np.concatenate([np.cos(ang), np.sin(ang)], 1) / np.sqrt(128)).astype(ml_dtypes.bfloat16)
    for nm, n in (("dft", cfg.T), ("dftc", cfg.NCTX)):
        t = np.arange(n, dtype=np.int64)
        a = 2 * np.pi * (np.outer(t, t) % n) / n
        c[nm + "_c"] = (np.cos(a) / np.sqrt(n)).astype(ml_dtypes.bfloat16)
        c[nm + "_ns"] = (-np.sin(a) / np.sqrt(n)).astype(ml_dtypes.bfloat16)
    rows = cfg.T // 64
    row = np.repeat(np.arange(rows, dtype=np.float32), 64)
    col = np.tile(np.arange(64, dtype=np.float32), rows)
    inv = (10000.0 ** (-np.arange(0, 64, 2, dtype=np.float32) / 64)).astype(np.float32)
    ang = np.concatenate([row[:, None] * inv, col[:, None] * inv], -1)
    c["rope"] = np.concatenate([np.cos(ang), np.sin(ang)], -1).astype(np.float32)
    t = np.arange(CH)
    mk = np.zeros((2, 5, CH, CH), np.float32)
    mk[0, 0] = t[:, None] <= t[None, :]; mk[0, 1] = t[:, None] > t[None, :]; mk[0, 2] = t[:, None] > t[None, :]
    mk[0, 3] = t[None, :] > t[:, None]; mk[0, 4] = t[None, :] >= t[:, None]
    mk[1, 0] = t[:, None] >= t[None, :]; mk[1, 1] = t[:, None] < t[None, :]; mk[1, 2] = t[:, None] < t[None, :]
    mk[1, 3] = t[None, :] < t[:, None]; mk[1, 4] = t[None, :] <= t[:, None]
    c["gmask"] = np.repeat(mk[:, :, :, None, :], GH, 3).copy()
    c["gmask_i"] = np.repeat(np.eye(CH, dtype=np.float32)[:, None, :], GH, 1).copy()
    ns = _moe_geom(cfg)[-1]
    tt = np.arange(P)
    c["ustr"] = (tt[:, None] < tt[None, :]).astype(np.float32)
    c["slotpos"] = np.tile((np.arange(ns, dtype=np.float32) * P)[None, :], (P, 1))
    c["pidx"] = np.arange(P, dtype=np.float32)[:, None].copy()
    c["iot8"] = np.tile(np.arange(8, dtype=np.float32)[None, :], (P, 1))
    return c


def host_inputs(cfg, inp, b, consts):
    L = cfg.L
    m = dict(consts)
    m["x"] = np.ascontiguousarray(inp["x"][b])
    m["ctx"] = np.ascontiguousarray(inp["ctx"][b])
    m["cc"] = np.ascontiguousarray(np.stack([inp["c"][b], inp["c_ctx"]], -1).reshape(KD, P, 2).transpose(1, 0, 2))
    m["w_mod"] = inp["w_mod"]
    m["bm"] = np.ascontiguousarray(np.stack([inp["b_mod"], inp["b_mod"]], 1))
    m["w_in"] = inp["w_in"]
    m["w_out"] = inp["w_out"]
    m["fn_w"] = inp["fn_w"]
    m["lnp"] = np.ascontiguousarray(np.stack([np.stack([inp["ln1_g"], inp["ln1_b"]], 1), np.stack([inp["ln2_g"], inp["ln2_b"]], 1)], 1))
    m["qknw"] = np.ascontiguousarray(np.stack([inp["q_norm"], inp["k_norm"]], 1))
    m["convw"] = np.ascontiguousarray(inp["gdn_conv"].reshape(L, 5, 36, P).transpose(0, 3, 2, 1))
    m["gdnp"] = np.ascontiguousarray(np.stack([inp["gdn_a_log"].reshape(L, 24), inp["gdn_dt_bias"].reshape(L, 24)], 1))
    m["gdnp2"] = np.ascontiguousarray(np.stack([inp["gdn_norm"], inp["gdn_norm"]], 1))
    m["wr"] = np.ascontiguousarray(np.concatenate([inp["router_g"], inp["router_e"]], -1))
    rbv = np.concatenate([inp["router_g_b"], inp["router_e_b"]], -1)
    m["rb"] = np.ascontiguousarray(np.stack([rbv, rbv], 1))
    m["w1"] = inp["w1"].reshape(L, NE * D, cfg.DFF)
    m["w3"] = inp["w3"].reshape(L, NE * D, cfg.DFF)
    m["w2"] = inp["w2"].reshape(L, NE * cfg.DFF, D)
    return m


_PROG = {}


def kernel(**inputs):
    inp = {k_: np.asarray(v) for k_, v in inputs.items()}
    B, T, _ = inp["x"].shape
    NCTX = inp["ctx"].shape[1]
    DFF = inp["w1"].shape[-1]
    cfg = Cfg(T=T, NCTX=NCTX, DFF=DFF, L=inp["w_mod"].shape[0])
    key = (T, NCTX, DFF, cfg.L)
    if key not in _PROG:
        _PROG[key] = build_program(cfg)
    k = _PROG[key]
    consts = host_consts(cfg)
    in_maps = [host_inputs(cfg, inp, b, consts) for b in range(B)]
    res = run_bass_kernel_spmd(k.nc, in_maps, core_ids=list(range(B)))
    return np.stack([res.results[b]["out"] for b in range(B)], 0).astype(np.float32)


U32 = mybir.dt.uint32


def _dma_custom(self, q, fn, reads, writes):
    d = "d%d" % self.dma_rr
    self.dma_rr = (self.dma_rr + 1) % 8
    if self.cnt[d] > 0 and self.waited.get((q, d), 0) < self.cnt[d]:
        self.prog[q].append(("wait", self.sem[d], self.cnt[d]))
        self.waited[(q, d)] = self.cnt[d]
    self._deps(q, reads, writes)
    self.cnt[d] += 16
    c = self.cnt[d]
    self.prog[q].append(("ins", fn, self.sem[d], 16))
    for b in reads:
        b.r[d] = c
    for b in writes:
        b.w = (d, c)
        b.r = {}


Sched.dma_custom = _dma_custom


def _moe_geom(cfg):
    DFF = cfg.DFF
    FC = DFF // P
    NPC1 = max(1, (KD * DFF * 2) // 16384)
    CPP = KD // NPC1
    NPC2 = max(1, (FC * D * 2) // 16384)
    FPP = FC // NPC2
    NS = (2 * cfg.S) // P + NE
    return FC, NPC1, CPP, NPC2, FPP, NS


def _alloc_moe(self):
    cfg = self.cfg
    FC, NPC1, CPP, NPC2, FPP, NS = _moe_geom(cfg)
    self.W1R = [[self.scratch("w1r%d_%d" % (l, i), [NE * P, CPP * cfg.DFF], BF16)[0] for i in range(NPC1)] for l in range(cfg.L)]
    self.W3R = [[self.scratch("w3r%d_%d" % (l, i), [NE * P, CPP * cfg.DFF], BF16)[0] for i in range(NPC1)] for l in range(cfg.L)]
    self.W2R = [[self.scratch("w2r%d_%d" % (l, i), [NE * P, FPP * D], BF16)[0] for i in range(NPC2)] for l in range(cfg.L)]
    HD2 = D // 2
    self.HS = [self.scratch("hs%d" % i, [NS * P, HD2])[0] for i in range(2)]
    self.YS = [self.scratch("ys%d" % i, [NS * P, HD2])[0] for i in range(2)]


def _cast_moe(self, l):
    import contextlib
    S, cfg = self.S, self.cfg
    DFF = cfg.DFF
    FC, NPC1, CPP, NPC2, FPP, NS = _moe_geom(cfg)
    items = []
    for src, dstl in ((self.ins["w1"][l], self.W1R[l]), (self.ins["w3"][l], self.W3R[l])):
        for e in range(NE):
            for c4 in range(0, KD, 4):
                s_ = src[e * D + c4 * P:e * D + (c4 + 4) * P, :].rearrange("(c p) f -> p c f", p=P)
                pc, off = c4 // CPP, (c4 % CPP) * DFF
                d_ = dstl[pc][e * P:(e + 1) * P, off:off + 4 * DFF].rearrange("p (c f) -> p c f", c=4)
                items.append((s_, d_, [P, 4, DFF]))
    for e in range(NE):
        for fc in range(FC):
            for n0 in range(0, D, 2048):
                s_ = self.ins["w2"][l][e * DFF + fc * P:e * DFF + (fc + 1) * P, n0:n0 + 2048]
                pc, off = fc // FPP, (fc % FPP) * D + n0
                d_ = self.W2R[l][pc][e * P:(e + 1) * P, off:off + 2048]
                items.append((s_, d_, [P, 2048]))
    with contextlib.ExitStack() as es:
        st = [self.sb(es, "cm_f%d" % i, [P, 2048]) for i in range(3)]
        sbf = [self.sb(es, "cm_b%d" % i, [P, 2048], BF16) for i in range(3)]
        engs = ["dve", "pool", "act"]
        for i, (s_, d_, shp) in enumerate(items):
            f, b_f = st[i % 3]
            o, b_o = sbf[i % 3]
            n = int(np.prod(shp[1:]))
            fv = f[:, 0:n] if len(shp) == 2 else f[:, 0:n].rearrange("p (c f) -> p c f", c=shp[1])
            ov = o[:, 0:n] if len(shp) == 2 else o[:, 0:n].rearrange("p (c f) -> p c f", c=shp[1])
            S.dma(fv, s_, writes=[b_f])
            en = engs[i % 3]
            if en == "act":
                S.op("act", lambda e, f=f, o=o, n=n: e.copy(out=o[:, 0:n], in_=f[:, 0:n]), reads=[b_f], writes=[b_o])
            else:
                S.op(en, lambda e, f=f, o=o, n=n: e.tensor_copy(out=o[:, 0:n], in_=f[:, 0:n]), reads=[b_f], writes=[b_o])
            S.dma(d_, ov, reads=[b_o])
    self.phase_end()


def _p6_moe_routed(self, l):
    import contextlib
    S, cfg = self.S, self.cfg
    DFF, NT = cfg.DFF, cfg.NT
    FC, NPC1, CPP, NPC2, FPP, NS = _moe_geom(cfg)
    IOA = bass.IndirectOffsetOnAxis
    with contextlib.ExitStack() as es:
        wr, b_wr = self.sb(es, "wr", [P, KD, 36])
        S.dma(wr[:], self.ins["wr"][l].rearrange("(c p) n -> p c n", p=P), writes=[b_wr])
        rb, b_rb = self.bcast_tile(es, "rb", self.ins["rb"][l], 0, 36)
        ustr, b_ustr = self.sb(es, "ustr", [P, P])
        S.dma(ustr[:], self.ins["ustr"], writes=[b_ustr])
        onesf, b_onesf = self.sb(es, "r_ones", [P, P])
        S.op("dve", lambda e: e.memset(onesf[:], 1.0), writes=[b_onesf])
        slotpos, b_slotpos = self.sb(es, "slotpos", [P, NS])
        S.dma(slotpos[:], self.ins["slotpos"], writes=[b_slotpos])
        pidx, b_pidx = self.sb(es, "pidx", [P, 1])
        S.dma(pidx[:], self.ins["pidx"], writes=[b_pidx])
        iot, b_iot = self.sb(es, "iot", [P, 8])
        S.dma(iot[:], self.ins["iot8"], writes=[b_iot])
        h, b_h = self.sb(es, "r_h", [P, D])
        hTf, b_hTf = self.sb(es, "r_hTf", [P, KD, P])
        OH, b_OH = self.sb(es, "r_OH", [P, NT, 2, NE])
        G12, b_G12 = self.G12, self.b_G12
        RANK, b_RANK = self.sb(es, "r_rank", [P, NT, 2])
        carry, b_carry = self.sb(es, "r_carry", [P, NE])
        S.op("dve", lambda e: e.memset(carry[:], 0.0), writes=[b_carry])
        tmp, b_tmp = self.sb(es, "r_tmp", [P, NE])
        sm = {nm: self.sb(es, "r_" + nm, shp) for nm, shp in (("lg", [P, 36]), ("gmax", [P, 1]), ("ngmax", [P, 1]), ("ohg", [P, 4]), ("ex", [P, 4]), ("se", [P, 1]),
                                                             ("les", [P, 8]), ("m1", [P, 1]), ("oh1", [P, 8]), ("le2", [P, 8]), ("m2", [P, 1]), ("oh2", [P, 8]),
                                                             ("dd", [P, 1]), ("w1", [P, 1]), ("w2", [P, 1]))}
        V = {k_: v_[0] for k_, v_ in sm.items()}
        Bf = {k_: v_[1] for k_, v_ in sm.items()}

        def dv(fn, r, w):
            S.op("dve", fn, reads=[Bf[x] if isinstance(x, str) else x for x in r], writes=[Bf[x] if isinstance(x, str) else x for x in w])
        for t in range(NT):
            S.dma(h[:], self.HM[t * P:(t + 1) * P, :], writes=[b_h])
            for c4 in range(0, KD, 4):
                ps, b_ps = self.next_ps()
                for c in range(c4, c4 + 4):
                    S.op("pe", lambda e, ps=ps, c=c, c4=c4: e.transpose(out=ps[:, (c - c4) * P:(c - c4 + 1) * P], in_=h[:, c * P:(c + 1) * P], identity=self.identf[:]),
                         reads=[b_h, self.b_identf], writes=[b_ps])
                S.op("act", lambda e, ps=ps, c4=c4: e.copy(out=hTf[:, c4:c4 + 4, :], in_=ps[:].rearrange("p (c t) -> p c t", c=4)), reads=[b_ps], writes=[b_hTf])
            ps, b_ps = self.next_ps()
            for c in range(KD):
                S.op("pe", lambda e, ps=ps, c=c: e.matmul(out=ps[:, 0:36], lhsT=hTf[:, c, :], rhs=wr[:, c, :], start=(c == 0), stop=(c == KD - 1)), reads=[b_hTf, b_wr], writes=[b_ps])
            dv(lambda e, ps=ps: e.tensor_tensor(out=V["lg"][:], in0=ps[:, 0:36], in1=rb[:], op=ALU.add), [b_ps, b_rb], ["lg"])
            dv(lambda e: e.tensor_reduce(out=V["gmax"][:], in_=V["lg"][:, 0:4], axis=AX.X, op=ALU.max), ["lg"], ["gmax"])
            dv(lambda e: e.tensor_scalar(out=V["ohg"][:], in0=V["lg"][:, 0:4], scalar1=V["gmax"][:, 0:1], scalar2=None, op0=ALU.is_ge), ["lg", "gmax"], ["ohg"])
            dv(lambda e: e.tensor_scalar(out=V["ngmax"][:], in0=V["gmax"][:], scalar1=-1.0, scalar2=None, op0=ALU.mult), ["gmax"], ["ngmax"])
            S.op("act", lambda e: e.activation(out=V["ex"][:], in_=V["lg"][:, 0:4], func=AF.Exp, bias=V["ngmax"][:, 0:1], scale=1.0), reads=[Bf["lg"], Bf["ngmax"]], writes=[Bf["ex"]])
            dv(lambda e: e.tensor_reduce(out=V["se"][:], in_=V["ex"][:], axis=AX.X, op=ALU.add), ["ex"], ["se"])
            dv(lambda e: e.reciprocal(out=V["se"][:], in_=V["se"][:]), ["se"], ["se"])
            dv(lambda e: e.tensor_scalar(out=V["les"][:], in0=V["lg"][:, 4:12], scalar1=V["ohg"][:, 0:1], scalar2=None, op0=ALU.mult), ["lg", "ohg"], ["les"])
            for g in range(1, 4):
                dv(lambda e, g=g: e.scalar_tensor_tensor(out=V["les"][:], in0=V["lg"][:, 4 + 8 * g:12 + 8 * g], scalar=V["ohg"][:, g:g + 1], in1=V["les"][:], op0=ALU.mult, op1=ALU.add),
                   ["lg", "ohg", "les"], ["les"])
            dv(lambda e: e.tensor_reduce(out=V["m1"][:], in_=V["les"][:], axis=AX.X, op=ALU.max), ["les"], ["m1"])
            dv(lambda e: e.tensor_scalar(out=V["oh1"][:], in0=V["les"][:], scalar1=V["m1"][:, 0:1], scalar2=None, op0=ALU.is_ge), ["les", "m1"], ["oh1"])
            dv(lambda e: e.scalar_tensor_tensor(out=V["le2"][:], in0=V["oh1"][:], scalar=-1e30, in1=V["les"][:], op0=ALU.mult, op1=ALU.add), ["oh1", "les"], ["le2"])
            dv(lambda e: e.tensor_reduce(out=V["m2"][:], in_=V["le2"][:], axis=AX.X, op=ALU.max), ["le2"], ["m2"])
            dv(lambda e: e.tensor_scalar(out=V["oh2"][:], in0=V["le2"][:], scalar1=V["m2"][:, 0:1], scalar2=None, op0=ALU.is_ge), ["le2", "m2"], ["oh2"])
            dv(lambda e: e.tensor_tensor(out=V["dd"][:], in0=V["m2"][:], in1=V["m1"][:], op=ALU.subtract), ["m1", "m2"], ["dd"])
            S.op("act", lambda e: e.activation(out=V["dd"][:], in_=V["dd"][:], func=AF.Exp), reads=[Bf["dd"]], writes=[Bf["dd"]])
            dv(lambda e: e.tensor_scalar(out=V["w1"][:], in0=V["dd"][:], scalar1=1.0, scalar2=None, op0=ALU.add), ["dd"], ["w1"])
            dv(lambda e: e.reciprocal(out=V["w1"][:], in_=V["w1"][:]), ["w1"], ["w1"])
            dv(lambda e: e.tensor_tensor(out=V["w2"][:], in0=V["dd"][:], in1=V["w1"][:], op=ALU.mult), ["dd", "w1"], ["w2"])
            dv(lambda e, t=t: e.tensor_tensor(out=G12[:, t, 0:1], in0=V["w1"][:], in1=V["se"][:], op=ALU.mult), ["w1", "se"], [b_G12])
            dv(lambda e, t=t: e.tensor_tensor(out=G12[:, t, 1:2], in0=V["w2"][:], in1=V["se"][:], op=ALU.mult), ["w2", "se"], [b_G12])
            for kk, ohn in ((0, "oh1"), (1, "oh2")):
                for g in range(4):
                    dv(lambda e, g=g, t=t, kk=kk, ohn=ohn: e.tensor_scalar(out=OH[:, t, kk, 8 * g:8 * g + 8], in0=V[ohn][:], scalar1=V["ohg"][:, g:g + 1], scalar2=None, op0=ALU.mult),
                       [ohn, "ohg"], [b_OH])
                ps2, b_ps2 = self.next_ps()
                S.op("pe", lambda e, ps2=ps2, t=t, kk=kk: e.matmul(out=ps2[:, 0:NE], lhsT=ustr[:], rhs=OH[:, t, kk, :], start=True, stop=True), reads=[b_ustr, b_OH], writes=[b_ps2])
                S.op("pe", lambda e, ps2=ps2, t=t, kk=kk: e.matmul(out=ps2[:, NE:2 * NE], lhsT=onesf[:], rhs=OH[:, t, kk, :], start=True, stop=True), reads=[b_onesf, b_OH], writes=[b_ps2])
                dv(lambda e, ps2=ps2: e.tensor_tensor(out=tmp[:], in0=ps2[:, 0:NE], in1=carry[:], op=ALU.add), [b_ps2, b_carry], [b_tmp])
                dv(lambda e, t=t, kk=kk: e.tensor_tensor(out=tmp[:], in0=tmp[:], in1=OH[:, t, kk, :], op=ALU.mult), [b_tmp, b_OH], [b_tmp])
                dv(lambda e, t=t, kk=kk: e.tensor_reduce(out=RANK[:, t, kk:kk + 1], in_=tmp[:], axis=AX.X, op=ALU.add), [b_tmp], [b_RANK])
                dv(lambda e, ps2=ps2: e.tensor_tensor(out=carry[:], in0=carry[:], in1=ps2[:, NE:2 * NE], op=ALU.add), [b_carry, b_ps2], [b_carry])
        ci, b_ci = self.sb(es, "r_ci", [P, NE], I32)
        pad, b_pad = self.sb(es, "r_pad", [P, NE])
        padT, b_padT = self.sb(es, "r_padT", [NE, P])
        base, b_base = self.sb(es, "r_base", [P, NE])
        bend, b_bend = self.sb(es, "r_bend", [P, NE])
        dv(lambda e: e.tensor_scalar(out=carry[:], in0=carry[:], scalar1=127.0, scalar2=None, op0=ALU.add), [b_carry], [b_carry])
        dv(lambda e: e.tensor_copy(out=ci[:], in_=carry[:]), [b_carry], [b_ci])
        dv(lambda e: e.tensor_scalar(out=ci[:], in0=ci[:], scalar1=7, scalar2=7, op0=ALU.arith_shift_right, op1=ALU.logical_shift_left), [b_ci], [b_ci])
        dv(lambda e: e.tensor_copy(out=pad[:], in_=ci[:]), [b_ci], [b_pad])
        ps, b_ps = self.next_ps()
        S.op("pe", lambda e, ps=ps: e.transpose(out=ps[0:NE, 0:P], in_=pad[:], identity=self.identf[:]), reads=[b_pad, self.b_identf], writes=[b_ps])
        S.op("act", lambda e, ps=ps: e.copy(out=padT[:], in_=ps[0:NE, 0:P]), reads=[b_ps], writes=[b_padT])
        ps, b_ps = self.next_ps()
        S.op("pe", lambda e, ps=ps: e.matmul(out=ps[:, 0:NE], lhsT=padT[:], rhs=ustr[0:NE, 0:NE], start=True, stop=True), reads=[b_padT, b_ustr], writes=[b_ps])
        dv(lambda e, ps=ps: e.tensor_copy(out=base[:], in_=ps[:, 0:NE]), [b_ps], [b_base])
        dv(lambda e: e.tensor_tensor(out=bend[:], in0=base[:], in1=pad[:], op=ALU.add), [b_base, b_pad], [b_bend])
        big3, b_big3 = self.sb(es, "r_big3", [P, max(NS, 2 * NT), NE])
        posf, b_posf = self.sb(es, "r_posf", [P, NT, 2])
        OHv = OH[:].rearrange("p t k e -> p (t k) e")
        dv(lambda e: e.tensor_tensor(out=big3[:, 0:2 * NT, :], in0=OHv, in1=base[:].unsqueeze(1).broadcast_to([P, 2 * NT, NE]), op=ALU.mult), [b_OH, b_base], [b_big3])
        dv(lambda e: e.tensor_reduce(out=posf[:].rearrange("p t k -> p (t k)"), in_=big3[:, 0:2 * NT, :], axis=AX.X, op=ALU.add), [b_big3], [b_posf])
        dv(lambda e: e.tensor_tensor(out=posf[:], in0=posf[:], in1=RANK[:], op=ALU.add), [b_posf, b_RANK], [b_posf])
        POSI, b_POSI = self.POSI, self.b_POSI
        dv(lambda e: e.tensor_scalar(out=posf[:], in0=posf[:], scalar1=0.0, scalar2=float(NS * P - 1), op0=ALU.max, op1=ALU.min), [b_posf], [b_posf])
        dv(lambda e: e.tensor_copy(out=POSI[:], in_=posf[:]), [b_posf], [b_POSI])
        esl, b_esl = self.sb(es, "r_esl", [P, NS])
        dv(lambda e: e.tensor_tensor(out=big3[:, 0:NS, :], in0=slotpos[:].unsqueeze(2).broadcast_to([P, NS, NE]), in1=bend[:].unsqueeze(1).broadcast_to([P, NS, NE]), op=ALU.is_ge),
           [b_slotpos, b_bend], [b_big3])
        dv(lambda e: e.tensor_reduce(out=esl[:], in_=big3[:, 0:NS, :], axis=AX.X, op=ALU.add), [b_big3], [b_esl])
        dv(lambda e: e.tensor_scalar(out=esl[:], in0=esl[:], scalar1=0.0, scalar2=float(NE - 1), op0=ALU.max, op1=ALU.min), [b_esl], [b_esl])
        dv(lambda e: e.tensor_scalar(out=esl[:], in0=esl[:], scalar1=float(P), scalar2=None, op0=ALU.mult), [b_esl], [b_esl])
        dv(lambda e: e.tensor_scalar(out=esl[:], in0=esl[:], scalar1=pidx[:, 0:1], scalar2=None, op0=ALU.add), [b_esl, b_pidx], [b_esl])
        IDXI, b_IDXI = self.IDXI, self.b_IDXI
        dv(lambda e: e.tensor_copy(out=IDXI[:], in_=esl[:]), [b_esl], [b_IDXI])
        hs2 = [self.sb(es, "r_hs%d" % i, [P, D]) for i in range(2)]
        for t in range(NT):
            hh, b_hh = hs2[t % 2]
            S.dma(hh[:], self.HM[t * P:(t + 1) * P, :], writes=[b_hh])
            for kk in range(2):
                off = IOA(ap=POSI[:, t, kk:kk + 1].bitcast(U32), axis=0)
                for hf in range(2):
                    S.dma_custom("pool", lambda eng, hh=hh, off=off, hf=hf: eng.indirect_dma_start(out=self.HS[hf], out_offset=off, in_=hh[:, hf * (D // 2):(hf + 1) * (D // 2)], in_offset=None),
                                 [b_hh, b_POSI], [])
    self.phase_end()
    with contextlib.ExitStack() as es:
        W1, b_W1 = self.sb(es, "s_W1", [P, KD * DFF], BF16)
        W3, b_W3 = self.sb(es, "s_W3", [P, KD * DFF], BF16)
        W2, b_W2 = self.sb(es, "s_W2", [P, FC * D], BF16)
        hs = [self.sb(es, "s_hs%d" % i, [P, D]) for i in range(2)]
        hT, b_hT = self.sb(es, "s_hT", [P, KD, P], BF16)
        s1, b_s1 = self.sb(es, "s_s1", [P, DFF])
        gg, b_gg = self.sb(es, "s_g", [P, DFF])
        gT, b_gT = self.sb(es, "s_gT", [P, FC, P], BF16)
        ysb = [self.sb(es, "s_y%d" % i, [P, D]) for i in range(2)]
        IDXI, b_IDXI = self.IDXI, self.b_IDXI
        PC1W, PC2W = CPP * DFF, FPP * D
        for s in range(NS):
            off = IOA(ap=IDXI[:, s:s + 1].bitcast(U32), axis=0)
            for pc in range(NPC1):
                S.dma_custom("pool", lambda eng, pc=pc, off=off: eng.indirect_dma_start(out=W1[:, pc * PC1W:(pc + 1) * PC1W], out_offset=None, in_=self.W1R[l][pc], in_offset=off), [b_IDXI], [b_W1])
                S.dma_custom("pool", lambda eng, pc=pc, off=off: eng.indirect_dma_start(out=W3[:, pc * PC1W:(pc + 1) * PC1W], out_offset=None, in_=self.W3R[l][pc], in_offset=off), [b_IDXI], [b_W3])
            for pc in range(NPC2):
                S.dma_custom("pool", lambda eng, pc=pc, off=off: eng.indirect_dma_start(out=W2[:, pc * PC2W:(pc + 1) * PC2W], out_offset=None, in_=self.W2R[l][pc], in_offset=off), [b_IDXI], [b_W2])
            x, b_x = hs[s % 2]
            for hf in range(2):
                S.dma(x[:, hf * (D // 2):(hf + 1) * (D // 2)], self.HS[hf][s * P:(s + 1) * P, :], writes=[b_x])
            for c4 in range(0, KD, 4):
                ps, b_ps = self.next_ps()
                for c in range(c4, c4 + 4):
                    S.op("pe", lambda e, ps=ps, x=x, c=c, c4=c4: e.transpose(out=ps[:, (c - c4) * P:(c - c4 + 1) * P], in_=x[:, c * P:(c + 1) * P], identity=self.identf[:]),
                         reads=[b_x, self.b_identf], writes=[b_ps])
                if (c4 // 4) % 2 == 0:
                    S.op("act", lambda e, ps=ps, c4=c4: e.copy(out=hT[:, c4:c4 + 4, :], in_=ps[:].rearrange("p (c t) -> p c t", c=4)), reads=[b_ps], writes=[b_hT])
                else:
                    S.op("dve", lambda e, ps=ps, c4=c4: e.tensor_copy(out=hT[:, c4:c4 + 4, :], in_=ps[:].rearrange("p (c t) -> p c t", c=4)), reads=[b_ps], writes=[b_hT])
            p1, b_p1 = self.next_ps()
            p3, b_p3 = self.next_ps()
            for c in range(KD):
                S.op("pe", lambda e, p1=p1, c=c: e.matmul(out=p1[:, 0:DFF], lhsT=hT[:, c, :], rhs=W1[:, c * DFF:(c + 1) * DFF], start=(c == 0), stop=(c == KD - 1)), reads=[b_hT, b_W1], writes=[b_p1])
            for c in range(KD):
                S.op("pe", lambda e, p3=p3, c=c: e.matmul(out=p3[:, 0:DFF], lhsT=hT[:, c, :], rhs=W3[:, c * DFF:(c + 1) * DFF], start=(c == 0), stop=(c == KD - 1)), reads=[b_hT, b_W3], writes=[b_p3])
            S.op("act", lambda e, p1=p1: e.activation(out=s1[:], in_=p1[:, 0:DFF], func=AF.Silu), reads=[b_p1], writes=[b_s1])
            S.op("dve", lambda e, p3=p3: e.tensor_tensor(out=gg[:], in0=p3[:, 0:DFF], in1=s1[:], op=ALU.mult), reads=[b_p3, b_s1], writes=[b_gg])
            ps, b_ps = self.next_ps()
            for fc in range(FC):
                S.op("pe", lambda e, ps=ps, fc=fc: e.transpose(out=ps[:, fc * P:(fc + 1) * P], in_=gg[:, fc * P:(fc + 1) * P], identity=self.identf[:]), reads=[b_gg, self.b_identf], writes=[b_ps])
            S.op("act", lambda e, ps=ps: e.copy(out=gT[:], in_=ps[:, 0:FC * P].rearrange("p (c t) -> p c t", c=FC)), reads=[b_ps], writes=[b_gT])
            y, b_y = ysb[s % 2]
            for ni, n0 in enumerate(range(0, D, 512)):
                py, b_py = self.next_ps()
                for fc in range(FC):
                    S.op("pe", lambda e, py=py, fc=fc, n0=n0: e.matmul(out=py[:], lhsT=gT[:, fc, :], rhs=W2[:, fc * D + n0:fc * D + n0 + 512], start=(fc == 0), stop=(fc == FC - 1)),
                         reads=[b_gT, b_W2], writes=[b_py])
                if ni % 2 == 0:
                    S.op("act", lambda e, py=py, y=y, n0=n0: e.copy(out=y[:, n0:n0 + 512], in_=py[:]), reads=[b_py], writes=[b_y])
                else:
                    S.op("dve", lambda e, py=py, y=y, n0=n0: e.tensor_copy(out=y[:, n0:n0 + 512], in_=py[:]), reads=[b_py], writes=[b_y])
            for hf in range(2):
                S.dma(self.YS[hf][s * P:(s + 1) * P, :], y[:, hf * (D // 2):(hf + 1) * (D // 2)], reads=[b_y])
    self.phase_end()
    with contextlib.ExitStack() as es:
        ya = [self.sb(es, "u_a%d" % i, [P, D]) for i in range(2)]
        yb = [self.sb(es, "u_b%d" % i, [P, D]) for i in range(2)]
        POSI, b_POSI = self.POSI, self.b_POSI
        G12, b_G12 = self.G12, self.b_G12
        for t in range(NT):
            a, b_a = ya[t % 2]
            b2, b_b2 = yb[t % 2]
            offa = IOA(ap=POSI[:, t, 0:1].bitcast(U32), axis=0)
            offb = IOA(ap=POSI[:, t, 1:2].bitcast(U32), axis=0)
            for hf in range(2):
                S.dma_custom("pool", lambda eng, a=a, offa=offa, hf=hf: eng.indirect_dma_start(out=a[:, hf * (D // 2):(hf + 1) * (D // 2)], out_offset=None, in_=self.YS[hf], in_offset=offa), [b_POSI], [b_a])
                S.dma_custom("pool", lambda eng, b2=b2, offb=offb, hf=hf: eng.indirect_dma_start(out=b2[:, hf * (D // 2):(hf + 1) * (D // 2)], out_offset=None, in_=self.YS[hf], in_offset=offb), [b_POSI], [b_b2])
            S.op("dve", lambda e, a=a, t=t: e.tensor_scalar(out=a[:], in0=a[:], scalar1=G12[:, t, 0:1], scalar2=None, op0=ALU.mult), reads=[b_a, b_G12], writes=[b_a])
            S.op("dve", lambda e, a=a, b2=b2, t=t: e.scalar_tensor_tensor(out=a[:], in0=b2[:], scalar=G12[:, t, 1:2], in1=a[:], op0=ALU.mult, op1=ALU.add), reads=[b_a, b_b2, b_G12], writes=[b_a])
            S.dma(self.Y1[t * P:(t + 1) * P, :], a[:], reads=[b_a])
    self.phase_end()
    self.ln_pass(l, 2, self.Y1, make_h=False)


K.alloc_moe = _alloc_moe
K.cast_moe = _cast_moe
K.p6_moe_routed = _p6_moe_routed
```
